# Optimizing a Trainium2 kernel written in Bass

```python
import jax
import jax.numpy as jnp
from jax import lax
import numpy as np

D_MODEL = 1024
BATCH = 8
SEQ = 2048
DEPTH = 4

GRID_W = 64
CTX_LEN = 256
HEAD_DIM = D_MODEL // 16
A_HEADS = 4
B_HEADS = 4
C_HEADS = 8
A_WIDTH = A_HEADS * HEAD_DIM
B_WIDTH = B_HEADS * HEAD_DIM
C_WIDTH = C_HEADS * HEAD_DIM
MIX_WIDTH = A_WIDTH + B_WIDTH + C_WIDTH
HGRN_CHUNK = 16
HGRN_F_FLOOR = 1e-20
RWKV_W_RANK = 64
RWKV_A_RANK = 64
RWKV_G_RANK = 128
RWKV_LN_EPS = 64e-5
NA_WIN_ROWS = 8
NA_WIN_COLS = 16
NA_COL_BLOCK = 16
NA_KEY_COLS = NA_COL_BLOCK + NA_WIN_COLS
ROPE_THETA = 10000.0
N_GROUPS = 4
EXPERTS_PER_GROUP = 8
TOP_K_IN_GROUP = 2
D_EXPERT = 512
EPS = 1e-6
NEG_INF = -1e30
A_PROJ = 5 * A_WIDTH
B_PROJ = 3 * B_WIDTH + 2 * RWKV_W_RANK + 2 * RWKV_A_RANK + RWKV_G_RANK
C_PROJ = 3 * C_WIDTH
P_TOTAL = A_PROJ + B_PROJ + C_PROJ
RWKV_SPLITS = (B_WIDTH, 2 * B_WIDTH, 3 * B_WIDTH, 3 * B_WIDTH + RWKV_W_RANK, 3 * B_WIDTH + 2 * RWKV_W_RANK,
               3 * B_WIDTH + 2 * RWKV_W_RANK + RWKV_A_RANK, 3 * B_WIDTH + 2 * RWKV_W_RANK + 2 * RWKV_A_RANK)

kernel_name = 'hybrid_diffusion_trunk'


def rms_norm(x, gain):
    xf = x.astype(jnp.float32)
    return xf * lax.rsqrt(jnp.mean(xf * xf, axis=-1, keepdims=True) + EPS) * gain.astype(jnp.float32)


def to_heads(t, n_heads):
    b, t_len, _ = t.shape
    return t.reshape(b, t_len, n_heads, HEAD_DIM).transpose(0, 2, 1, 3)


def chunk_gla(q, k, v, log_f, s0):
    b, h, t_len, _ = q.shape
    n = t_len // HGRN_CHUNK

    def blocks(u):
        return u.reshape(b, h, n, HGRN_CHUNK, u.shape[-1])

    q, k, v, log_f = blocks(q), blocks(k), blocks(v), blocks(log_f)
    cum = jnp.cumsum(log_f, axis=3)
    tri = jnp.tril(jnp.ones((HGRN_CHUNK, HGRN_CHUNK), dtype=bool))[:, :, None]
    diff = cum[:, :, :, :, None, :] - cum[:, :, :, None, :, :]
    decay = jnp.where(tri, jnp.exp(jnp.where(tri, diff, 0.0)), 0.0)
    scores = jnp.einsum('bhntd,bhnsd,bhntsd->bhnts', q, k, decay)
    o_intra = jnp.einsum('bhnts,bhnsv->bhntv', scores, v)
    q_in = q * jnp.exp(cum)
    k_out = k * jnp.exp(cum[:, :, :, -1:, :] - cum)
    kv = jnp.einsum('bhnsd,bhnsv->bhndv', k_out, v)
    chunk_decay = jnp.exp(cum[:, :, :, -1, :])

    def step(s, inp):
        q_c, dec_c, kv_c = inp
        o_c = jnp.einsum('bhtd,bhdv->bhtv', q_c, s)
        return s * dec_c[..., None] + kv_c, o_c

    s_fin, o_inter = lax.scan(step, s0, (jnp.moveaxis(q_in, 2, 0), jnp.moveaxis(chunk_decay, 2, 0),
                                         jnp.moveaxis(kv, 2, 0)))
    o = o_intra + jnp.moveaxis(o_inter, 0, 2)
    return o.reshape(b, h, t_len, v.shape[-1]), s_fin


def hgrn2_forget(f_pre, lb):
    f = lb + (1.0 - lb) * jax.nn.sigmoid(f_pre)
    log_f = jnp.log(jnp.maximum(f, HGRN_F_FLOOR))
    k = (1.0 - lb) * jax.nn.sigmoid(-f_pre)
    return to_heads(log_f, A_HEADS), to_heads(k, A_HEADS)


def hgrn2_mixer(p_ctx, p_lat, lb_fwd, lb_bwd, gn_gain):
    qc, ffc, fbc, ic, gc = jnp.split(p_ctx, 5, axis=-1)
    ql, ffl, fbl, il, gl = jnp.split(p_lat, 5, axis=-1)
    qc, ic, ql, il = (to_heads(t, A_HEADS) for t in (qc, ic, ql, il))
    s0 = jnp.zeros((p_lat.shape[0], A_HEADS, HEAD_DIM, HEAD_DIM), jnp.float32)

    def flip(t):
        return t[:, :, ::-1]

    lf, kf = hgrn2_forget(ffc, lb_fwd)
    oc_f, s_f = chunk_gla(qc, kf, ic, lf, s0)
    lf, kf = hgrn2_forget(ffl, lb_fwd)
    ol_f, _ = chunk_gla(ql, kf, il, lf, s_f)
    lbk, kb = hgrn2_forget(fbc, lb_bwd)
    oc_b, s_b = chunk_gla(flip(qc), flip(kb), flip(ic), flip(lbk), s0)
    lbk, kb = hgrn2_forget(fbl, lb_bwd)
    ol_b, _ = chunk_gla(flip(ql), flip(kb), flip(il), flip(lbk), s_b)

    def readout(o, g):
        o = rms_norm(o.transpose(0, 2, 1, 3), gn_gain)
        return o.reshape(g.shape) * jax.nn.silu(g)

    return readout(ol_f + flip(ol_b), gl), readout(oc_f + flip(oc_b), gc)


def centred_shift(u):
    pad = jnp.pad(u, ((0, 0), (1, 1), (0, 0)))
    return 0.5 * (pad[:, :-2] + pad[:, 2:])


def rwkv7_scan(r, w, k, v, kk, ak, s0):
    def step(s, inp):
        r_t, w_t, k_t, v_t, kk_t, ak_t = inp
        sa = jnp.einsum('bhvk,bhk->bhv', s, kk_t)
        s = s * w_t[:, :, None, :] - sa[..., None] * ak_t[:, :, None, :] + v_t[..., None] * k_t[:, :, None, :]
        return s, jnp.einsum('bhvk,bhk->bhv', s, r_t)

    xs = tuple(jnp.moveaxis(u, 1, 0) for u in (r, w, k, v, kk, ak))
    s_fin, ys = lax.scan(step, s0, xs)
    return jnp.moveaxis(ys, 0, 1), s_fin


def rwkv7_mixer(p_ctx, p_lat, mu, w0, w_up, a0, a_up, g_up, kk_scale, k_a, r_k, ln_gain, ln_bias):
    def hd(t):
        return t.reshape(t.shape[0], t.shape[1], B_HEADS, HEAD_DIM)

    def prep(p):
        p = p + (centred_shift(p) - p) * mu
        r, k, v, wdf, wdb, adf, adb, gd = jnp.split(p, list(RWKV_SPLITS), axis=-1)
        kk = hd(k * kk_scale)
        kk = kk / jnp.maximum(jnp.sqrt(jnp.sum(kk * kk, axis=-1, keepdims=True)), 1e-12)
        dirs = []
        for d, (wd, ad) in enumerate(((wdf, adf), (wdb, adb))):
            w = -jax.nn.softplus(-(w0[d] + jnp.tanh(wd) @ w_up[d])) - 0.5
            decay = jnp.exp(-jnp.exp(w))
            a = jax.nn.sigmoid(a0[d] + ad @ a_up[d])
            k_d = k * (1.0 + (a - 1.0) * k_a)
            dirs.append((hd(decay), hd(k_d), hd(a) * kk))
        g = jax.nn.sigmoid(gd) @ g_up
        return hd(r), hd(v), kk, dirs, g

    rc, vc, kkc, dc, gc = prep(p_ctx)
    rl, vl, kkl, dl, gl = prep(p_lat)
    s0 = jnp.zeros((p_lat.shape[0], B_HEADS, HEAD_DIM, HEAD_DIM), jnp.float32)

    def flip(t):
        return t[:, ::-1]

    (wcf, kcf, akcf), (wcb, kcb, akcb) = dc
    (wlf, klf, aklf), (wlb, klb, aklb) = dl
    yc_f, s_f = rwkv7_scan(rc, wcf, kcf, vc, kkc, akcf, s0)
    yl_f, _ = rwkv7_scan(rl, wlf, klf, vl, kkl, aklf, s_f)
    yc_b, s_b = rwkv7_scan(flip(rc), flip(wcb), flip(kcb), flip(vc), flip(kkc), flip(akcb), s0)
    yl_b, _ = rwkv7_scan(flip(rl), flip(wlb), flip(klb), flip(vl), flip(kkl), flip(aklb), s_b)

    def readout(y, r, v, k_f, k_b, g):
        mean = jnp.mean(y, axis=-1, keepdims=True)
        var = jnp.mean(jnp.square(y - mean), axis=-1, keepdims=True)
        y = ((y - mean) * lax.rsqrt(var + RWKV_LN_EPS)).reshape(g.shape) * ln_gain + ln_bias
        bonus = (jnp.sum(r * (k_f + k_b) * r_k, axis=-1, keepdims=True) * v).reshape(g.shape)
        return (y + bonus) * g

    out_lat = readout(yl_f + flip(yl_b), rl, vl, klf, klb, gl)
    out_ctx = readout(yc_f + flip(yc_b), rc, vc, kcf, kcb, gc)
    return out_lat, out_ctx


def axial_rope(t, pos_r, pos_c):
    quarter = HEAD_DIM // 4
    inv = ROPE_THETA ** (-jnp.arange(quarter, dtype=jnp.float32) / quarter)
    ang = jnp.concatenate([pos_r[:, None] * inv, pos_c[:, None] * inv], axis=-1)
    cos = jnp.cos(ang)[None, :, None, :]
    sin = jnp.sin(ang)[None, :, None, :]
    t1, t2 = jnp.split(t, 2, axis=-1)
    return jnp.concatenate([t1 * cos - t2 * sin, t2 * cos + t1 * sin], axis=-1)


def neighbourhood_attention(p_ctx, p_lat, q_gain, k_gain, rpb, pos_r, pos_c, need_ctx):
    bsz, t_lat, _ = p_lat.shape
    n_ctx = p_ctx.shape[1]
    rows = t_lat // GRID_W
    win_r = min(NA_WIN_ROWS, rows)
    scale = HEAD_DIM ** -0.5

    def qkv(p):
        q, k, v = jnp.split(p, 3, axis=-1)
        hd = lambda t: t.reshape(t.shape[0], t.shape[1], C_HEADS, HEAD_DIM)
        return rms_norm(hd(q), q_gain), rms_norm(hd(k), k_gain), hd(v)

    qc, kc, vc = qkv(p_ctx)
    ql, kl, vl = qkv(p_lat)
    ql, kl = axial_rope(ql, pos_r, pos_c), axial_rope(kl, pos_r, pos_c)
    kc_h, vc_h = kc.transpose(0, 2, 1, 3), vc.transpose(0, 2, 1, 3)

    def grid(t):
        return t.transpose(0, 2, 1, 3).reshape(bsz, C_HEADS, rows, GRID_W, HEAD_DIM)

    qg, kg, vg = grid(ql), grid(kl), grid(vl)
    n_cb = GRID_W // NA_COL_BLOCK
    q_col = jnp.arange(GRID_W).reshape(n_cb, NA_COL_BLOCK)
    k_start = jnp.clip(jnp.arange(n_cb) * NA_COL_BLOCK - NA_WIN_COLS // 2, 0, GRID_W - NA_KEY_COLS)
    k_col = k_start[:, None] + jnp.arange(NA_KEY_COLS)
    w_start = jnp.clip(q_col - NA_WIN_COLS // 2, 0, GRID_W - NA_WIN_COLS)
    kc3 = k_col[:, None, :]
    in_win = (kc3 >= w_start[..., None]) & (kc3 < w_start[..., None] + NA_WIN_COLS)
    col_idx = jnp.clip(kc3 - q_col[..., None] + NA_WIN_COLS - 1, 0, 2 * NA_WIN_COLS - 2)
    n_win = win_r * NA_KEY_COLS

    def one_row(r):
        r_start = jnp.clip(r - win_r // 2, 0, rows - win_r)
        k_blk = lax.dynamic_slice_in_dim(kg, r_start, win_r, axis=2)[:, :, :, k_col]
        v_blk = lax.dynamic_slice_in_dim(vg, r_start, win_r, axis=2)[:, :, :, k_col]
        q_row = lax.dynamic_index_in_dim(qg, r, axis=2, keepdims=False).reshape(
            bsz, C_HEADS, n_cb, NA_COL_BLOCK, HEAD_DIM)
        s_win = jnp.einsum('bhjqd,bhrjkd->bhjqrk', q_row, k_blk) * scale
        row_idx = r_start + jnp.arange(win_r) - r + NA_WIN_ROWS - 1
        bias = rpb[:, row_idx[None, None, :, None], col_idx[:, :, None, :]]
        s_win = jnp.where(in_win[:, :, None, :], s_win + bias, NEG_INF)
        s_ctx = jnp.einsum('bhjqd,bhld->bhjql', q_row, kc_h) * scale
        s_all = jnp.concatenate([s_win.reshape(s_win.shape[:4] + (n_win,)), s_ctx], axis=-1)
        prob = jax.nn.softmax(s_all, axis=-1)
        p_win = prob[..., :n_win].reshape(s_win.shape)
        p_ctx = prob[..., n_win:]
        o = (jnp.einsum('bhjqrk,bhrjkd->bhjqd', p_win, v_blk)
             + jnp.einsum('bhjql,bhld->bhjqd', p_ctx, vc_h))
        return o.reshape(bsz, C_HEADS, GRID_W, HEAD_DIM)

    o = lax.map(one_row, jnp.arange(rows))
    out_lat = o.transpose(1, 0, 3, 2, 4).reshape(bsz, t_lat, C_WIDTH)
    out_ctx = None
    if need_ctx:
        qc_h = qc.transpose(0, 2, 1, 3)
        pc = jax.nn.softmax(jnp.einsum('bhqd,bhkd->bhqk', qc_h, kc_h) * scale, axis=-1)
        out_ctx = jnp.einsum('bhqk,bhkd->bhqd', pc, vc_h).transpose(0, 2, 1, 3).reshape(bsz, n_ctx, C_WIDTH)
    return out_lat, out_ctx


def hierarchical_moe(h, w_rg, b_rg, w_re, b_re, w_gate, w_up, w_down):
    g_logits = h @ w_rg + b_rg
    g_prob = jax.nn.softmax(g_logits, axis=-1)
    g_sel = jnp.argmax(g_logits, axis=-1)
    g_w = jnp.take_along_axis(g_prob, g_sel[:, None], axis=-1)
    e_logits = (h @ w_re + b_re).reshape(-1, N_GROUPS, EXPERTS_PER_GROUP)
    e_logits = jnp.take_along_axis(e_logits, g_sel[:, None, None], axis=1)[:, 0]
    e_prob = jax.nn.softmax(e_logits, axis=-1)
    top_w, top_i = lax.top_k(e_prob, TOP_K_IN_GROUP)
    top_w = top_w / jnp.sum(top_w, axis=-1, keepdims=True) * g_w
    e_w = jnp.sum(jax.nn.one_hot(top_i, EXPERTS_PER_GROUP, dtype=jnp.float32) * top_w[..., None], axis=1)
    comb = jax.nn.one_hot(g_sel, N_GROUPS, dtype=jnp.float32)[:, :, None] * e_w[:, None, :]
    y = jnp.zeros(h.shape, jnp.float32)
    for gi in range(N_GROUPS):
        hid = jax.nn.silu(jnp.einsum('nd,edf->nef', h, w_gate[gi])) * jnp.einsum('nd,edf->nef', h, w_up[gi])
        y = y + jnp.einsum('nef,efd->nd', hid * comb[:, gi, :, None], w_down[gi])
    return y


def setup_inputs(seed: int = 0) -> dict:
    key = jax.random.key(seed)
    ks = iter(jax.random.split(key, 40))
    D, G, E, F = D_MODEL, N_GROUPS, EXPERTS_PER_GROUP, D_EXPERT

    def nrm(shape, s):
        return s * jax.random.normal(next(ks), shape, jnp.float32)

    return {
        'x': nrm((BATCH, SEQ, D), 1.0),
        'c': nrm((BATCH, D), 1.0),
        'ctx': nrm((BATCH, CTX_LEN, D), 1.0),
        'c_ctx': nrm((D,), 1.0),
        'norm1_gain': 1.0 + nrm((DEPTH, D), 0.02),
        'norm2_gain': 1.0 + nrm((DEPTH, D), 0.02),
        'w_ada': nrm((DEPTH, D, 6 * D), 0.5 * D ** -0.5),
        'b_ada': nrm((DEPTH, 6 * D), 0.02),
        'w_in': nrm((DEPTH, D, P_TOTAL), D ** -0.5),
        'w_out': nrm((DEPTH, MIX_WIDTH, D), MIX_WIDTH ** -0.5),
        'hgrn_lb_logits': nrm((2, DEPTH, A_WIDTH), 0.5),
        'hgrn_gn_gain': 1.0 + nrm((DEPTH, A_HEADS, HEAD_DIM), 0.02),
        'rwkv_mu': jax.random.uniform(next(ks), (DEPTH, B_PROJ), jnp.float32, 0.0, 1.0),
        'rwkv_w0': -1.5 + nrm((DEPTH, 2, B_WIDTH), 0.5),
        'rwkv_w_up': nrm((DEPTH, 2, RWKV_W_RANK, B_WIDTH), RWKV_W_RANK ** -0.5),
        'rwkv_a0': nrm((DEPTH, 2, B_WIDTH), 0.1),
        'rwkv_a_up': nrm((DEPTH, 2, RWKV_A_RANK, B_WIDTH), RWKV_A_RANK ** -0.5),
        'rwkv_g_up': nrm((DEPTH, RWKV_G_RANK, B_WIDTH), RWKV_G_RANK ** -0.5),
        'rwkv_kk_scale': 0.85 + nrm((DEPTH, B_WIDTH), 0.02),
        'rwkv_k_a': 1.0 + nrm((DEPTH, B_WIDTH), 0.02),
        'rwkv_r_k': nrm((DEPTH, B_HEADS, HEAD_DIM), 0.1),
        'rwkv_ln_gain': 1.0 + nrm((DEPTH, B_WIDTH), 0.02),
        'rwkv_ln_bias': nrm((DEPTH, B_WIDTH), 0.02),
        'na_q_gain': 1.0 + nrm((DEPTH, HEAD_DIM), 0.02),
        'na_k_gain': 1.0 + nrm((DEPTH, HEAD_DIM), 0.02),
        'na_rpb': nrm((DEPTH, C_HEADS, 2 * NA_WIN_ROWS - 1, 2 * NA_WIN_COLS - 1), 0.1),
        'w_router_group': nrm((DEPTH, D, G), D ** -0.5),
        'b_router_group': nrm((DEPTH, G), 0.01),
        'w_router_expert': nrm((DEPTH, D, G * E), D ** -0.5),
        'b_router_expert': nrm((DEPTH, G * E), 0.01),
        'w_exp_gate': nrm((DEPTH, G, E, D, F), D ** -0.5),
        'w_exp_up': nrm((DEPTH, G, E, D, F), D ** -0.5),
        'w_exp_down': nrm((DEPTH, G, E, F, D), F ** -0.5),
    }


def reference(x, c, ctx, c_ctx, norm1_gain, norm2_gain, w_ada, b_ada, w_in, w_out,
              hgrn_lb_logits, hgrn_gn_gain, rwkv_mu, rwkv_w0, rwkv_w_up, rwkv_a0, rwkv_a_up,
              rwkv_g_up, rwkv_kk_scale, rwkv_k_a, rwkv_r_k, rwkv_ln_gain, rwkv_ln_bias,
              na_q_gain, na_k_gain, na_rpb, w_router_group, b_router_group,
              w_router_expert, b_router_expert, w_exp_gate, w_exp_up, w_exp_down):
    f32 = jnp.float32
    bsz, t_lat, _ = x.shape
    n_ctx = ctx.shape[1]
    pos = jnp.arange(t_lat)
    pos_r = (pos // GRID_W).astype(f32)
    pos_c = (pos % GRID_W).astype(f32)
    lb_p = jax.nn.softmax(hgrn_lb_logits.astype(f32), axis=1)
    lower_bounds = jnp.cumsum(lb_p, axis=1) - lb_p[:, :1]
    silu_c = jax.nn.silu(c.astype(f32))
    silu_cc = jax.nn.silu(c_ctx.astype(f32))
    xl = x.astype(f32)
    xc = ctx.astype(f32)
    for layer in range(DEPTH):
        last = layer == DEPTH - 1
        mod_l = (silu_c @ w_ada[layer] + b_ada[layer])[:, None, :]
        mod_c = (silu_cc @ w_ada[layer] + b_ada[layer])[None, None, :]
        sh1l, sc1l, g1l, sh2l, sc2l, g2l = jnp.split(mod_l, 6, axis=-1)
        sh1c, sc1c, g1c, sh2c, sc2c, g2c = jnp.split(mod_c, 6, axis=-1)
        hl = rms_norm(xl, norm1_gain[layer]) * (1.0 + sc1l) + sh1l
        hc = rms_norm(xc, norm1_gain[layer]) * (1.0 + sc1c) + sh1c
        pl = hl @ w_in[layer]
        pc = hc @ w_in[layer]
        al, bl, cl = jnp.split(pl, [A_PROJ, A_PROJ + B_PROJ], axis=-1)
        ac, bc, cc = jnp.split(pc, [A_PROJ, A_PROJ + B_PROJ], axis=-1)
        a_lat, a_ctx = hgrn2_mixer(ac, al, lower_bounds[0, layer], lower_bounds[1, layer], hgrn_gn_gain[layer])
        b_lat, b_ctx = rwkv7_mixer(bc, bl, rwkv_mu[layer], rwkv_w0[layer], rwkv_w_up[layer], rwkv_a0[layer],
                                   rwkv_a_up[layer], rwkv_g_up[layer], rwkv_kk_scale[layer], rwkv_k_a[layer],
                                   rwkv_r_k[layer], rwkv_ln_gain[layer], rwkv_ln_bias[layer])
        c_lat, c_ctxo = neighbourhood_attention(cc, cl, na_q_gain[layer], na_k_gain[layer], na_rpb[layer],
                                                pos_r, pos_c, not last)
        xl = xl + g1l * (jnp.concatenate([a_lat, b_lat, c_lat], axis=-1) @ w_out[layer])
        h2l = (rms_norm(xl, norm2_gain[layer]) * (1.0 + sc2l) + sh2l).reshape(-1, D_MODEL)
        moe_w = (w_router_group[layer], b_router_group[layer], w_router_expert[layer], b_router_expert[layer],
                 w_exp_gate[layer], w_exp_up[layer], w_exp_down[layer])
        if last:
            xl = xl + g2l * hierarchical_moe(h2l, *moe_w).reshape(xl.shape)
        else:
            xc = xc + g1c * (jnp.concatenate([a_ctx, b_ctx, c_ctxo], axis=-1) @ w_out[layer])
            h2c = (rms_norm(xc, norm2_gain[layer]) * (1.0 + sc2c) + sh2c).reshape(-1, D_MODEL)
            y = hierarchical_moe(jnp.concatenate([h2c, h2l], axis=0), *moe_w)
            xc = xc + g2c * y[:bsz * n_ctx].reshape(xc.shape)
            xl = xl + g2l * y[bsz * n_ctx:].reshape(xl.shape)
    return xl.astype(x.dtype)
```

```python
import numpy as np
import concourse.bass as bass
import concourse.mybir as mybir
from concourse.bass_utils import run_bass_kernel_spmd

F32 = mybir.dt.float32
BF16 = mybir.dt.bfloat16
ALU = mybir.AluOpType
AF = mybir.ActivationFunctionType
AX = mybir.AxisListType


class Tk:
    __slots__ = ("w", "r", "name")

    def __init__(self, name=""):
        self.w = None
        self.r = {}
        self.name = name


class _Eng:
    def __init__(self, mgr, name, eng):
        self.mgr = mgr
        self.name = name
        self.eng = eng
        self.seen = {}
        self.sid = None
        self.cnt = 0
        self.dma_sids = []
        self.dma_cnt = []
        self.dma_rr = 0
        self.n_inst = 0

    def comp_token(self):
        if self.sid is None or self.cnt >= 30000:
            self.sid = self.mgr.new_sem(f"s_{self.name}_{len(self.mgr.sems)}")
            self.cnt = 0
        self.cnt += 1
        return (self.sid, self.cnt)

    def dma_token(self):
        if not self.dma_sids:
            for i in range(8):
                self.dma_sids.append(self.mgr.new_sem(f"d_{self.name}_{i}"))
                self.dma_cnt.append(0)
        k = self.dma_rr
        self.dma_rr = (self.dma_rr + 1) % len(self.dma_sids)
        if self.dma_cnt[k] >= 30000:
            self.dma_sids[k] = self.mgr.new_sem(f"d_{self.name}_{len(self.mgr.sems)}")
            self.dma_cnt[k] = 0
        self.dma_cnt[k] += 16
        return (self.dma_sids[k], self.dma_cnt[k])


class Mgr:
    def __init__(self, nc):
        self.nc = nc
        self.sems = []
        self.E = {
            "pe": _Eng(self, "pe", nc.tensor),
            "act": _Eng(self, "act", nc.scalar),
            "dve": _Eng(self, "dve", nc.vector),
            "pool": _Eng(self, "pool", nc.gpsimd),
            "sp": _Eng(self, "sp", nc.sync),
        }
        self.last_out_tok = []

    def new_sem(self, name):
        h = self.nc.alloc_semaphore(name)
        self.sems.append(h)
        return len(self.sems) - 1

    def op(self, q, fn, r=(), w=(), dma=False):
        E = self.E[q]
        waits = {}

        def need(tok):
            if tok is None:
                return
            sid, val = tok
            if waits.get(sid, 0) < val:
                waits[sid] = val

        for t in r:
            need(t.w)
        for t in w:
            if not (q == "pe" and not dma and t.w is not None and t.w[0] == E.sid):
                need(t.w)
            for tok in t.r.items():
                need(tok)
        for sid, val in waits.items():
            if E.seen.get(sid, 0) < val:
                E.eng.wait_ge(self.sems[sid], val)
                E.seen[sid] = val
        ins = fn(E.eng)
        tok = E.dma_token() if dma else E.comp_token()
        ins.then_inc(self.sems[tok[0]], 16 if dma else 1)
        E.n_inst += 1
        for t in r:
            if t.r.get(tok[0], 0) < tok[1]:
                t.r[tok[0]] = tok[1]
        for t in w:
            t.w = tok
            t.r = {}
        return tok

    def pe(self, fn, r=(), w=()):
        return self.op("pe", fn, r, w)

    def act(self, fn, r=(), w=()):
        return self.op("act", fn, r, w)

    def dve(self, fn, r=(), w=()):
        return self.op("dve", fn, r, w)

    def pool(self, fn, r=(), w=()):
        return self.op("pool", fn, r, w)

    def dma(self, q, out, in_, r=(), w=()):
        return self.op(q, lambda e: e.dma_start(out=out, in_=in_), r, w, dma=True)

    def barrier(self):
        toks = []
        for E in self.E.values():
            if E.sid is not None and E.cnt > 0:
                toks.append((E.sid, E.cnt))
            for s, c in zip(E.dma_sids, E.dma_cnt):
                if c > 0:
                    toks.append((s, c))
        for E in self.E.values():
            for sid, val in toks:
                if E.seen.get(sid, 0) < val:
                    E.eng.wait_ge(self.sems[sid], val)
                    E.seen[sid] = val

    def wait_tok(self, q, tok):
        E = self.E[q]
        if E.seen.get(tok[0], 0) < tok[1]:
            E.eng.wait_ge(self.sems[tok[0]], tok[1])
            E.seen[tok[0]] = tok[1]


NT = 18
TALL = 2304
D = 1024
PTOT = 3968
NEGB = -30000.0


def _tri(chunk, kind):
    i = np.arange(128)
    same = (i[:, None] // chunk) == (i[None, :] // chunk)
    s, t = i[:, None], i[None, :]
    m = {"U": s <= t, "L": s >= t, "SU": s < t, "SL": s > t}[kind]
    return (same & m).astype(np.float32)


C_ID, C_U32, C_L32, C_SU32, C_SL32, C_U64, C_L64, C_SU64, C_SL64 = [128 * i for i in range(9)]
C_CM32 = 1152
C_COL32 = 1156
C_CM64 = 1668
C_END = 1672


def make_consts():
    c = np.zeros((128, C_END), np.float32)
    c[:, C_ID:C_ID + 128] = np.eye(128)
    for off, (ch, kd) in zip([C_U32, C_L32, C_SU32, C_SL32, C_U64, C_L64, C_SU64, C_SL64],
                             [(32, "U"), (32, "L"), (32, "SU"), (32, "SL"), (64, "U"), (64, "L"), (64, "SU"), (64, "SL")]):
        c[:, off:off + 128] = _tri(ch, kd)
    p = np.arange(128)
    for cc in range(4):
        c[:, C_CM32 + cc] = (p // 32 == cc)
        c[:, C_COL32 + cc * 128:C_COL32 + (cc + 1) * 128] = (p[None, :] // 32 == cc)
    for cc in range(2):
        c[:, C_CM64 + cc] = (p // 64 == cc)
    return c


class B_:
    __slots__ = ("t", "k")

    def __init__(self, t, name):
        self.t = t
        self.k = Tk(name)


EPS = 1e-6
IN_SPECS = [
    ("x_all", [TALL, D]), ("c2", [D, 2]), ("consts", [128, C_END]),
    ("norm1_gain", [4, D]), ("norm2_gain", [4, D]), ("w_ada", [4, D, 6 * D]), ("b_ada", [4, 6 * D]),
    ("w_in", [4, D, PTOT]), ("w_out", [4, D, D]), ("hgrn_lb_logits", [2, 4, 256]), ("hgrn_gn_gain", [4, 256]),
    ("rwkv_mu", [4, 1152]), ("rwkv_w0", [4, 2, 256]), ("rwkv_w_up", [4, 2, 64, 256]), ("rwkv_a0", [4, 2, 256]),
    ("rwkv_a_up", [4, 2, 64, 256]), ("rwkv_g_up", [4, 128, 256]), ("rwkv_kk_scale", [4, 256]), ("rwkv_k_a", [4, 256]),
    ("rwkv_r_k", [4, 256]), ("rwkv_ln_gain", [4, 256]), ("rwkv_ln_bias", [4, 256]),
    ("na_q_gain", [4, 64]), ("na_k_gain", [4, 64]), ("rpbT", [4, 128, 8 * 14 * 64]), ("rope_cs", [TALL, 64]),
    ("w_router", [4, D, 36]), ("b_router", [4, 36]),
    ("w_exp_gate", [4, 32, D, 512]), ("w_exp_up", [4, 32, D, 512]), ("w_exp_down", [4, 32, 512, D]),
]


class Prog:
    def __init__(self, NL=4, dbg=False, moe_in=True):
        self.NL = NL
        self.dbg = dbg
        nc = self.nc = bass.Bass("TRN2", target_bir_lowering=False)
        self.M = Mgr(nc)
        self.I = {}
        self.moe_in = moe_in
        for name, shape in IN_SPECS:
            shape = list(shape)
            if shape[0] == 4 and name not in ("hgrn_lb_logits",):
                shape[0] = NL
            if name.startswith("w_exp") and not moe_in:
                shape = [1, 1, 128, 128]
            self.I[name] = nc.dram_tensor(name, shape, F32, kind="ExternalInput").ap()
        sk = "ExternalOutput" if dbg else "Internal"
        self.out = nc.dram_tensor("out", [2048, D], F32, kind="ExternalOutput").ap()
        self.xs = B_(nc.dram_tensor("xs", [TALL, D], F32, kind=sk).ap(), "xs")
        self.mods = B_(nc.dram_tensor("mods", [4, 2, 6 * D], F32, kind=sk).ap(), "mods")
        self.ptm = B_(nc.dram_tensor("ptm", [TALL, PTOT], F32, kind=sk).ap(), "ptm")
        self.mixs = B_(nc.dram_tensor("mixs", [TALL, D], BF16, kind=sk).ap(), "mixs")
        self.lbs = B_(nc.dram_tensor("lbs", [2, 4, 256], F32, kind=sk).ap(), "lbs")
        self.uid = 0

    def sb(self, es, name, shape, dt=F32):
        self.uid += 1
        t = es.enter_context(self.nc.sbuf_tensor(f"{name}_{self.uid}", list(shape), dt))
        return B_(t, name)

    def ps(self, es, name, shape, dt=F32):
        self.uid += 1
        t = es.enter_context(self.nc.psum_tensor(f"{name}_{self.uid}", list(shape), dt))
        return B_(t, name)

    def dump(self, name, buf, ap=None):
        if not self.dbg:
            return
        ap = buf.t[:] if ap is None else ap
        dt_ = self.nc.dram_tensor(name, list(ap.shape), buf.t.dtype, kind="ExternalOutput").ap()
        self.M.op("sp", lambda e: e.dma_start(out=dt_, in_=ap), r=[buf.k], w=[], dma=True)

    def bload(self, q, dst, src_row_ap, n):
        self.M.dma(q, dst.t[:, 0:n], src_row_ap.partition_broadcast(128), w=[dst.k])

    def setup(self, es):
        M, nc, I = self.M, self.nc, self.I
        self.cst = self.sb(es, "cst", [128, C_END])
        M.dma("sp", self.cst.t[:], I["consts"][:, :], w=[self.cst.k])
        self.idb = self.sb(es, "idb", [128, 128], BF16)
        M.dve(lambda e: e.tensor_copy(out=self.idb.t[:], in_=self.cst.t[:, C_ID:C_ID + 128]), r=[self.cst.k], w=[self.idb.k])
        self.ones = self.sb(es, "ones", [128, 128])
        M.dve(lambda e: e.memset(self.ones.t[:], 1.0), w=[self.ones.k])
        self.scT = self.sb(es, "scT", [128, 8, 2])
        M.dma("sp", self.scT.t[:], I["c2"].rearrange("(k p) j -> p k j", p=128), w=[self.scT.k])
        M.act(lambda e: e.activation(out=self.scT.t[:], in_=self.scT.t[:], func=AF.Silu), r=[self.scT.k], w=[self.scT.k])
        from contextlib import ExitStack
        with ExitStack() as s2:
            lg = self.sb(s2, "lg", [2, 4, 256])
            mx = self.sb(s2, "mx", [2, 256])
            sm = self.sb(s2, "sm", [2, 256])
            M.dma("sp", lg.t[:], I["hgrn_lb_logits"][:, :, :], w=[lg.k])
            M.dve(lambda e: e.tensor_tensor(out=mx.t[:], in0=lg.t[:, 0, :], in1=lg.t[:, 1, :], op=ALU.max), r=[lg.k], w=[mx.k])
            M.dve(lambda e: e.tensor_tensor(out=mx.t[:], in0=mx.t[:], in1=lg.t[:, 2, :], op=ALU.max), r=[lg.k, mx.k], w=[mx.k])
            M.dve(lambda e: e.tensor_tensor(out=mx.t[:], in0=mx.t[:], in1=lg.t[:, 3, :], op=ALU.max), r=[lg.k, mx.k], w=[mx.k])
            M.dve(lambda e: e.tensor_tensor(out=lg.t[:], in0=lg.t[:], in1=mx.t[:].unsqueeze(1).to_broadcast([2, 4, 256]), op=ALU.subtract),
                  r=[lg.k, mx.k], w=[lg.k])
            M.act(lambda e: e.activation(out=lg.t[:], in_=lg.t[:], func=AF.Exp), r=[lg.k], w=[lg.k])
            M.dve(lambda e: e.tensor_tensor(out=sm.t[:], in0=lg.t[:, 0, :], in1=lg.t[:, 1, :], op=ALU.add), r=[lg.k], w=[sm.k])
            M.dve(lambda e: e.tensor_tensor(out=sm.t[:], in0=sm.t[:], in1=lg.t[:, 2, :], op=ALU.add), r=[lg.k, sm.k], w=[sm.k])
            M.dve(lambda e: e.tensor_tensor(out=sm.t[:], in0=sm.t[:], in1=lg.t[:, 3, :], op=ALU.add), r=[lg.k, sm.k], w=[sm.k])
            M.dve(lambda e: e.reciprocal(out=sm.t[:], in_=sm.t[:]), r=[sm.k], w=[sm.k])
            M.dve(lambda e: e.tensor_tensor(out=lg.t[:], in0=lg.t[:], in1=sm.t[:].unsqueeze(1).to_broadcast([2, 4, 256]), op=ALU.mult),
                  r=[lg.k, sm.k], w=[lg.k])
            M.dve(lambda e: e.memset(lg.t[:, 0, :], 0.0), r=[lg.k], w=[lg.k])
            for l in range(2, 4):
                M.dve(lambda e, l=l: e.tensor_tensor(out=lg.t[:, l, :], in0=lg.t[:, l, :], in1=lg.t[:, l - 1, :], op=ALU.add), r=[lg.k], w=[lg.k])
            M.dma("sp", self.lbs.t[:, :, :], lg.t[:], r=[lg.k], w=[self.lbs.k])
            M.barrier()

    def stage_mods(self, l):
        from contextlib import ExitStack
        M, nc, I = self.M, self.nc, self.I
        with ExitStack() as es:
            wb = [self.sb(es, f"wada{i}", [128, 8, 512]) for i in range(2)]
            brow = self.sb(es, "brow", [1, 6 * D])
            mrow = self.sb(es, "mrow", [2, 6 * D])
            pm = [self.ps(es, f"pm{i}", [2, 512]) for i in range(2)]
            M.dma("sp", brow.t[:], I["b_ada"][l:l + 1, :], w=[brow.k])
            wv = I["w_ada"][l].rearrange("(k p) n -> p k n", p=128)
            for n in range(12):
                w_ = wb[n % 2]
                p_ = pm[n % 2]
                M.dma("sp" if n % 2 == 0 else "pool", w_.t[:], wv[:, :, n * 512:(n + 1) * 512], w=[w_.k])
                for k in range(8):
                    M.pe(lambda e, k=k: e.matmul(p_.t[:, :], lhsT=self.scT.t[:, k, :], rhs=w_.t[:, k, :], start=(k == 0), stop=False),
                         r=[self.scT.k, w_.k], w=[p_.k])
                M.pe(lambda e: e.matmul(p_.t[:, :], lhsT=self.ones.t[0:1, 0:2], rhs=brow.t[0:1, n * 512:(n + 1) * 512], start=False, stop=True),
                     r=[self.ones.k, brow.k], w=[p_.k])
                M.act(lambda e: e.copy(out=mrow.t[:, n * 512:(n + 1) * 512], in_=p_.t[:, :]), r=[p_.k], w=[mrow.k])
            M.dma("sp", self.mods.t[l], mrow.t[:], r=[mrow.k], w=[self.mods.k])
            M.barrier()

    def modrow(self, l, which, idx):
        return self.mods.t[l, which:which + 1, idx * D:(idx + 1) * D]

    def norm_mod_tiles(self, es, l, gain_name, i_sc, i_sh):
        M, I = self.M, self.I
        gb = self.sb(es, "gb", [128, D])
        self.bload("sp", gb, I[gain_name][l:l + 1, :], D)
        A, S = [], []
        for which in range(2):
            a = self.sb(es, f"A{which}", [128, D])
            s = self.sb(es, f"S{which}", [128, D])
            M.dma("sp", a.t[:], self.modrow(l, which, i_sc).partition_broadcast(128), r=[self.mods.k], w=[a.k])
            M.dma("sp", s.t[:], self.modrow(l, which, i_sh).partition_broadcast(128), r=[self.mods.k], w=[s.k])
            M.dve(lambda e, a=a: e.scalar_tensor_tensor(out=a.t[:], in0=a.t[:], scalar=1.0, in1=gb.t[:], op0=ALU.add, op1=ALU.mult),
                  r=[a.k, gb.k], w=[a.k])
            A.append(a)
            S.append(s)
        return A, S

    def rms_mod(self, xt, A, S, sq, ss, hf, hout):
        M = self.M
        M.act(lambda e: e.activation(out=sq.t[:], in_=xt.t[:], func=AF.Square, accum_out=ss.t[:]), r=[xt.k], w=[sq.k, ss.k])
        M.dve(lambda e: e.tensor_scalar(out=ss.t[:], in0=ss.t[:], scalar1=1.0 / D, scalar2=EPS, op0=ALU.mult, op1=ALU.add), r=[ss.k], w=[ss.k])
        M.act(lambda e: e.activation(out=ss.t[:], in_=ss.t[:], func=AF.Sqrt), r=[ss.k], w=[ss.k])
        M.dve(lambda e: e.reciprocal(out=ss.t[:], in_=ss.t[:]), r=[ss.k], w=[ss.k])
        M.dve(lambda e: e.scalar_tensor_tensor(out=hf.t[:], in0=xt.t[:], scalar=ss.t[:, 0:1], in1=A.t[:], op0=ALU.mult, op1=ALU.mult),
              r=[xt.k, ss.k, A.k], w=[hf.k])
        M.dve(lambda e: e.tensor_tensor(out=hout.t[:], in0=hf.t[:], in1=S.t[:], op=ALU.add), r=[hf.k, S.k], w=[hout.k])

    def stage_proj(self, l, lrT):
        from contextlib import ExitStack
        M, nc, I = self.M, self.nc, self.I
        with ExitStack() as es:
            hT = self.sb(es, "hT", [128, 8, TALL], BF16)
            sT = self.sb(es, "sT", [128, 8, TALL], BF16)
            wbuf = self.sb(es, "wbuf", [128, 8, 2304], BF16)
            wbufC = self.sb(es, "wbufC", [128, 8, 1536], BF16)
            with ExitStack() as e2:
                A, S = self.norm_mod_tiles(e2, l, "norm1_gain", 1, 0)
                xt = [self.sb(e2, f"xt{i}", [128, D]) for i in range(2)]
                sq = self.sb(e2, "sq", [128, D])
                hf = self.sb(e2, "hf", [128, D])
                hb = [self.sb(e2, f"hb{i}", [128, D], BF16) for i in range(2)]
                ss = [self.sb(e2, f"ss{i}", [128, 1]) for i in range(2)]
                ptr = [self.ps(e2, f"ptr{i}", [128, 8, 128], BF16) for i in range(2)]
                wv = I["w_in"][l].rearrange("(k p) n -> p k n", p=128)
                for k in range(8):
                    M.dma("pool", wbuf.t[:, k, 0:1280], wv[:, k, 0:1280], w=[wbuf.k])
                for k in range(8):
                    M.dma("pool", wbufC.t[:, k, 0:1536], wv[:, k, 2432:3968], w=[wbufC.k])

                def shift_tile(jj):
                    a, b_ = (0, 256) if jj < 2 else (256, TALL)
                    c0, c1 = jj * 128, (jj + 1) * 128
                    lo = max(c0, a + 1)
                    if c0 == a:
                        M.dve(lambda e: e.memset(sT.t[:, :, a:a + 1], 0.0), w=[sT.k])
                    M.dve(lambda e: e.tensor_scalar(out=sT.t[:, :, lo:c1], in0=hT.t[:, :, lo - 1:c1 - 1], scalar1=0.5, scalar2=None, op0=ALU.mult),
                          r=[hT.k], w=[sT.k])
                    hi = min(c1, b_ - 1)
                    M.dve(lambda e: e.scalar_tensor_tensor(out=sT.t[:, :, c0:hi], in0=hT.t[:, :, c0 + 1:hi + 1], scalar=0.5, in1=sT.t[:, :, c0:hi],
                                                           op0=ALU.mult, op1=ALU.add), r=[hT.k, sT.k], w=[sT.k])

                for j in range(NT):
                    x_ = xt[j % 2]
                    which = 1 if j < 2 else 0
                    if l == 0:
                        M.dma("sp", x_.t[:], I["x_all"][j * 128:(j + 1) * 128, :], w=[x_.k])
                    else:
                        M.dma("sp", x_.t[:], self.xs.t[j * 128:(j + 1) * 128, :], r=[self.xs.k], w=[x_.k])
                    self.rms_mod(x_, A[which], S[which], sq, ss[j % 2], hf, hb[j % 2])
                    p_ = ptr[j % 2]
                    for k in range(8):
                        M.pe(lambda e, k=k: e.transpose(p_.t[:, k, :], hb[j % 2].t[:, k * 128:(k + 1) * 128], self.idb.t[:]),
                             r=[hb[j % 2].k, self.idb.k], w=[p_.k])
                    M.act(lambda e: e.copy(out=hT.t[:, :, j * 128:(j + 1) * 128], in_=p_.t[:, :, :]), r=[p_.k], w=[hT.k])
                    if j >= 1:
                        shift_tile(j - 1)
                shift_tile(NT - 1)
                M.barrier()
            pp = [self.ps(es, f"pp{i}", [128, 512]) for i in range(4)]
            ob = [self.sb(es, f"ob{i}", [128, 512]) for i in range(4)]
            st = [self.sb(es, f"wst{i}", [128, 1152]) for i in range(2)]
            mub = self.sb(es, "mub", [128, 1152])
            omub = self.sb(es, "omub", [128, 1152])
            cnt = [0]

            def tok_major(col0, wcols, ncols, dual, wbuf=wbuf):
                for j in range(NT):
                    for c0 in range(0, ncols, 512):
                        c1 = min(ncols, c0 + 512)
                        i = cnt[0] % 4
                        cnt[0] += 1
                        p_, o_ = pp[i], ob[i]
                        for k in range(8):
                            M.pe(lambda e, k=k: e.matmul(p_.t[:, 0:c1 - c0], lhsT=hT.t[:, k, j * 128:(j + 1) * 128], rhs=wbuf.t[:, k, wcols + c0:wcols + c1],
                                                         start=(k == 0), stop=(k == 7 and not dual)), r=[hT.k, wbuf.k], w=[p_.k])
                        if dual:
                            for k in range(8):
                                M.pe(lambda e, k=k: e.matmul(p_.t[:, 0:c1 - c0], lhsT=sT.t[:, k, j * 128:(j + 1) * 128],
                                                             rhs=wbuf.t[:, k, 1152 + wcols + c0:1152 + wcols + c1], start=False, stop=(k == 7)),
                                     r=[sT.k, wbuf.k], w=[p_.k])
                        M.act(lambda e: e.copy(out=o_.t[:, 0:c1 - c0], in_=p_.t[:, 0:c1 - c0]), r=[p_.k], w=[o_.k])
                        M.dma("sp", self.ptm.t[j * 128:(j + 1) * 128, col0 + c0:col0 + c1], o_.t[:, 0:c1 - c0], r=[o_.k], w=[self.ptm.k])

            tok_major(0, 0, 1280, False)
            self.bload("sp", mub, I["rwkv_mu"][l:l + 1, :], 1152)
            M.dve(lambda e: e.tensor_scalar(out=omub.t[:], in0=mub.t[:], scalar1=-1.0, scalar2=1.0, op0=ALU.mult, op1=ALU.add), r=[mub.k], w=[omub.k])
            for k in range(8):
                s_ = st[k % 2]
                M.dma("sp", s_.t[:], I["w_in"][l, k * 128:(k + 1) * 128, 1280:2432], w=[s_.k])
                M.dve(lambda e, k=k: e.tensor_tensor(out=wbuf.t[:, k, 0:1152], in0=s_.t[:], in1=omub.t[:], op=ALU.mult), r=[s_.k, omub.k], w=[wbuf.k])
                M.dve(lambda e, k=k: e.tensor_tensor(out=wbuf.t[:, k, 1152:2304], in0=s_.t[:], in1=mub.t[:], op=ALU.mult), r=[s_.k, mub.k], w=[wbuf.k])
            tok_major(2432, 0, 1536, False, wbufC)
            tok_major(1280, 0, 768, True)
            for m in range(3):
                for t0 in range(0, TALL, 512):
                    t1 = min(TALL, t0 + 512)
                    i = cnt[0] % 4
                    cnt[0] += 1
                    p_ = pp[i]
                    wc = 768 + m * 128
                    for k in range(8):
                        M.pe(lambda e, k=k: e.matmul(p_.t[:, 0:t1 - t0], lhsT=wbuf.t[:, k, wc:wc + 128], rhs=hT.t[:, k, t0:t1], start=(k == 0), stop=False),
                             r=[hT.k, wbuf.k], w=[p_.k])
                    for k in range(8):
                        M.pe(lambda e, k=k: e.matmul(p_.t[:, 0:t1 - t0], lhsT=wbuf.t[:, k, 1152 + wc:1152 + wc + 128], rhs=sT.t[:, k, t0:t1], start=False, stop=(k == 7)),
                             r=[sT.k, wbuf.k], w=[p_.k])
                    M.act(lambda e: e.copy(out=lrT.t[:, m, t0:t1], in_=p_.t[:, 0:t1 - t0]), r=[p_.k], w=[lrT.k])
            M.barrier()


def _rope_table():
    t = np.arange(2048)
    inv = (10000.0 ** (-np.arange(16, dtype=np.float32) / 16)).astype(np.float32)
    ang = np.concatenate([(t // 64).astype(np.float32)[:, None] * inv, (t % 64).astype(np.float32)[:, None] * inv], -1)
    cs = np.zeros((TALL, 64), np.float32)
    cs[:256, 0:32] = 1.0
    cs[256:, 0:32] = np.cos(ang)
    cs[256:, 32:64] = np.sin(ang)
    return cs


def _rpb_table(rpb):
    L = rpb.shape[0]
    a = np.arange(2)[:, None, None, None]
    kc = np.arange(64)[None, :, None, None]
    dr = np.arange(14)[None, None, :, None]
    c = np.arange(64)[None, None, None, :]
    ws = np.clip(c - 8, 0, 48)
    inw = (kc >= ws) & (kc < ws + 16)
    ci = np.clip(kc - c + 15, 0, 30)
    ri = dr + a
    out = np.empty((L, 2, 64, 8, 14, 64), np.float32)
    for h in range(8):
        g = rpb[:, h][:, np.broadcast_to(ri, (2, 64, 14, 64)), np.broadcast_to(ci, (2, 64, 14, 64))]
        out[:, :, :, h] = np.where(np.broadcast_to(inw, (2, 64, 14, 64))[None], g, np.float32(NEGB))
    return np.ascontiguousarray(out.reshape(L, 128, 8 * 14 * 64))


def prep_shared(inp, NL=4, moe_in=True):
    f = lambda a: np.ascontiguousarray(np.asarray(a, dtype=np.float32))
    sh = {
        "consts": make_consts(),
        "norm1_gain": f(inp["norm1_gain"]), "norm2_gain": f(inp["norm2_gain"]), "w_ada": f(inp["w_ada"]), "b_ada": f(inp["b_ada"]),
        "w_in": f(inp["w_in"]), "w_out": f(inp["w_out"]), "hgrn_lb_logits": f(inp["hgrn_lb_logits"]),
        "hgrn_gn_gain": f(inp["hgrn_gn_gain"]).reshape(4, 256),
        "rwkv_mu": f(inp["rwkv_mu"]), "rwkv_w0": f(inp["rwkv_w0"]), "rwkv_w_up": f(inp["rwkv_w_up"]), "rwkv_a0": f(inp["rwkv_a0"]),
        "rwkv_a_up": f(inp["rwkv_a_up"]), "rwkv_g_up": f(inp["rwkv_g_up"]), "rwkv_kk_scale": f(inp["rwkv_kk_scale"]),
        "rwkv_k_a": f(inp["rwkv_k_a"]), "rwkv_r_k": f(inp["rwkv_r_k"]).reshape(4, 256), "rwkv_ln_gain": f(inp["rwkv_ln_gain"]),
        "rwkv_ln_bias": f(inp["rwkv_ln_bias"]), "na_q_gain": f(inp["na_q_gain"]), "na_k_gain": f(inp["na_k_gain"]),
        "rpbT": _rpb_table(f(inp["na_rpb"])), "rope_cs": _rope_table(),
        "w_router": np.ascontiguousarray(np.concatenate([f(inp["w_router_group"]), f(inp["w_router_expert"])], -1)),
        "b_router": np.ascontiguousarray(np.concatenate([f(inp["b_router_group"]), f(inp["b_router_expert"])], -1)),
        "w_exp_gate": f(inp["w_exp_gate"]).reshape(4, 32, D, 512), "w_exp_up": f(inp["w_exp_up"]).reshape(4, 32, D, 512),
        "w_exp_down": f(inp["w_exp_down"]).reshape(4, 32, 512, D),
    }
    for k in list(sh.keys()):
        if sh[k].shape[0] == 4 and k != "consts" and NL < 4:
            sh[k] = np.ascontiguousarray(sh[k][:NL])
        if k.startswith("w_exp") and not moe_in:
            sh[k] = np.zeros((1, 1, 128, 128), np.float32)
    return sh


def prep_core(inp, b, shared):
    d = dict(shared)
    d["x_all"] = np.ascontiguousarray(np.concatenate([np.asarray(inp["ctx"][b], np.float32), np.asarray(inp["x"][b], np.float32)], 0))
    d["c2"] = np.ascontiguousarray(np.stack([np.asarray(inp["c"][b], np.float32), np.asarray(inp["c_ctx"], np.float32)], 1))
    return d


def _bc(ap, shape, axis):
    return ap.unsqueeze(axis).to_broadcast(list(shape))


class ProgMix(Prog):
    def stage_hgrn(self, l):
        from contextlib import ExitStack
        M, nc, I = self.M, self.nc, self.I
        cst = self.cst
        with ExitStack() as es:
            lb = [self.sb(es, f"lb{d}", [128, 256]) for d in range(2)]
            oml = [self.sb(es, f"oml{d}", [128, 256]) for d in range(2)]
            for d in range(2):
                M.dma("sp", lb[d].t[:], self.lbs.t[d, l:l + 1, :].partition_broadcast(128), r=[self.lbs.k], w=[lb[d].k])
                M.dve(lambda e: e.tensor_scalar(out=oml[d].t[:], in0=lb[d].t[:], scalar1=-1.0, scalar2=1.0, op0=ALU.mult, op1=ALU.add),
                      r=[lb[d].k], w=[oml[d].k])
            gnb = self.sb(es, "gnb", [128, 256])
            self.bload("sp", gnb, I["hgrn_gn_gain"][l:l + 1, :], 256)
            ofw = self.sb(es, "ofw", [128, NT, 256])
            Sr = [self.sb(es, f"S{i}", [64, 4, 64]) for i in range(4)]
            NB = 2
            q_ = [self.sb(es, f"q{i}", [128, 256]) for i in range(NB)]
            f_ = [self.sb(es, f"f{i}", [128, 256]) for i in range(NB)]
            v_ = [self.sb(es, f"v{i}", [128, 256]) for i in range(NB)]
            g_ = [self.sb(es, f"g{i}", [128, 256]) for i in range(NB)]
            kk = self.sb(es, "kk", [128, 256])
            ec = self.sb(es, "ec", [128, 256])
            en = self.sb(es, "en", [128, 256])
            er = self.sb(es, "er", [128, 256])
            qt = self.sb(es, "qt", [128, 256])
            kt = self.sb(es, "kt", [128, 256])
            kh = self.sb(es, "kh", [128, 256])
            khm = self.sb(es, "khm", [128, 4, 256])
            qtT = self.sb(es, "qtT", [64, 4, 128])
            ktT = self.sb(es, "ktT", [64, 4, 128])
            ecT = self.sb(es, "ecT", [64, 4, 128])
            qtTm = self.sb(es, "qtTm", [64, 4, 4, 128])
            scm = self.sb(es, "scm", [128, 4, 128])
            osum = self.sb(es, "osum", [128, 256])
            sq = self.sb(es, "sqh", [128, 256])
            st4 = self.sb(es, "st4", [128, 4])
            mo = [self.sb(es, f"mo{i}", [128, 256], BF16) for i in range(2)]
            pcr = self.ps(es, "pcr", [128, 512])
            pT = [self.ps(es, f"pT{i}", [64, 4, 128]) for i in range(3)]
            psc = self.ps(es, "psc", [128, 4, 128])
            pkv = [self.ps(es, f"pkv{i}", [64, 8, 64]) for i in range(2)]
            po = self.ps(es, "po", [128, 4, 64])
            C = cst.t
            it = 0
            for d in range(2):
                CUM = [C_U32, C_L32][d]
                REM = [C_SL32, C_SU32][d]
                fcol = 256 + 256 * d
                order = list(range(NT)) if d == 0 else [1, 0] + list(range(NT - 1, 1, -1))
                chunks = [0, 1, 2, 3] if d == 0 else [3, 2, 1, 0]
                si = 0
                S = Sr[0]
                M.dve(lambda e: e.memset(S.t[:], 0.0), w=[S.k])
                for j in order:
                    b = it % NB
                    it += 1
                    rows = slice(j * 128, (j + 1) * 128)
                    q, f, v = q_[b], f_[b], v_[b]
                    M.dma("sp", q.t[:], self.ptm.t[rows, 0:256], r=[self.ptm.k], w=[q.k])
                    M.dma("sp", f.t[:], self.ptm.t[rows, fcol:fcol + 256], r=[self.ptm.k], w=[f.k])
                    M.dma("sp", v.t[:], self.ptm.t[rows, 768:1024], r=[self.ptm.k], w=[v.k])
                    if d == 1:
                        M.dma("sp", g_[b].t[:], self.ptm.t[rows, 1024:1280], r=[self.ptm.k], w=[g_[b].k])
                    M.act(lambda e: e.activation(out=f.t[:], in_=f.t[:], func=AF.Sigmoid), r=[f.k], w=[f.k])
                    M.dve(lambda e: e.tensor_tensor(out=f.t[:], in0=f.t[:], in1=oml[d].t[:], op=ALU.mult), r=[f.k, oml[d].k], w=[f.k])
                    M.dve(lambda e: e.tensor_tensor(out=f.t[:], in0=f.t[:], in1=lb[d].t[:], op=ALU.add), r=[f.k, lb[d].k], w=[f.k])
                    M.dve(lambda e: e.tensor_scalar(out=kk.t[:], in0=f.t[:], scalar1=-1.0, scalar2=1.0, op0=ALU.mult, op1=ALU.add), r=[f.k], w=[kk.k])
                    M.dve(lambda e: e.tensor_scalar(out=f.t[:], in0=f.t[:], scalar1=1e-20, scalar2=None, op0=ALU.max), r=[f.k], w=[f.k])
                    M.act(lambda e: e.activation(out=f.t[:], in_=f.t[:], func=AF.Ln), r=[f.k], w=[f.k])
                    M.pe(lambda e: e.matmul(pcr.t[:, 0:256], lhsT=C[:, CUM:CUM + 128], rhs=f.t[:], start=True, stop=True), r=[cst.k, f.k], w=[pcr.k])
                    M.pe(lambda e: e.matmul(pcr.t[:, 256:512], lhsT=C[:, REM:REM + 128], rhs=f.t[:], start=True, stop=True), r=[cst.k, f.k], w=[pcr.k])
                    M.act(lambda e: e.activation(out=ec.t[:], in_=pcr.t[:, 0:256], func=AF.Exp), r=[pcr.k], w=[ec.k])
                    M.act(lambda e: e.activation(out=en.t[:], in_=pcr.t[:, 0:256], func=AF.Exp, scale=-1.0), r=[pcr.k], w=[en.k])
                    M.act(lambda e: e.activation(out=er.t[:], in_=pcr.t[:, 256:512], func=AF.Exp), r=[pcr.k], w=[er.k])
                    M.dve(lambda e: e.tensor_tensor(out=qt.t[:], in0=q.t[:], in1=ec.t[:], op=ALU.mult), r=[q.k, ec.k], w=[qt.k])
                    M.dve(lambda e: e.tensor_tensor(out=kt.t[:], in0=kk.t[:], in1=en.t[:], op=ALU.mult), r=[kk.k, en.k], w=[kt.k])
                    M.dve(lambda e: e.tensor_tensor(out=kh.t[:], in0=kk.t[:], in1=er.t[:], op=ALU.mult), r=[kk.k, er.k], w=[kh.k])
                    for c in range(4):
                        M.dve(lambda e: e.tensor_scalar(out=khm.t[:, c, :], in0=kh.t[:], scalar1=C[:, C_CM32 + c:C_CM32 + c + 1], scalar2=None, op0=ALU.mult),
                              r=[kh.k, cst.k], w=[khm.k])
                    for (src, pt_, dst) in ((qt, pT[0], qtT), (kt, pT[1], ktT), (ec, pT[2], ecT)):
                        for h in range(4):
                            M.pe(lambda e: e.transpose(pt_.t[:, h, :], src.t[:, h * 64:(h + 1) * 64], C[:, C_ID:C_ID + 128]), r=[src.k, cst.k], w=[pt_.k])
                        M.act(lambda e: e.copy(out=dst.t[:], in_=pt_.t[:]), r=[pt_.k], w=[dst.k])
                    for c in range(4):
                        M.dve(lambda e: e.tensor_tensor(out=qtTm.t[:, c, :, :], in0=qtT.t[:], in1=_bc(C[0:64, C_COL32 + c * 128:C_COL32 + (c + 1) * 128], [64, 4, 128], 1),
                                                        op=ALU.mult), r=[qtT.k, cst.k], w=[qtTm.k])
                    for h in range(4):
                        M.pe(lambda e: e.matmul(psc.t[:, h, :], lhsT=ktT.t[:, h, :], rhs=qtT.t[:, h, :], start=True, stop=True), r=[ktT.k, qtT.k], w=[psc.k])
                    M.dve(lambda e: e.tensor_tensor(out=scm.t[:], in0=psc.t[:], in1=_bc(C[:, CUM:CUM + 128], [128, 4, 128], 1), op=ALU.mult),
                          r=[psc.k, cst.k], w=[scm.k])
                    for c in range(4):
                        for h in range(4):
                            M.pe(lambda e: e.matmul(pkv[c // 2].t[:, (c % 2) * 4 + h, :], lhsT=khm.t[:, c, h * 64:(h + 1) * 64], rhs=v.t[:, h * 64:(h + 1) * 64],
                                                    start=True, stop=True), r=[khm.k, v.k], w=[pkv[c // 2].k])
                    for h in range(4):
                        M.pe(lambda e: e.matmul(po.t[:, h, :], lhsT=scm.t[:, h, :], rhs=v.t[:, h * 64:(h + 1) * 64], start=(h == 0), stop=False, skip_group_check=True), r=[scm.k, v.k], w=[po.k])
                    for ci, c in enumerate(chunks):
                        for h in range(4):
                            M.pe(lambda e: e.matmul(po.t[:, h, :], lhsT=qtTm.t[:, c, h, :], rhs=S.t[:, h, :], start=False, stop=(ci == 3), skip_group_check=True),
                                 r=[qtTm.k, S.k], w=[po.k])
                        si = (si + 1) % 4
                        Sn = Sr[si]
                        col = 32 * c + 31 if d == 0 else 32 * c
                        M.dve(lambda e: e.tensor_tensor(out=Sn.t[:], in0=S.t[:], in1=ecT.t[:, :, col:col + 1].to_broadcast([64, 4, 64]), op=ALU.mult),
                              r=[S.k, ecT.k], w=[Sn.k])
                        M.dve(lambda e: e.tensor_tensor(out=Sn.t[:], in0=Sn.t[:], in1=pkv[c // 2].t[:, (c % 2) * 4:(c % 2) * 4 + 4, :], op=ALU.add),
                              r=[Sn.k, pkv[c // 2].k], w=[Sn.k])
                        S = Sn
                    if d == 0 and j == 0:
                        for nm_, b_ in (("d_logf", f), ("d_ec", ec), ("d_qt", qt), ("d_kt", kt), ("d_kh", kh), ("d_qtT", qtT), ("d_ktT", ktT), ("d_ecT", ecT),
                                        ("d_scm", scm), ("d_S", S), ("d_khm", khm), ("d_qtTm", qtTm)):
                            self.dump(nm_, b_)
                    if d == 0:
                        M.act(lambda e: e.copy(out=ofw.t[:, j, :], in_=po.t[:].rearrange("p h d -> p (h d)")), r=[po.k], w=[ofw.k])
                        if j == 0:
                            self.dump("d_o0", ofw, ofw.t[:, 0, :])
                    else:
                        g = g_[b]
                        m_ = mo[it % 2]
                        M.dve(lambda e: e.tensor_tensor(out=osum.t[:], in0=po.t[:].rearrange("p h d -> p (h d)"), in1=ofw.t[:, j, :], op=ALU.add),
                              r=[po.k, ofw.k], w=[osum.k])
                        M.act(lambda e: e.activation(out=sq.t[:], in_=osum.t[:], func=AF.Square), r=[osum.k], w=[sq.k])
                        M.dve(lambda e: e.tensor_reduce(out=st4.t[:], in_=sq.t[:].rearrange("p (h d) -> p h d", h=4), axis=AX.X, op=ALU.add), r=[sq.k], w=[st4.k])
                        M.dve(lambda e: e.tensor_scalar(out=st4.t[:], in0=st4.t[:], scalar1=1.0 / 64, scalar2=EPS, op0=ALU.mult, op1=ALU.add), r=[st4.k], w=[st4.k])
                        M.act(lambda e: e.activation(out=st4.t[:], in_=st4.t[:], func=AF.Sqrt), r=[st4.k], w=[st4.k])
                        M.dve(lambda e: e.reciprocal(out=st4.t[:], in_=st4.t[:]), r=[st4.k], w=[st4.k])
                        M.dve(lambda e: e.tensor_tensor(out=osum.t[:].rearrange("p (h d) -> p h d", h=4), in0=osum.t[:].rearrange("p (h d) -> p h d", h=4),
                                                        in1=_bc(st4.t[:], [128, 4, 64], 2), op=ALU.mult), r=[osum.k, st4.k], w=[osum.k])
                        M.dve(lambda e: e.tensor_tensor(out=osum.t[:], in0=osum.t[:], in1=gnb.t[:], op=ALU.mult), r=[osum.k, gnb.k], w=[osum.k])
                        M.act(lambda e: e.activation(out=g.t[:], in_=g.t[:], func=AF.Silu), r=[g.k], w=[g.k])
                        M.dve(lambda e: e.tensor_tensor(out=m_.t[:], in0=osum.t[:], in1=g.t[:], op=ALU.mult), r=[osum.k, g.k], w=[m_.k])
                        M.dma("pool", self.mixs.t[rows, 0:256], m_.t[:], r=[m_.k], w=[self.mixs.k])
            M.barrier()


RWKV_LN_EPS = 64e-5
WSCALE = -float(np.exp(-0.5))


class ProgMix2(ProgMix):
    def stage_rwkv(self, l, lrT):
        from contextlib import ExitStack
        M, nc, I = self.M, self.nc, self.I
        cst = self.cst
        C = cst.t
        with ExitStack() as es:
            wup = self.sb(es, "wup", [128, 256])
            aup = self.sb(es, "aup", [128, 256])
            gup = self.sb(es, "gup", [128, 256])
            brow = self.sb(es, "browr", [1, 4, 256])
            M.dma("sp", wup.t[:], I["rwkv_w_up"][l].rearrange("d r n -> (d r) n"), w=[wup.k])
            M.dma("sp", aup.t[:], I["rwkv_a_up"][l].rearrange("d r n -> (d r) n"), w=[aup.k])
            M.dma("sp", gup.t[:], I["rwkv_g_up"][l], w=[gup.k])
            M.dma("sp", brow.t[0:1, 0:2, :], I["rwkv_w0"][l:l + 1, :, :], w=[brow.k])
            M.dma("sp", brow.t[0:1, 2:4, :], I["rwkv_a0"][l:l + 1, :, :], w=[brow.k])
            bt = {}
            for nm in ("rwkv_kk_scale", "rwkv_k_a", "rwkv_r_k", "rwkv_ln_gain", "rwkv_ln_bias"):
                bt[nm] = self.sb(es, nm, [64, 256])
                M.dma("sp", bt[nm].t[:], I[nm][l:l + 1, :].partition_broadcast(64), w=[bt[nm].k])
            kab = bt["rwkv_k_a"]
            omka = self.sb(es, "omka", [64, 256])
            M.dve(lambda e: e.tensor_scalar(out=omka.t[:], in0=kab.t[:], scalar1=-1.0, scalar2=1.0, op0=ALU.mult, op1=ALU.add), r=[kab.k], w=[omka.k])
            yfw = self.sb(es, "yfw", [64, 36, 256])
            Sr = [self.sb(es, f"RS{i}", [64, 4, 64]) for i in range(3)]
            r_ = [self.sb(es, f"r{i}", [64, 256]) for i in range(2)]
            k_ = [self.sb(es, f"k{i}", [64, 256]) for i in range(2)]
            v_ = [self.sb(es, f"v{i}", [64, 256]) for i in range(2)]
            tw = self.sb(es, "tw", [128, 64])
            sz = self.sb(es, "sz", [64, 512])
            lw = self.sb(es, "lw", [64, 256])
            kk = self.sb(es, "kk", [64, 256])
            sq = self.sb(es, "sq", [64, 256])
            s4 = self.sb(es, "s4", [64, 4])
            t1 = self.sb(es, "t1", [64, 256])
            kd = self.sb(es, "kd", [64, 256])
            ak = self.sb(es, "ak", [64, 256])
            ecx = self.sb(es, "ecx", [64, 512])
            en = self.sb(es, "en", [64, 256])
            er = self.sb(es, "er", [64, 256])
            rt = self.sb(es, "rt", [64, 256])
            kkt = self.sb(es, "kkt", [64, 256])
            akt = self.sb(es, "akt", [64, 256])
            kdt = self.sb(es, "kdt", [64, 256])
            akh = self.sb(es, "akh", [64, 256])
            kdh = self.sb(es, "kdh", [64, 256])
            X2 = self.sb(es, "X2", [64, 4, 2, 64])
            aktT = self.sb(es, "aktT", [64, 4, 64])
            kdtT = self.sb(es, "kdtT", [64, 4, 64])
            ecT = self.sb(es, "ecT", [64, 4, 64])
            NTm = self.sb(es, "NTm", [64, 4, 64])
            BakT = self.sb(es, "BakT", [64, 4, 64])
            AkT = self.sb(es, "AkT", [64, 4, 64])
            BkT = self.sb(es, "BkT", [64, 4, 64])
            Xa = [self.sb(es, f"Xa{i}", [64, 4, 64]) for i in range(2)]
            XTa = [self.sb(es, f"XTa{i}", [64, 4, 64]) for i in range(2)]
            PT = [self.sb(es, f"PT{i}", [64, 4, 64]) for i in range(2)]
            Wn = self.sb(es, "Wn", [64, 4, 64])
            U = self.sb(es, "U", [64, 4, 64])
            ysum = self.sb(es, "ysum", [64, 256])
            sgd = self.sb(es, "sgd", [128, 64])
            af = self.sb(es, "af", [64, 256])
            bon = self.sb(es, "bon", [64, 256])
            mo = [self.sb(es, f"rmo{i}", [64, 256], BF16) for i in range(2)]
            pz = self.ps(es, "pz", [64, 512])
            pc0 = self.ps(es, "pc0", [64, 512])
            pc1 = self.ps(es, "pc1", [64, 512])
            pT = [self.ps(es, f"rpT{i}", [64, 4, 64]) for i in range(2)]
            pg = [self.ps(es, f"pg{i}", [64, 4, 64]) for i in range(3)]
            pYb = B_(pc1.t[:, 256:512].rearrange("p (h d) -> p h d", h=4), "pY")
            gi = [0]

            def nextpg():
                gi[0] = (gi[0] + 1) % 3
                return pg[gi[0]]

            it = 0
            for d in range(2 if getattr(self, 'rw_stop', 99) >= 9 else 1):
                CUM, SCUM, REM = [(C_U64, C_SU64, C_SL64), (C_L64, C_SL64, C_SU64)][d]
                MSTR, MINC, MSTRT = [(C_SU64, C_U64, C_SL64), (C_SL64, C_L64, C_SU64)][d]
                order = list(range(36)) if d == 0 else [3, 2, 1, 0] + list(range(35, 3, -1))
                chunks = [0]
                P0 = 64 * d
                si = 0
                S = Sr[0]
                M.dve(lambda e: e.memset(S.t[:], 0.0), w=[S.k])
                for j in order:
                    b = it % 2
                    it += 1
                    rows = slice(j * 64, (j + 1) * 64)
                    tok = slice(j * 64, (j + 1) * 64)
                    r, k, v = r_[b], k_[b], v_[b]
                    M.dma("sp", r.t[:], self.ptm.t[rows, 1280:1536], r=[self.ptm.k], w=[r.k])
                    M.dma("sp", k.t[:], self.ptm.t[rows, 1536:1792], r=[self.ptm.k], w=[k.k])
                    M.dma("sp", v.t[:], self.ptm.t[rows, 1792:2048], r=[self.ptm.k], w=[v.k])
                    M.act(lambda e: e.activation(out=tw.t[P0:P0 + 64, :], in_=lrT.t[P0:P0 + 64, 0, tok], func=AF.Tanh), r=[lrT.k], w=[tw.k])
                    M.pe(lambda e: e.matmul(pz.t[:, 0:256], lhsT=tw.t[P0:P0 + 64, :], rhs=wup.t[P0:P0 + 64, :], start=True, stop=False, skip_group_check=True),
                         r=[tw.k, wup.k], w=[pz.k])
                    M.pe(lambda e: e.matmul(pz.t[:, 0:256], lhsT=self.ones.t[0:1, 0:64], rhs=brow.t[0:1, d, :], start=False, stop=False, skip_group_check=True),
                         r=[self.ones.k, brow.k], w=[pz.k])
                    M.pe(lambda e: e.matmul(pz.t[:, 256:512], lhsT=lrT.t[P0:P0 + 64, 1, tok], rhs=aup.t[P0:P0 + 64, :], start=False, stop=False, skip_group_check=True),
                         r=[lrT.k, aup.k], w=[pz.k])
                    M.pe(lambda e: e.matmul(pz.t[:, 256:512], lhsT=self.ones.t[0:1, 0:64], rhs=brow.t[0:1, 2 + d, :], start=False, stop=True, skip_group_check=True),
                         r=[self.ones.k, brow.k], w=[pz.k])
                    M.act(lambda e: e.activation(out=sz.t[:], in_=pz.t[:], func=AF.Sigmoid), r=[pz.k], w=[sz.k])
                    M.dve(lambda e: e.tensor_scalar(out=lw.t[:], in0=sz.t[:, 0:256], scalar1=WSCALE, scalar2=None, op0=ALU.mult), r=[sz.k], w=[lw.k])
                    if getattr(self, 'rw_stop', 99) == 1:
                        M.barrier()
                        return

                    M.dve(lambda e: e.tensor_tensor(out=kk.t[:], in0=k.t[:], in1=bt["rwkv_kk_scale"].t[:], op=ALU.mult), r=[k.k, bt["rwkv_kk_scale"].k], w=[kk.k])
                    M.act(lambda e: e.activation(out=sq.t[:], in_=kk.t[:], func=AF.Square), r=[kk.k], w=[sq.k])
                    M.dve(lambda e: e.tensor_reduce(out=s4.t[:], in_=sq.t[:].rearrange("p (h d) -> p h d", h=4), axis=AX.X, op=ALU.add), r=[sq.k], w=[s4.k])
                    M.act(lambda e: e.activation(out=s4.t[:], in_=s4.t[:], func=AF.Sqrt), r=[s4.k], w=[s4.k])
                    M.dve(lambda e: e.tensor_scalar(out=s4.t[:], in0=s4.t[:], scalar1=1e-12, scalar2=None, op0=ALU.max), r=[s4.k], w=[s4.k])
                    M.dve(lambda e: e.reciprocal(out=s4.t[:], in_=s4.t[:]), r=[s4.k], w=[s4.k])
                    M.dve(lambda e: e.tensor_tensor(out=kk.t[:].rearrange("p (h d) -> p h d", h=4), in0=kk.t[:].rearrange("p (h d) -> p h d", h=4),
                                                    in1=_bc(s4.t[:], [64, 4, 64], 2), op=ALU.mult), r=[kk.k, s4.k], w=[kk.k])
                    a_ = sz.t[:, 256:512]
                    M.dve(lambda e: e.tensor_tensor(out=t1.t[:], in0=a_, in1=kab.t[:], op=ALU.mult), r=[sz.k, kab.k], w=[t1.k])
                    M.dve(lambda e: e.tensor_tensor(out=t1.t[:], in0=t1.t[:], in1=omka.t[:], op=ALU.add), r=[t1.k, omka.k], w=[t1.k])
                    M.dve(lambda e: e.tensor_tensor(out=kd.t[:], in0=k.t[:], in1=t1.t[:], op=ALU.mult), r=[k.k, t1.k], w=[kd.k])
                    M.dve(lambda e: e.tensor_tensor(out=ak.t[:], in0=a_, in1=kk.t[:], op=ALU.mult), r=[sz.k, kk.k], w=[ak.k])
                    if getattr(self, 'rw_stop', 99) == 2:
                        M.barrier()
                        return

                    M.pe(lambda e: e.matmul(pc0.t[:, 0:256], lhsT=C[0:64, CUM:CUM + 64], rhs=lw.t[:], start=True, stop=True), r=[cst.k, lw.k], w=[pc0.k])
                    M.pe(lambda e: e.matmul(pc0.t[:, 256:512], lhsT=C[0:64, SCUM:SCUM + 64], rhs=lw.t[:], start=True, stop=True), r=[cst.k, lw.k], w=[pc0.k])
                    M.pe(lambda e: e.matmul(pc1.t[:, 0:256], lhsT=C[0:64, REM:REM + 64], rhs=lw.t[:], start=True, stop=True), r=[cst.k, lw.k], w=[pc1.k])
                    M.act(lambda e: e.activation(out=ecx.t[:], in_=pc0.t[:], func=AF.Exp), r=[pc0.k], w=[ecx.k])
                    M.act(lambda e: e.activation(out=en.t[:], in_=pc0.t[:, 0:256], func=AF.Exp, scale=-1.0), r=[pc0.k], w=[en.k])
                    M.act(lambda e: e.activation(out=er.t[:], in_=pc1.t[:, 0:256], func=AF.Exp), r=[pc1.k], w=[er.k])
                    for (o_, a0, a1) in ((rt, r.t[:], ecx.t[:, 0:256]), (kkt, kk.t[:], ecx.t[:, 256:512]), (akt, ak.t[:], en.t[:]), (kdt, kd.t[:], en.t[:]),
                                         (akh, ak.t[:], er.t[:]), (kdh, kd.t[:], er.t[:])):
                        M.dve(lambda e: e.tensor_tensor(out=o_.t[:], in0=a0, in1=a1, op=ALU.mult), r=[r.k, kk.k, ak.k, kd.k, ecx.k, en.k, er.k], w=[o_.k])
                    if getattr(self, 'rw_stop', 99) == 3:
                        M.barrier()
                        return

                    ti = 0
                    for (src, dst_ap, dstb) in ((kkt, X2.t[:, :, 0, :], X2), (rt, X2.t[:, :, 1, :], X2), (akt, aktT.t[:], aktT), (kdt, kdtT.t[:], kdtT),
                                                (ecx, ecT.t[:], ecT)):
                        p_ = pT[ti % 2]
                        ti += 1
                        for h in range(4):
                            M.pe(lambda e: e.transpose(p_.t[:, h, :], src.t[:, h * 64:(h + 1) * 64], C[0:64, C_ID:C_ID + 64]), r=[src.k, cst.k], w=[p_.k])
                        M.act(lambda e: e.copy(out=dst_ap, in_=p_.t[:]), r=[p_.k], w=[dstb.k])
                    if getattr(self, 'rw_stop', 99) == 4:
                        M.barrier()
                        return

                    for (lhs, ridx, msk, dst) in ((aktT, 0, MSTR, NTm), (aktT, 1, MINC, BakT), (kdtT, 0, MSTR, AkT), (kdtT, 1, MINC, BkT)):
                        p_ = nextpg()
                        for h in range(4):
                            M.pe(lambda e: e.matmul(p_.t[:, h, :], lhsT=lhs.t[:, h, :], rhs=X2.t[:, h, ridx, :], start=True, stop=True), r=[lhs.k, X2.k], w=[p_.k])
                        M.dve(lambda e: e.tensor_tensor(out=dst.t[:], in0=p_.t[:], in1=_bc(C[0:64, msk:msk + 64], [64, 4, 64], 1), op=ALU.mult),
                              r=[p_.k, cst.k], w=[dst.k])
                    p_ = nextpg()
                    for h in range(4):
                        M.pe(lambda e: e.matmul(p_.t[:, h, :], lhsT=X2.t[:, h, 0, :], rhs=aktT.t[:, h, :], start=True, stop=True), r=[aktT.k, X2.k], w=[p_.k])
                    X, XT, P_ = Xa[0], NTm, PT[0]
                    M.dve(lambda e: e.tensor_tensor(out=X.t[:], in0=p_.t[:], in1=_bc(C[0:64, MSTRT:MSTRT + 64], [64, 4, 64], 1), op=ALU.mult),
                          r=[p_.k, cst.k], w=[X.k])
                    if getattr(self, 'rw_stop', 99) == 5:
                        M.barrier()
                        return

                    M.dve(lambda e: e.scalar_tensor_tensor(out=P_.t[:], in0=XT.t[:], scalar=-1.0, in1=_bc(C[0:64, C_ID:C_ID + 64], [64, 4, 64], 1), op0=ALU.mult, op1=ALU.add),
                          r=[XT.k, cst.k], w=[P_.k])
                    for i in range(1, 6):
                        Xn = Xa[i % 2]
                        XTn = XTa[i % 2]
                        pX = nextpg()
                        for h in range(4):
                            M.pe(lambda e: e.matmul(pX.t[:, h, :], lhsT=XT.t[:, h, :], rhs=X.t[:, h, :], start=True, stop=True), r=[XT.k, X.k], w=[pX.k])
                        M.act(lambda e: e.copy(out=Xn.t[:], in_=pX.t[:]), r=[pX.k], w=[Xn.k])
                        if i < 5:
                            pXT = nextpg()
                            for h in range(4):
                                M.pe(lambda e: e.matmul(pXT.t[:, h, :], lhsT=X.t[:, h, :], rhs=XT.t[:, h, :], start=True, stop=True), r=[XT.k, X.k], w=[pXT.k])
                            M.act(lambda e: e.copy(out=XTn.t[:], in_=pXT.t[:]), r=[pXT.k], w=[XTn.k])
                        pP = nextpg()
                        for h in range(4):
                            M.pe(lambda e: e.matmul(pP.t[:, h, :], lhsT=Xn.t[:, h, :], rhs=P_.t[:, h, :], start=True, stop=True), r=[Xn.k, P_.k], w=[pP.k])
                        Pn = PT[i % 2]
                        M.dve(lambda e: e.tensor_tensor(out=Pn.t[:], in0=P_.t[:], in1=pP.t[:], op=ALU.add), r=[P_.k, pP.k], w=[Pn.k])
                        X, XT, P_ = Xn, XTn, Pn
                    if getattr(self, 'rw_stop', 99) == 6:
                        M.barrier()
                        return

                    pY = pYb
                    for ci, c in enumerate(chunks):
                        R = slice(64 * c, 64 * c + 64)
                        pW = nextpg()
                        for h in range(4):
                            hs = slice(h * 64, (h + 1) * 64)
                            M.pe(lambda e: e.matmul(pW.t[R, h, 0:64], lhsT=X2.t[:, h, 0, R], rhs=S.t[:, h, :], start=(h == 0), stop=False, skip_group_check=True),
                                 r=[X2.k, S.k], w=[pW.k])
                            M.pe(lambda e: e.matmul(pW.t[R, h, 0:64], lhsT=AkT.t[R, h, R], rhs=v.t[R, hs], start=False, stop=True, skip_group_check=True),
                                 r=[AkT.k, v.k], w=[pW.k])
                        M.act(lambda e: e.mul(out=Wn.t[R, :, :], in_=pW.t[R, :, 0:64], mul=-1.0), r=[pW.k], w=[Wn.k])
                        if getattr(self, 'rw_stop', 99) == 61 or (getattr(self, 'rw_stop', 99) == 65 and ci == 1):
                            M.barrier()
                            return

                        pU = nextpg()
                        for h in range(4):
                            M.pe(lambda e: e.matmul(pU.t[R, h, 0:64], lhsT=P_.t[R, h, R], rhs=Wn.t[R, h, :], start=True, stop=True), r=[P_.k, Wn.k], w=[pU.k])
                        M.act(lambda e: e.copy(out=U.t[R, :, :], in_=pU.t[R, :, 0:64]), r=[pU.k], w=[U.k])
                        if getattr(self, 'rw_stop', 99) == 62 or (getattr(self, 'rw_stop', 99) == 66 and ci == 1):
                            M.barrier()
                            return

                        for h in range(4):
                            hs = slice(h * 64, (h + 1) * 64)
                            M.pe(lambda e: e.matmul(pY.t[R, h, 0:64], lhsT=X2.t[:, h, 1, R], rhs=S.t[:, h, :], start=(h == 0), stop=False, skip_group_check=True),
                                 r=[X2.k, S.k], w=[pY.k])
                            M.pe(lambda e: e.matmul(pY.t[R, h, 0:64], lhsT=BakT.t[R, h, R], rhs=U.t[R, h, :], start=False, stop=False, skip_group_check=True),
                                 r=[BakT.k, U.k], w=[pY.k])
                            M.pe(lambda e: e.matmul(pY.t[R, h, 0:64], lhsT=BkT.t[R, h, R], rhs=v.t[R, hs], start=False, stop=True, skip_group_check=True),
                                 r=[BkT.k, v.k], w=[pY.k])
                        if getattr(self, 'rw_stop', 99) == 63 or (getattr(self, 'rw_stop', 99) == 67 and ci == 1):
                            M.barrier()
                            return

                        pS = nextpg()
                        for h in range(4):
                            hs = slice(h * 64, (h + 1) * 64)
                            M.pe(lambda e: e.matmul(pS.t[0:64, h, 0:64], lhsT=akh.t[R, hs], rhs=U.t[R, h, :], start=(h == 0), stop=False, skip_group_check=True),
                                 r=[akh.k, U.k], w=[pS.k])
                            M.pe(lambda e: e.matmul(pS.t[0:64, h, 0:64], lhsT=kdh.t[R, hs], rhs=v.t[R, hs], start=False, stop=True, skip_group_check=True),
                                 r=[kdh.k, v.k], w=[pS.k])
                        si = (si + 1) % 3
                        Sn = Sr[si]
                        col = 64 * c + 63 if d == 0 else 64 * c
                        M.dve(lambda e: e.tensor_tensor(out=Sn.t[:], in0=S.t[:], in1=ecT.t[:, :, col:col + 1].to_broadcast([64, 4, 64]), op=ALU.mult),
                              r=[S.k, ecT.k], w=[Sn.k])
                        M.dve(lambda e: e.tensor_tensor(out=Sn.t[:], in0=Sn.t[:], in1=pS.t[0:64, :, 0:64], op=ALU.add), r=[Sn.k, pS.k], w=[Sn.k])
                        if getattr(self, 'rw_stop', 99) == 64:
                            M.barrier()
                            return

                        S = Sn
                    if getattr(self, 'rw_stop', 99) == 7:
                        M.barrier()
                        return
                    if d == 0:
                        M.act(lambda e: e.copy(out=yfw.t[:, j, :].rearrange("p (h d) -> p h d", h=4), in_=pY.t[:, :, 0:64]), r=[pY.k], w=[yfw.k])
                    else:
                        m_ = mo[it % 2]
                        y3 = ysum.t[:].rearrange("p (h d) -> p h d", h=4)
                        M.dve(lambda e: e.tensor_tensor(out=y3, in0=pY.t[:, :, 0:64], in1=yfw.t[:, j, :].rearrange("p (h d) -> p h d", h=4), op=ALU.add),
                              r=[pY.k, yfw.k], w=[ysum.k])
                        M.dve(lambda e: e.tensor_reduce(out=s4.t[:], in_=y3, axis=AX.X, op=ALU.add), r=[ysum.k], w=[s4.k])
                        M.dve(lambda e: e.tensor_scalar(out=s4.t[:], in0=s4.t[:], scalar1=1.0 / 64, scalar2=None, op0=ALU.mult), r=[s4.k], w=[s4.k])
                        M.dve(lambda e: e.tensor_tensor(out=y3, in0=y3, in1=_bc(s4.t[:], [64, 4, 64], 2), op=ALU.subtract), r=[ysum.k, s4.k], w=[ysum.k])
                        M.act(lambda e: e.activation(out=sq.t[:], in_=ysum.t[:], func=AF.Square), r=[ysum.k], w=[sq.k])
                        M.dve(lambda e: e.tensor_reduce(out=s4.t[:], in_=sq.t[:].rearrange("p (h d) -> p h d", h=4), axis=AX.X, op=ALU.add), r=[sq.k], w=[s4.k])
                        M.dve(lambda e: e.tensor_scalar(out=s4.t[:], in0=s4.t[:], scalar1=1.0 / 64, scalar2=RWKV_LN_EPS, op0=ALU.mult, op1=ALU.add), r=[s4.k], w=[s4.k])
                        M.act(lambda e: e.activation(out=s4.t[:], in_=s4.t[:], func=AF.Sqrt), r=[s4.k], w=[s4.k])
                        M.dve(lambda e: e.reciprocal(out=s4.t[:], in_=s4.t[:]), r=[s4.k], w=[s4.k])
                        M.dve(lambda e: e.tensor_tensor(out=y3, in0=y3, in1=_bc(s4.t[:], [64, 4, 64], 2), op=ALU.mult), r=[ysum.k, s4.k], w=[ysum.k])
                        M.dve(lambda e: e.tensor_tensor(out=ysum.t[:], in0=ysum.t[:], in1=bt["rwkv_ln_gain"].t[:], op=ALU.mult), r=[ysum.k, bt["rwkv_ln_gain"].k], w=[ysum.k])
                        M.dve(lambda e: e.tensor_tensor(out=ysum.t[:], in0=ysum.t[:], in1=bt["rwkv_ln_bias"].t[:], op=ALU.add), r=[ysum.k, bt["rwkv_ln_bias"].k], w=[ysum.k])
                        M.pe(lambda e: e.matmul(pz.t[:, 0:256], lhsT=lrT.t[0:64, 1, tok], rhs=aup.t[0:64, :], start=True, stop=False, skip_group_check=True),
                             r=[lrT.k, aup.k], w=[pz.k])
                        M.pe(lambda e: e.matmul(pz.t[:, 0:256], lhsT=self.ones.t[0:1, 0:64], rhs=brow.t[0:1, 2, :], start=False, stop=True, skip_group_check=True),
                             r=[self.ones.k, brow.k], w=[pz.k])
                        M.act(lambda e: e.activation(out=af.t[:], in_=pz.t[:, 0:256], func=AF.Sigmoid), r=[pz.k], w=[af.k])
                        M.dve(lambda e: e.tensor_tensor(out=af.t[:], in0=af.t[:], in1=sz.t[:, 256:512], op=ALU.add), r=[af.k, sz.k], w=[af.k])
                        M.dve(lambda e: e.tensor_scalar(out=af.t[:], in0=af.t[:], scalar1=-2.0, scalar2=None, op0=ALU.add), r=[af.k], w=[af.k])
                        M.dve(lambda e: e.tensor_tensor(out=af.t[:], in0=af.t[:], in1=kab.t[:], op=ALU.mult), r=[af.k, kab.k], w=[af.k])
                        M.dve(lambda e: e.tensor_scalar(out=af.t[:], in0=af.t[:], scalar1=2.0, scalar2=None, op0=ALU.add), r=[af.k], w=[af.k])
                        M.dve(lambda e: e.tensor_tensor(out=af.t[:], in0=af.t[:], in1=k.t[:], op=ALU.mult), r=[af.k, k.k], w=[af.k])
                        M.dve(lambda e: e.tensor_tensor(out=af.t[:], in0=af.t[:], in1=r.t[:], op=ALU.mult), r=[af.k, r.k], w=[af.k])
                        M.dve(lambda e: e.tensor_tensor(out=af.t[:], in0=af.t[:], in1=bt["rwkv_r_k"].t[:], op=ALU.mult), r=[af.k, bt["rwkv_r_k"].k], w=[af.k])
                        M.dve(lambda e: e.tensor_reduce(out=s4.t[:], in_=af.t[:].rearrange("p (h d) -> p h d", h=4), axis=AX.X, op=ALU.add), r=[af.k], w=[s4.k])
                        M.dve(lambda e: e.tensor_tensor(out=bon.t[:].rearrange("p (h d) -> p h d", h=4), in0=v.t[:].rearrange("p (h d) -> p h d", h=4),
                                                        in1=_bc(s4.t[:], [64, 4, 64], 2), op=ALU.mult), r=[v.k, s4.k], w=[bon.k])
                        M.dve(lambda e: e.tensor_tensor(out=ysum.t[:], in0=ysum.t[:], in1=bon.t[:], op=ALU.add), r=[ysum.k, bon.k], w=[ysum.k])
                        M.act(lambda e: e.activation(out=sgd.t[:], in_=lrT.t[:, 2, tok], func=AF.Sigmoid), r=[lrT.k], w=[sgd.k])
                        M.pe(lambda e: e.matmul(pz.t[:, 256:512], lhsT=sgd.t[:], rhs=gup.t[:], start=True, stop=True), r=[sgd.k, gup.k], w=[pz.k])
                        M.dve(lambda e: e.tensor_tensor(out=m_.t[:], in0=ysum.t[:], in1=pz.t[:, 256:512], op=ALU.mult), r=[ysum.k, pz.k], w=[m_.k])
                        M.dma("pool", self.mixs.t[rows, 256:512], m_.t[:], r=[m_.k], w=[self.mixs.k])
            M.barrier()


class ProgFull(ProgMix2):
    def stage_na(self, l):
        from contextlib import ExitStack
        M, nc, I = self.M, self.nc, self.I
        cst = self.cst
        C = cst.t
        with ExitStack() as es:
            T2 = self.sb(es, "T2", [128, 8, 14, 64])
            M.dma("sp", T2.t[:].rearrange("p h r c -> p (h r c)"), I["rpbT"][l], w=[T2.k])
            qT = self.sb(es, "qT", [128, 4, TALL], BF16)
            kT = self.sb(es, "kT", [128, 4, TALL], BF16)
            Va = self.sb(es, "Va", [128, NT, 8, 65], BF16)
            M.dve(lambda e: e.memset(Va.t[:], 1.0), w=[Va.k])
            gb = [self.sb(es, f"nag{i}", [128, 64]) for i in range(2)]
            M.dma("sp", gb[0].t[:], I["na_q_gain"][l:l + 1, :].partition_broadcast(128), w=[gb[0].k])
            M.dma("sp", gb[1].t[:], I["na_k_gain"][l:l + 1, :].partition_broadcast(128), w=[gb[1].k])
            M.dve(lambda e: e.tensor_scalar(out=gb[0].t[:], in0=gb[0].t[:], scalar1=0.125, scalar2=None, op0=ALU.mult), r=[gb[0].k], w=[gb[0].k])
            with ExitStack() as e2:
                qk_ = [self.sb(e2, f"qk{i}", [128, 512]) for i in range(4)]
                vv_ = [self.sb(e2, f"vv{i}", [128, 512]) for i in range(2)]
                cs_ = [self.sb(e2, f"cs{i}", [128, 64]) for i in range(2)]
                sq_ = [self.sb(e2, f"nsq{i}", [128, 512]) for i in range(2)] * 2
                s8_ = [self.sb(e2, f"s8{i}", [128, 8]) for i in range(4)]
                ta_ = [self.sb(e2, f"ta{i}", [128, 8, 32]) for i in range(4)]
                tb_ = [self.sb(e2, f"tb{i}", [128, 8, 32]) for i in range(4)]
                tc_, td_ = ta_, tb_
                qr_ = [self.sb(e2, f"qr{i}", [128, 8, 2, 32], BF16) for i in range(4)]
                pT = [self.ps(e2, f"npT{i}", [128, 4, 128], BF16) for i in range(4)]
                for j in range(NT):
                    vv, cs = vv_[j % 2], cs_[j % 2]
                    qk = [qk_[(j % 2) * 2], qk_[(j % 2) * 2 + 1]]
                    rows = slice(j * 128, (j + 1) * 128)
                    M.dma("sp", cs.t[:], I["rope_cs"][rows, :], w=[cs.k])
                    M.dma("sp", vv.t[:], self.ptm.t[rows, 3456:3968], r=[self.ptm.k], w=[vv.k])
                    M.dve(lambda e: e.tensor_copy(out=Va.t[:, j, :, 0:64], in_=vv.t[:].rearrange("p (h d) -> p h d", h=8)), r=[vv.k], w=[Va.k])
                    cosb = _bc(cs.t[:, 0:32], [128, 8, 32], 1)
                    sinb = _bc(cs.t[:, 32:64], [128, 8, 32], 1)
                    for i, (c0, dstT) in enumerate(((2432, qT), (2944, kT))):
                        x = qk[i]
                        si_ = (j % 2) * 2 + i
                        sq, s8, ta, tb, tc, td, qr = sq_[si_], s8_[si_], ta_[si_], tb_[si_], tc_[si_], td_[si_], qr_[si_]
                        M.dma("sp", x.t[:], self.ptm.t[rows, c0:c0 + 512], r=[self.ptm.k], w=[x.k])
                        M.act(lambda e: e.activation(out=sq.t[:], in_=x.t[:], func=AF.Square), r=[x.k], w=[sq.k])
                        M.dve(lambda e: e.tensor_reduce(out=s8.t[:], in_=sq.t[:].rearrange("p (h d) -> p h d", h=8), axis=AX.X, op=ALU.add), r=[sq.k], w=[s8.k])
                        M.dve(lambda e: e.tensor_scalar(out=s8.t[:], in0=s8.t[:], scalar1=1.0 / 64, scalar2=EPS, op0=ALU.mult, op1=ALU.add), r=[s8.k], w=[s8.k])
                        M.act(lambda e: e.activation(out=s8.t[:], in_=s8.t[:], func=AF.Sqrt), r=[s8.k], w=[s8.k])
                        M.dve(lambda e: e.reciprocal(out=s8.t[:], in_=s8.t[:]), r=[s8.k], w=[s8.k])
                        x3 = x.t[:].rearrange("p (h d) -> p h d", h=8)
                        M.dve(lambda e: e.tensor_tensor(out=x3, in0=x3, in1=_bc(s8.t[:], [128, 8, 64], 2), op=ALU.mult), r=[x.k, s8.k], w=[x.k])
                        M.dve(lambda e: e.tensor_tensor(out=x3, in0=x3, in1=_bc(gb[i].t[:], [128, 8, 64], 1), op=ALU.mult), r=[x.k, gb[i].k], w=[x.k])
                        x4 = x.t[:].rearrange("p (h t d) -> p h t d", h=8, t=2)
                        t1_, t2_ = x4[:, :, 0, :], x4[:, :, 1, :]
                        M.dve(lambda e: e.tensor_tensor(out=ta.t[:], in0=t1_, in1=cosb, op=ALU.mult), r=[x.k, cs.k], w=[ta.k])
                        M.dve(lambda e: e.tensor_tensor(out=tb.t[:], in0=t2_, in1=sinb, op=ALU.mult), r=[x.k, cs.k], w=[tb.k])
                        M.dve(lambda e: e.tensor_tensor(out=qr.t[:, :, 0, :], in0=ta.t[:], in1=tb.t[:], op=ALU.subtract), r=[ta.k, tb.k], w=[qr.k])
                        M.dve(lambda e: e.tensor_tensor(out=tc.t[:], in0=t2_, in1=cosb, op=ALU.mult), r=[x.k, cs.k], w=[tc.k])
                        M.dve(lambda e: e.tensor_tensor(out=td.t[:], in0=t1_, in1=sinb, op=ALU.mult), r=[x.k, cs.k], w=[td.k])
                        M.dve(lambda e: e.tensor_tensor(out=qr.t[:, :, 1, :], in0=tc.t[:], in1=td.t[:], op=ALU.add), r=[tc.k, td.k], w=[qr.k])
                        p_ = pT[si_]
                        qf = qr.t[:].rearrange("p h t d -> p (h t d)")
                        for hp in range(4):
                            M.pe(lambda e: e.transpose(p_.t[:, hp, :], qf[:, hp * 128:(hp + 1) * 128], self.idb.t[:]), r=[qr.k, self.idb.k], w=[p_.k])
                        M.act(lambda e: e.copy(out=dstT.t[:, :, rows], in_=p_.t[:]), r=[p_.k], w=[dstT.k])
                M.barrier()
            NB = 4
            E = [self.sb(es, f"E{i}", [128, 7, 128], BF16) for i in range(NB)]
            TM = [self.sb(es, f"TM{i}", [128, 5, 64]) for i in range(NB)]
            ost = [self.sb(es, f"ost{i}", [128, 8, 64], BF16) for i in range(3)]
            rec = [self.sb(es, f"rec{i}", [128, 1]) for i in range(NB)]
            pS = [self.ps(es, f"pS{i}", [128, 512]) for i in range(NB)]
            pO = [self.ps(es, f"pO{i}", [128, 65]) for i in range(NB)]
            units = []
            for qt_ in range(2):
                for h in range(8):
                    units.append(("c", qt_, h))
            for r in range(32):
                for h in range(8):
                    units.append(("l", r, h))

            def front(u):
                kind, r, h = units[u]
                hp, po = h // 2, (h % 2) * 64
                ps_, e_ = pS[u % NB], E[u % NB]
                if kind == "c":
                    pv = ps_.t[:, 0:256].rearrange("p (m q) -> p m q", m=2)
                    for m in range(2):
                        M.pe(lambda e: e.matmul(pv[:, m, :], lhsT=kT.t[po:po + 64, hp, m * 128:(m + 1) * 128], rhs=qT.t[po:po + 64, hp, r * 128:(r + 1) * 128],
                                                start=True, stop=True), r=[kT.k, qT.k], w=[ps_.k])
                    M.act(lambda e: e.activation(out=e_.t[:, 0:2, :], in_=pv, func=AF.Exp), r=[ps_.k], w=[e_.k])
                    return
                rs = min(max(r - 4, 0), 24)
                odd = rs % 2
                e0 = rs - odd
                nch = 5 if odd else 4
                dre = e0 - r + 7
                q0 = 256 + r * 64
                pv = ps_.t[:, 0:448].rearrange("p (m q) -> p m q", m=7)
                for m in range(nch + 2):
                    k0 = 256 + (e0 + 2 * m) * 64 if m < nch else (m - nch) * 128
                    M.pe(lambda e: e.matmul(pv[:, m, :], lhsT=kT.t[po:po + 64, hp, k0:k0 + 128], rhs=qT.t[po:po + 64, hp, q0:q0 + 64], start=True, stop=True),
                         r=[kT.k, qT.k], w=[ps_.k])
                tm_ = TM[u % NB]
                M.dve(lambda e: e.tensor_tensor(out=tm_.t[:, 0:nch, :], in0=pv[:, 0:nch, :], in1=T2.t[:, h, dre:dre + 2 * nch - 1:2, :], op=ALU.add),
                      r=[ps_.k, T2.k], w=[tm_.k])
                M.act(lambda e: e.activation(out=e_.t[:, 0:nch, 0:64], in_=tm_.t[:, 0:nch, :], func=AF.Exp), r=[tm_.k], w=[e_.k])
                M.act(lambda e: e.activation(out=e_.t[:, nch:nch + 2, 0:64], in_=pv[:, nch:nch + 2, :], func=AF.Exp), r=[ps_.k], w=[e_.k])
                if odd:
                    M.dve(lambda e: e.tensor_scalar(out=e_.t[:, 0, 0:64], in0=e_.t[:, 0, 0:64], scalar1=C[:, C_CM64 + 1:C_CM64 + 2], scalar2=None, op0=ALU.mult),
                          r=[e_.k, cst.k], w=[e_.k])
                    M.dve(lambda e: e.tensor_scalar(out=e_.t[:, 4, 0:64], in0=e_.t[:, 4, 0:64], scalar1=C[:, C_CM64:C_CM64 + 1], scalar2=None, op0=ALU.mult),
                          r=[e_.k, cst.k], w=[e_.k])

            def back(u):
                kind, r, h = units[u]
                e_, pO_, rc = E[u % NB], pO[u % NB], rec[u % NB]
                if kind == "c":
                    o_ = ost[r % 3]
                    for m in range(2):
                        M.pe(lambda e: e.matmul(pO_.t[:, :], lhsT=e_.t[:, m, :], rhs=Va.t[:, m, h, :], start=(m == 0), stop=(m == 1)), r=[e_.k, Va.k], w=[pO_.k])
                    M.dve(lambda e: e.reciprocal(out=rc.t[:], in_=pO_.t[:, 64:65]), r=[pO_.k], w=[rc.k])
                    M.dve(lambda e: e.tensor_scalar(out=o_.t[:, h, :], in0=pO_.t[:, 0:64], scalar1=rc.t[:, 0:1], scalar2=None, op0=ALU.mult), r=[pO_.k, rc.k], w=[o_.k])
                    if h == 7:
                        M.dma("pool", self.mixs.t[r * 128:(r + 1) * 128, 512:1024], o_.t[:].rearrange("p h d -> p (h d)"), r=[o_.k], w=[self.mixs.k])
                    return
                rs = min(max(r - 4, 0), 24)
                odd = rs % 2
                e0 = rs - odd
                nch = 5 if odd else 4
                q0 = 256 + r * 64
                o_ = ost[r % 3]
                for m in range(nch + 2):
                    kt_ = 2 + (e0 + 2 * m) // 2 if m < nch else (m - nch)
                    M.pe(lambda e: e.matmul(pO_.t[0:64, :], lhsT=e_.t[:, m, 0:64], rhs=Va.t[:, kt_, h, :], start=(m == 0), stop=(m == nch + 1)),
                         r=[e_.k, Va.k], w=[pO_.k])
                M.dve(lambda e: e.reciprocal(out=rc.t[0:64, :], in_=pO_.t[0:64, 64:65]), r=[pO_.k], w=[rc.k])
                M.dve(lambda e: e.tensor_scalar(out=o_.t[0:64, h, :], in0=pO_.t[0:64, 0:64], scalar1=rc.t[0:64, 0:1], scalar2=None, op0=ALU.mult),
                      r=[pO_.k, rc.k], w=[o_.k])
                if h == 7:
                    M.dma("pool", self.mixs.t[q0:q0 + 64, 512:1024], o_.t[0:64, :, :].rearrange("p h d -> p (h d)"), r=[o_.k], w=[self.mixs.k])

            LA = NB - 1
            nu = len(units)
            for u in range(min(LA, nu)):
                front(u)
            for u in range(nu):
                if u + LA < nu:
                    front(u + LA)
                back(u)
            M.barrier()


class ProgAll(ProgFull):
    def stage_mid(self, l, es, xres, h2T, comb, last=False):
        from contextlib import ExitStack
        M, nc, I = self.M, self.nc, self.I
        cst = self.cst
        C = cst.t
        with ExitStack() as e2:
            wout = self.sb(e2, "wout", [128, 8, D], BF16)
            wv = I["w_out"][l].rearrange("(k p) n -> p k n", p=128)
            for k in range(8):
                M.dma("pool", wout.t[:, k, :], wv[:, k, :], w=[wout.k])
            g1b = [self.sb(e2, f"g1b{w}", [128, D]) for w in range(2)]
            for w in range(2):
                M.dma("sp", g1b[w].t[:], self.modrow(l, w, 2).partition_broadcast(128), r=[self.mods.k], w=[g1b[w].k])
            A, S = self.norm_mod_tiles(e2, l, "norm2_gain", 4, 3)
            wr = self.sb(e2, "wr", [128, 8, 36])
            br = self.sb(e2, "br", [1, 36])
            M.dma("sp", wr.t[:], I["w_router"][l].rearrange("(k p) n -> p k n", p=128), w=[wr.k])
            M.dma("sp", br.t[:], I["b_router"][l:l + 1, :], w=[br.k])
            mx = [self.sb(e2, f"mx{i}", [128, D], BF16) for i in range(2)]
            mT = self.sb(e2, "mT", [128, 8, 128], BF16)
            tmp = self.sb(e2, "tmpo", [128, 512])
            sq = self.sb(e2, "sq2", [128, D])
            ss = self.sb(e2, "ss2", [128, 1])
            hf = self.sb(e2, "hf2", [128, D])
            hb = self.sb(e2, "hb2", [128, D], BF16)
            hTf = self.sb(e2, "hTf", [128, 8, 128])
            lg = self.sb(e2, "lg", [128, 36])
            sm = {n: self.sb(e2, n, [128, 8]) for n in ("oh", "esel", "mk1", "e2", "mk2", "ew", "msk")}
            sc = {n: self.sb(e2, n, [128, 1]) for n in ("gm", "ngm", "gs", "m1", "m2", "dm", "w1", "w2")}
            junk = self.sb(e2, "junk", [128, 4])
            msk3 = self.sb(e2, "msk3", [128, 4, 8])
            ptb = self.ps(e2, "ptb", [128, 8, 128], BF16)
            ptf = [self.ps(e2, f"ptf{i}", [128, 4, 128]) for i in range(2)]
            pp = [self.ps(e2, f"ppo{i}", [128, 512]) for i in range(2)]
            plg = self.ps(e2, "plg", [128, 36])
            for j in range(2 if last else 0, NT):
                rows = slice(j * 128, (j + 1) * 128)
                which = 1 if j < 2 else 0
                m_ = mx[j % 2]
                xj = xres.t[:, j, :]
                M.dma("sp", m_.t[:], self.mixs.t[rows, :], r=[self.mixs.k], w=[m_.k])
                M.dma("sp", xj, self.xs.t[rows, :], r=[self.xs.k], w=[xres.k])
                for k in range(8):
                    M.pe(lambda e: e.transpose(ptb.t[:, k, :], m_.t[:, k * 128:(k + 1) * 128], self.idb.t[:]), r=[m_.k, self.idb.k], w=[ptb.k])
                M.act(lambda e: e.copy(out=mT.t[:], in_=ptb.t[:]), r=[ptb.k], w=[mT.k])
                for hf_ in range(2):
                    p_ = pp[hf_]
                    cs_ = slice(hf_ * 512, (hf_ + 1) * 512)
                    for k in range(8):
                        M.pe(lambda e: e.matmul(p_.t[:, :], lhsT=mT.t[:, k, :], rhs=wout.t[:, k, cs_], start=(k == 0), stop=(k == 7)), r=[mT.k, wout.k], w=[p_.k])
                    M.dve(lambda e: e.tensor_tensor(out=tmp.t[:], in0=p_.t[:], in1=g1b[which].t[:, cs_], op=ALU.mult), r=[p_.k, g1b[which].k], w=[tmp.k])
                    M.dve(lambda e: e.tensor_tensor(out=xres.t[:, j, cs_], in0=xres.t[:, j, cs_], in1=tmp.t[:], op=ALU.add), r=[xres.k, tmp.k], w=[xres.k])
                M.act(lambda e: e.activation(out=sq.t[:], in_=xj, func=AF.Square, accum_out=ss.t[:]), r=[xres.k], w=[sq.k, ss.k])
                M.dve(lambda e: e.tensor_scalar(out=ss.t[:], in0=ss.t[:], scalar1=1.0 / D, scalar2=EPS, op0=ALU.mult, op1=ALU.add), r=[ss.k], w=[ss.k])
                M.act(lambda e: e.activation(out=ss.t[:], in_=ss.t[:], func=AF.Sqrt), r=[ss.k], w=[ss.k])
                M.dve(lambda e: e.reciprocal(out=ss.t[:], in_=ss.t[:]), r=[ss.k], w=[ss.k])
                M.dve(lambda e: e.scalar_tensor_tensor(out=hf.t[:], in0=xj, scalar=ss.t[:, 0:1], in1=A[which].t[:], op0=ALU.mult, op1=ALU.mult),
                      r=[xres.k, ss.k, A[which].k], w=[hf.k])
                M.dve(lambda e: e.tensor_tensor(out=hf.t[:], in0=hf.t[:], in1=S[which].t[:], op=ALU.add), r=[hf.k, S[which].k], w=[hf.k])
                M.act(lambda e: e.copy(out=hb.t[:], in_=hf.t[:]), r=[hf.k], w=[hb.k])
                for k in range(8):
                    M.pe(lambda e: e.transpose(ptb.t[:, k, :], hb.t[:, k * 128:(k + 1) * 128], self.idb.t[:]), r=[hb.k, self.idb.k], w=[ptb.k])
                M.act(lambda e: e.copy(out=h2T.t[:, :, rows], in_=ptb.t[:]), r=[ptb.k], w=[h2T.k])
                for q4 in range(2):
                    for k in range(4):
                        kk_ = q4 * 4 + k
                        M.pe(lambda e: e.transpose(ptf[q4].t[:, k, :], hf.t[:, kk_ * 128:(kk_ + 1) * 128], C[:, C_ID:C_ID + 128]), r=[hf.k, cst.k], w=[ptf[q4].k])
                    M.act(lambda e: e.copy(out=hTf.t[:, q4 * 4:(q4 + 1) * 4, :], in_=ptf[q4].t[:]), r=[ptf[q4].k], w=[hTf.k])
                for k in range(8):
                    M.pe(lambda e: e.matmul(plg.t[:, :], lhsT=hTf.t[:, k, :], rhs=wr.t[:, k, :], start=(k == 0), stop=False), r=[hTf.k, wr.k], w=[plg.k])
                M.pe(lambda e: e.matmul(plg.t[:, :], lhsT=self.ones.t[0:1, 0:128], rhs=br.t[0:1, :], start=False, stop=True), r=[self.ones.k, br.k], w=[plg.k])
                M.act(lambda e: e.copy(out=lg.t[:], in_=plg.t[:]), r=[plg.k], w=[lg.k])
                G = lg.t[:, 0:4]
                E3 = lg.t[:, 4:36].rearrange("p (g e) -> p g e", g=4)
                oh, esel, mk1, e2_, mk2, ew = (sm[n] for n in ("oh", "esel", "mk1", "e2", "mk2", "ew"))
                gm, ngm, gs, m1, m2, dm, w1, w2 = (sc[n] for n in ("gm", "ngm", "gs", "m1", "m2", "dm", "w1", "w2"))
                M.dve(lambda e: e.tensor_reduce(out=gm.t[:], in_=G, axis=AX.X, op=ALU.max), r=[lg.k], w=[gm.k])
                M.dve(lambda e: e.tensor_scalar(out=oh.t[:, 0:4], in0=G, scalar1=gm.t[:, 0:1], scalar2=None, op0=ALU.is_equal), r=[lg.k, gm.k], w=[oh.k])
                M.dve(lambda e: e.tensor_scalar(out=ngm.t[:], in0=gm.t[:], scalar1=-1.0, scalar2=None, op0=ALU.mult), r=[gm.k], w=[ngm.k])
                M.act(lambda e: e.activation(out=junk.t[:], in_=G, func=AF.Exp, bias=ngm.t[:, 0:1], accum_out=gs.t[:]), r=[lg.k, ngm.k], w=[junk.k, gs.k])
                M.dve(lambda e: e.reciprocal(out=gs.t[:], in_=gs.t[:]), r=[gs.k], w=[gs.k])
                M.dve(lambda e: e.tensor_tensor(out=msk3.t[:], in0=E3, in1=_bc(oh.t[:, 0:4], [128, 4, 8], 2), op=ALU.mult), r=[lg.k, oh.k], w=[msk3.k])
                M.dve(lambda e: e.tensor_reduce(out=esel.t[:], in_=msk3.t[:].rearrange("p g e -> p e g"), axis=AX.X, op=ALU.add), r=[msk3.k], w=[esel.k])
                M.dve(lambda e: e.tensor_reduce(out=m1.t[:], in_=esel.t[:], axis=AX.X, op=ALU.max), r=[esel.k], w=[m1.k])
                M.dve(lambda e: e.tensor_scalar(out=mk1.t[:], in0=esel.t[:], scalar1=m1.t[:, 0:1], scalar2=None, op0=ALU.is_equal), r=[esel.k, m1.k], w=[mk1.k])
                M.dve(lambda e: e.scalar_tensor_tensor(out=e2_.t[:], in0=mk1.t[:], scalar=-1e30, in1=esel.t[:], op0=ALU.mult, op1=ALU.add), r=[mk1.k, esel.k], w=[e2_.k])
                M.dve(lambda e: e.tensor_reduce(out=m2.t[:], in_=e2_.t[:], axis=AX.X, op=ALU.max), r=[e2_.k], w=[m2.k])
                M.dve(lambda e: e.tensor_scalar(out=mk2.t[:], in0=e2_.t[:], scalar1=m2.t[:, 0:1], scalar2=None, op0=ALU.is_equal), r=[e2_.k, m2.k], w=[mk2.k])
                M.dve(lambda e: e.tensor_tensor(out=dm.t[:], in0=m2.t[:], in1=m1.t[:], op=ALU.subtract), r=[m1.k, m2.k], w=[dm.k])
                M.act(lambda e: e.activation(out=dm.t[:], in_=dm.t[:], func=AF.Exp), r=[dm.k], w=[dm.k])
                M.dve(lambda e: e.tensor_scalar(out=w1.t[:], in0=dm.t[:], scalar1=1.0, scalar2=None, op0=ALU.add), r=[dm.k], w=[w1.k])
                M.dve(lambda e: e.reciprocal(out=w1.t[:], in_=w1.t[:]), r=[w1.k], w=[w1.k])
                M.dve(lambda e: e.tensor_tensor(out=w1.t[:], in0=w1.t[:], in1=gs.t[:], op=ALU.mult), r=[w1.k, gs.k], w=[w1.k])
                M.dve(lambda e: e.tensor_tensor(out=w2.t[:], in0=w1.t[:], in1=dm.t[:], op=ALU.mult), r=[w1.k, dm.k], w=[w2.k])
                M.dve(lambda e: e.tensor_scalar(out=ew.t[:], in0=mk1.t[:], scalar1=w1.t[:, 0:1], scalar2=None, op0=ALU.mult), r=[mk1.k, w1.k], w=[ew.k])
                M.dve(lambda e: e.scalar_tensor_tensor(out=ew.t[:], in0=mk2.t[:], scalar=w2.t[:, 0:1], in1=ew.t[:], op0=ALU.mult, op1=ALU.add), r=[mk2.k, w2.k, ew.k], w=[ew.k])
                for g in range(4):
                    M.dve(lambda e: e.tensor_scalar(out=comb.t[:, j, g * 8:(g + 1) * 8], in0=ew.t[:], scalar1=oh.t[:, g:g + 1], scalar2=None, op0=ALU.mult),
                          r=[ew.k, oh.k], w=[comb.k])
            M.barrier()

    def stage_moe(self, l, xres, h2T, comb, last):
        from contextlib import ExitStack
        M, nc, I = self.M, self.nc, self.I
        with ExitStack() as e2:
            g2b = [self.sb(e2, f"g2b{w}", [128, D]) for w in range(2)]
            for w in range(2):
                M.dma("sp", g2b[w].t[:], self.modrow(l, w, 5).partition_broadcast(128), r=[self.mods.k], w=[g2b[w].k])
            Wg = [self.sb(e2, f"Wg{i}", [128, 8, 512], BF16) for i in range(2)]
            Wu = [self.sb(e2, f"Wu{i}", [128, 8, 512], BF16) for i in range(2)]
            Wd = [self.sb(e2, f"Wd{i}", [128, 4, D], BF16) for i in range(2)]
            hid = [self.sb(e2, f"hid{i}", [128, 4, 512], BF16) for i in range(2)]
            sg = [self.sb(e2, f"sg{i}", [128, 512]) for i in range(2)]
            tmp = [self.sb(e2, f"tmpm{i}", [128, 512]) for i in range(2)]
            pG = [self.ps(e2, f"pG{i}", [128, 512]) for i in range(2)]
            pU = [self.ps(e2, f"pU{i}", [128, 512]) for i in range(2)]
            pD = [self.ps(e2, f"pD{i}", [128, 512]) for i in range(2)]
            cnt = 0
            for ge in range(32):
                b = ge % 2
                gv = I["w_exp_gate"][l, ge].rearrange("(k p) f -> p k f", p=128)
                uv = I["w_exp_up"][l, ge].rearrange("(k p) f -> p k f", p=128)
                dv = I["w_exp_down"][l, ge].rearrange("(c p) d -> p c d", p=128)
                if not (getattr(self, "moe_nodma", False) and ge >= 2):
                    for k in range(0, 8, 2):
                        M.dma("pool", Wg[b].t[:, k:k + 2, :], gv[:, k:k + 2, :], w=[Wg[b].k])
                        M.dma("pool", Wu[b].t[:, k:k + 2, :], uv[:, k:k + 2, :], w=[Wu[b].k])
                    for c in range(4):
                        M.dma("pool", Wd[b].t[:, c, :], dv[:, c, :], w=[Wd[b].k])
                for t0 in range(256 if last else 0, TALL, 512):
                    t1 = min(TALL, t0 + 512)
                    n = t1 - t0
                    hd = hid[(t0 // 512) % 2]
                    for fc in range(4):
                        cnt += 1
                        g_, u_, s_ = pG[cnt % 2], pU[cnt % 2], sg[cnt % 2]
                        for k in range(8):
                            M.pe(lambda e: e.matmul(g_.t[:, 0:n], lhsT=Wg[b].t[:, k, fc * 128:(fc + 1) * 128], rhs=h2T.t[:, k, t0:t1], start=(k == 0), stop=(k == 7)),
                                 r=[Wg[b].k, h2T.k], w=[g_.k])
                        for k in range(8):
                            M.pe(lambda e: e.matmul(u_.t[:, 0:n], lhsT=Wu[b].t[:, k, fc * 128:(fc + 1) * 128], rhs=h2T.t[:, k, t0:t1], start=(k == 0), stop=(k == 7)),
                                 r=[Wu[b].k, h2T.k], w=[u_.k])
                        M.act(lambda e: e.activation(out=s_.t[:, 0:n], in_=g_.t[:, 0:n], func=AF.Silu), r=[g_.k], w=[s_.k])
                        M.dve(lambda e: e.tensor_tensor(out=hd.t[:, fc, 0:n], in0=s_.t[:, 0:n], in1=u_.t[:, 0:n], op=ALU.mult), r=[s_.k, u_.k], w=[hd.k])
                    for jt in range(n // 128):
                        j = t0 // 128 + jt
                        which = 1 if j < 2 else 0
                        for hf_ in range(2):
                            cnt += 1
                            d_, tm = pD[cnt % 2], tmp[cnt % 2]
                            cs_ = slice(hf_ * 512, (hf_ + 1) * 512)
                            for fc in range(4):
                                M.pe(lambda e: e.matmul(d_.t[:, :], lhsT=hd.t[:, fc, jt * 128:(jt + 1) * 128], rhs=Wd[b].t[:, fc, cs_], start=(fc == 0), stop=(fc == 3)),
                                     r=[hd.k, Wd[b].k], w=[d_.k])
                            M.dve(lambda e: e.tensor_tensor(out=tm.t[:], in0=d_.t[:], in1=g2b[which].t[:, cs_], op=ALU.mult), r=[d_.k, g2b[which].k], w=[tm.k])
                            M.dve(lambda e: e.scalar_tensor_tensor(out=xres.t[:, j, cs_], in0=tm.t[:], scalar=comb.t[:, j, ge:ge + 1], in1=xres.t[:, j, cs_],
                                                                   op0=ALU.mult, op1=ALU.add), r=[tm.k, comb.k, xres.k], w=[xres.k])
            for j in range(NT):
                rows = slice(j * 128, (j + 1) * 128)
                if last:
                    if j >= 2:
                        M.dma("sp", self.out[(j - 2) * 128:(j - 1) * 128, :], xres.t[:, j, :], r=[xres.k], w=[self.xs.k])
                else:
                    M.dma("sp", self.xs.t[rows, :], xres.t[:, j, :], r=[xres.k], w=[self.xs.k])
            M.barrier()

    def build_all(self):
        from contextlib import ExitStack
        with ExitStack() as es:
            self.setup(es)
            for l in range(self.NL):
                self.stage_mods(l)
                with ExitStack() as e1:
                    lrT = self.sb(e1, "lrT", [128, 3, TALL])
                    self.stage_proj(l, lrT)
                    self.stage_hgrn(l)
                    self.stage_rwkv(l, lrT)
                    self.M.barrier()
                self.stage_na(l)
                with ExitStack() as e1:
                    xres = self.sb(e1, "xres", [128, NT, D])
                    h2T = self.sb(e1, "h2T", [128, 8, TALL], BF16)
                    comb = self.sb(e1, "comb", [128, NT, 32])
                    self.stage_mid(l, e1, xres, h2T, comb, last=(l == self.NL - 1))
                    self.stage_moe(l, xres, h2T, comb, last=(l == self.NL - 1))
            self.M.barrier()


def run_streams(gens):
    active = list(gens)
    while active:
        for g in list(active):
            try:
                next(g)
            except StopIteration:
                active.remove(g)


class ProgV2(ProgAll):
    def stage_rwkv(self, l, lrT):
        from contextlib import ExitStack
        M, nc, I = self.M, self.nc, self.I
        cst = self.cst
        C = cst.t
        NTL = 36
        if not hasattr(self, "yscr"):
            self.yscr = B_(nc.dram_tensor("yscr", [2, TALL, 256], F32, kind=("ExternalOutput" if self.dbg else "Internal")).ap(), "yscr")
        yscr = self.yscr
        with ExitStack() as es:
            wup = self.sb(es, "wup", [128, 256])
            aup = self.sb(es, "aup", [128, 256])
            gup = self.sb(es, "gup", [128, 256])
            brow = self.sb(es, "browr", [1, 4, 256])
            M.dma("sp", wup.t[:], I["rwkv_w_up"][l].rearrange("d r n -> (d r) n"), w=[wup.k])
            M.dma("sp", aup.t[:], I["rwkv_a_up"][l].rearrange("d r n -> (d r) n"), w=[aup.k])
            M.dma("sp", gup.t[:], I["rwkv_g_up"][l], w=[gup.k])
            M.dma("sp", brow.t[0:1, 0:2, :], I["rwkv_w0"][l:l + 1, :, :], w=[brow.k])
            M.dma("sp", brow.t[0:1, 2:4, :], I["rwkv_a0"][l:l + 1, :, :], w=[brow.k])
            bt = {}
            for nm in ("rwkv_kk_scale", "rwkv_k_a", "rwkv_r_k", "rwkv_ln_gain", "rwkv_ln_bias"):
                bt[nm] = self.sb(es, nm, [64, 256])
                M.dma("sp", bt[nm].t[:], I[nm][l:l + 1, :].partition_broadcast(64), w=[bt[nm].k])
            kab = bt["rwkv_k_a"]
            omka = self.sb(es, "omka", [64, 256])
            M.dve(lambda e: e.tensor_scalar(out=omka.t[:], in0=kab.t[:], scalar1=-1.0, scalar2=1.0, op0=ALU.mult, op1=ALU.add), r=[kab.k], w=[omka.k])
            pgr = [self.ps(es, f"pgr{i}", [64, 512]) for i in range(6)]
            pYd_ = [self.ps(es, f"pYd{i}", [64, 512]) for i in range(2)]
            pYd = []
            for p__ in pYd_:
                bb = B_(p__.t[:, 0:256].rearrange("p (h d) -> p h d", h=4), "pYv")
                bb.k = p__.k
                pYd.append(bb)
            gi = [0]

            busy = [False] * 6

            def alloc():
                for i in range(6):
                    kx = (gi[0] + 1 + i) % 6
                    if not busy[kx]:
                        busy[kx] = True
                        gi[0] = kx
                        return pgr[kx]
                return None

            def rel(p):
                busy[pgr.index(p)] = False

            def galloc():
                while True:
                    p = alloc()
                    if p is not None:
                        return p
                    yield

            def v4(p_):
                return p_.t[:, 0:256].rearrange("p (h d) -> p h d", h=4)

            with ExitStack() as e1:
                KT, NBU = 3, 6
                TS = []
                for i in range(KT):
                    t = {}
                    for nm in ("r", "k", "lw", "kk", "sq", "t1", "kd", "ak", "en", "er", "rt", "kkt", "akt", "kdt"):
                        t[nm] = self.sb(e1, f"{nm}{i}", [64, 256])
                    for nm in ("sz", "ecx"):
                        t[nm] = self.sb(e1, f"{nm}{i}", [64, 512])
                    t["tw"] = self.sb(e1, f"tw{i}", [128, 64])
                    t["s4"] = self.sb(e1, f"s4{i}", [64, 4])
                    for nm in ("NTm", "P0", "P1"):
                        t[nm] = self.sb(e1, f"{nm}{i}", [64, 4, 64])
                    for nm in ("aktT", "kdtT"):
                        t[nm] = self.sb(e1, f"{nm}{i}", [64, 4, 64], BF16)
                    for nm in ("Xa0", "Xa1", "XT0", "XT1", "NTb", "Pb0", "Pb1"):
                        t[nm] = self.sb(e1, f"{nm}{i}", [64, 4, 64], BF16)
                    TS.append(t)
                BS = []
                for i in range(NBU):
                    b = {}
                    for nm in ("v", "akh", "kdh"):
                        b[nm] = self.sb(e1, f"b{nm}{i}", [64, 256], BF16)
                    b["X2"] = self.sb(e1, f"bX2{i}", [64, 4, 2, 64], BF16)
                    for nm in ("AkT", "BakT", "BkT", "PT"):
                        b[nm] = self.sb(e1, f"b{nm}{i}", [64, 4, 64], BF16)
                    b["ecT"] = self.sb(e1, f"becT{i}", [64, 4, 64])
                    BS.append(b)
                Sr = [[self.sb(e1, f"RS{d}{i}", [64, 4, 64]) for i in range(3)] for d in range(2)]
                Wn = [self.sb(e1, f"Wn{d}", [64, 4, 64], BF16) for d in range(2)]
                U = [self.sb(e1, f"U{d}", [64, 4, 64], BF16) for d in range(2)]
                Sbr = [[self.sb(e1, f"RSb{d}{i}", [64, 4, 64], BF16) for i in range(3)] for d in range(2)]
                yo = [[self.sb(e1, f"yo{d}{i}", [64, 256]) for i in range(2)] for d in range(2)]
                orders = [list(range(NTL)), [3, 2, 1, 0] + list(range(NTL - 1, 3, -1))]
                ready = [dict(), dict()]
                free_T = list(range(KT))
                free_B = list(range(NBU))

                def prep(d, idx, ti, bi):
                    T, Bn = TS[ti], BS[bi]
                    j = orders[d][idx]
                    CUM, SCUM, REM = [(C_U64, C_SU64, C_SL64), (C_L64, C_SL64, C_SU64)][d]
                    MSTR, MINC, MSTRT = [(C_SU64, C_U64, C_SL64), (C_SL64, C_L64, C_SU64)][d]
                    P0 = 64 * d
                    rows = slice(j * 64, (j + 1) * 64)
                    r, k, v = T["r"], T["k"], Bn["v"]
                    tw, sz, lw, kk, sq, s4, t1, kd, ak, ecx, en, er = (T[n] for n in ("tw", "sz", "lw", "kk", "sq", "s4", "t1", "kd", "ak", "ecx", "en", "er"))
                    M.dma("sp", r.t[:], self.ptm.t[rows, 1280:1536], r=[self.ptm.k], w=[r.k])
                    M.dma("sp", k.t[:], self.ptm.t[rows, 1536:1792], r=[self.ptm.k], w=[k.k])
                    M.dma("pool", v.t[:], self.ptm.t[rows, 1792:2048], r=[self.ptm.k], w=[v.k])
                    M.act(lambda e: e.activation(out=tw.t[P0:P0 + 64, :], in_=lrT.t[P0:P0 + 64, 0, rows], func=AF.Tanh), r=[lrT.k], w=[tw.k])
                    yield
                    pz = yield from galloc()
                    M.pe(lambda e: e.matmul(pz.t[:, 0:256], lhsT=tw.t[P0:P0 + 64, :], rhs=wup.t[P0:P0 + 64, :], start=True, stop=False, skip_group_check=True),
                         r=[tw.k, wup.k], w=[pz.k])
                    M.pe(lambda e: e.matmul(pz.t[:, 0:256], lhsT=self.ones.t[0:1, 0:64], rhs=brow.t[0:1, d, :], start=False, stop=False, skip_group_check=True),
                         r=[self.ones.k, brow.k], w=[pz.k])
                    M.pe(lambda e: e.matmul(pz.t[:, 256:512], lhsT=lrT.t[P0:P0 + 64, 1, rows], rhs=aup.t[P0:P0 + 64, :], start=False, stop=False, skip_group_check=True),
                         r=[lrT.k, aup.k], w=[pz.k])
                    M.pe(lambda e: e.matmul(pz.t[:, 256:512], lhsT=self.ones.t[0:1, 0:64], rhs=brow.t[0:1, 2 + d, :], start=False, stop=True, skip_group_check=True),
                         r=[self.ones.k, brow.k], w=[pz.k])
                    yield
                    M.act(lambda e: e.activation(out=sz.t[:], in_=pz.t[:], func=AF.Sigmoid), r=[pz.k], w=[sz.k])
                    rel(pz)
                    M.dve(lambda e: e.tensor_tensor(out=kk.t[:], in0=k.t[:], in1=bt["rwkv_kk_scale"].t[:], op=ALU.mult), r=[k.k, bt["rwkv_kk_scale"].k], w=[kk.k])
                    yield
                    M.dve(lambda e: e.tensor_scalar(out=lw.t[:], in0=sz.t[:, 0:256], scalar1=WSCALE, scalar2=None, op0=ALU.mult), r=[sz.k], w=[lw.k])
                    M.act(lambda e: e.activation(out=sq.t[:], in_=kk.t[:], func=AF.Square), r=[kk.k], w=[sq.k])
                    yield
                    pc0 = yield from galloc()
                    pc1 = yield from galloc()
                    M.pe(lambda e: e.matmul(pc0.t[:, 0:256], lhsT=C[0:64, CUM:CUM + 64], rhs=lw.t[:], start=True, stop=True), r=[cst.k, lw.k], w=[pc0.k])
                    M.pe(lambda e: e.matmul(pc0.t[:, 256:512], lhsT=C[0:64, SCUM:SCUM + 64], rhs=lw.t[:], start=True, stop=True), r=[cst.k, lw.k], w=[pc0.k])
                    M.pe(lambda e: e.matmul(pc1.t[:, 0:256], lhsT=C[0:64, REM:REM + 64], rhs=lw.t[:], start=True, stop=True), r=[cst.k, lw.k], w=[pc1.k])
                    M.dve(lambda e: e.tensor_reduce(out=s4.t[:], in_=sq.t[:].rearrange("p (h d) -> p h d", h=4), axis=AX.X, op=ALU.add), r=[sq.k], w=[s4.k])
                    yield
                    M.act(lambda e: e.activation(out=s4.t[:], in_=s4.t[:], func=AF.Sqrt), r=[s4.k], w=[s4.k])
                    M.act(lambda e: e.activation(out=ecx.t[:], in_=pc0.t[:], func=AF.Exp), r=[pc0.k], w=[ecx.k])
                    M.act(lambda e: e.activation(out=en.t[:], in_=pc0.t[:, 0:256], func=AF.Exp, scale=-1.0), r=[pc0.k], w=[en.k])
                    M.act(lambda e: e.activation(out=er.t[:], in_=pc1.t[:, 0:256], func=AF.Exp), r=[pc1.k], w=[er.k])
                    rel(pc0)
                    rel(pc1)
                    a_ = sz.t[:, 256:512]
                    M.dve(lambda e: e.tensor_tensor(out=t1.t[:], in0=a_, in1=kab.t[:], op=ALU.mult), r=[sz.k, kab.k], w=[t1.k])
                    M.dve(lambda e: e.tensor_tensor(out=t1.t[:], in0=t1.t[:], in1=omka.t[:], op=ALU.add), r=[t1.k, omka.k], w=[t1.k])
                    M.dve(lambda e: e.tensor_tensor(out=kd.t[:], in0=k.t[:], in1=t1.t[:], op=ALU.mult), r=[k.k, t1.k], w=[kd.k])
                    yield
                    M.dve(lambda e: e.tensor_scalar(out=s4.t[:], in0=s4.t[:], scalar1=1e-12, scalar2=None, op0=ALU.max), r=[s4.k], w=[s4.k])
                    M.dve(lambda e: e.reciprocal(out=s4.t[:], in_=s4.t[:]), r=[s4.k], w=[s4.k])
                    M.dve(lambda e: e.tensor_tensor(out=kk.t[:].rearrange("p (h d) -> p h d", h=4), in0=kk.t[:].rearrange("p (h d) -> p h d", h=4),
                                                    in1=_bc(s4.t[:], [64, 4, 64], 2), op=ALU.mult), r=[kk.k, s4.k], w=[kk.k])
                    M.dve(lambda e: e.tensor_tensor(out=ak.t[:], in0=a_, in1=kk.t[:], op=ALU.mult), r=[sz.k, kk.k], w=[ak.k])
                    rt, kkt, akt, kdt = T["rt"], T["kkt"], T["akt"], T["kdt"]
                    akh, kdh = Bn["akh"], Bn["kdh"]
                    for pi_, (o_, a0, a1, rr) in enumerate(((rt, r.t[:], ecx.t[:, 0:256], [r.k, ecx.k]), (kkt, kk.t[:], ecx.t[:, 256:512], [kk.k, ecx.k]),
                                             (akt, ak.t[:], en.t[:], [ak.k, en.k]), (kdt, kd.t[:], en.t[:], [kd.k, en.k]),
                                             (akh, ak.t[:], er.t[:], [ak.k, er.k]), (kdh, kd.t[:], er.t[:], [kd.k, er.k]))):
                        M.op("dve", lambda e: e.tensor_tensor(out=o_.t[:], in0=a0, in1=a1, op=ALU.mult), r=rr, w=[o_.k])
                    yield
                    X2, aktT, kdtT, ecT = Bn["X2"], T["aktT"], T["kdtT"], Bn["ecT"]
                    for gidx, (src, dst_ap, dstb) in enumerate(((kkt, X2.t[:, :, 0, :], X2), (rt, X2.t[:, :, 1, :], X2), (akt, aktT.t[:], aktT),
                                                                (kdt, kdtT.t[:], kdtT), (ecx, ecT.t[:], ecT))):
                        p_ = yield from galloc()
                        for h in range(4):
                            M.pe(lambda e: e.transpose(v4(p_)[:, h, :], src.t[:, h * 64:(h + 1) * 64], C[0:64, C_ID:C_ID + 64]), r=[src.k, cst.k], w=[p_.k])
                        M.act(lambda e: e.copy(out=dst_ap, in_=v4(p_)), r=[p_.k], w=[dstb.k])
                        rel(p_)
                        if gidx % 2 == 1:
                            yield
                    yield
                    NTm = T["NTm"]
                    for gidx, (lhs, ridx, msk, dst) in enumerate(((aktT, 0, MSTR, NTm), (aktT, 1, MINC, Bn["BakT"]), (kdtT, 0, MSTR, Bn["AkT"]), (kdtT, 1, MINC, Bn["BkT"]))):
                        p_ = yield from galloc()
                        for h in range(4):
                            M.pe(lambda e: e.matmul(v4(p_)[:, h, :], lhsT=lhs.t[:, h, :], rhs=X2.t[:, h, ridx, :], start=True, stop=True), r=[lhs.k, X2.k], w=[p_.k])
                        M.dve(lambda e: e.tensor_tensor(out=dst.t[:], in0=v4(p_), in1=_bc(C[0:64, msk:msk + 64], [64, 4, 64], 1), op=ALU.mult),
                              r=[p_.k, cst.k], w=[dst.k])
                        if dst is NTm:
                            M.dve(lambda e: e.tensor_tensor(out=T["NTb"].t[:], in0=v4(p_), in1=_bc(C[0:64, msk:msk + 64], [64, 4, 64], 1), op=ALU.mult),
                                  r=[p_.k, cst.k], w=[T["NTb"].k])
                        rel(p_)
                        if gidx % 2 == 1:
                            yield
                    p_ = yield from galloc()
                    for h in range(4):
                        M.pe(lambda e: e.matmul(v4(p_)[:, h, :], lhsT=X2.t[:, h, 0, :], rhs=aktT.t[:, h, :], start=True, stop=True), r=[aktT.k, X2.k], w=[p_.k])
                    Xs = [T["Xa0"], T["Xa1"]]
                    XTs = [T["XT0"], T["XT1"]]
                    Ps = [T["P0"], T["P1"]]
                    Pbs = [T["Pb0"], T["Pb1"]]
                    X, XT, P_, Pb = Xs[0], T["NTb"], Ps[0], Pbs[0]
                    M.dve(lambda e: e.tensor_tensor(out=X.t[:], in0=v4(p_), in1=_bc(C[0:64, MSTRT:MSTRT + 64], [64, 4, 64], 1), op=ALU.mult),
                          r=[p_.k, cst.k], w=[X.k])
                    rel(p_)
                    M.dve(lambda e: e.scalar_tensor_tensor(out=P_.t[:], in0=NTm.t[:], scalar=-1.0, in1=_bc(C[0:64, C_ID:C_ID + 64], [64, 4, 64], 1), op0=ALU.mult, op1=ALU.add),
                          r=[NTm.k, cst.k], w=[P_.k])
                    M.dve(lambda e: e.scalar_tensor_tensor(out=Pb.t[:], in0=NTm.t[:], scalar=-1.0, in1=_bc(C[0:64, C_ID:C_ID + 64], [64, 4, 64], 1), op0=ALU.mult, op1=ALU.add),
                          r=[NTm.k, cst.k], w=[Pb.k])
                    yield
                    for i in range(1, 6):
                        Xn, XTn = Xs[i % 2], XTs[i % 2]
                        pX = yield from galloc()
                        for h in range(4):
                            M.pe(lambda e: e.matmul(v4(pX)[:, h, :], lhsT=XT.t[:, h, :], rhs=X.t[:, h, :], start=True, stop=True), r=[XT.k, X.k], w=[pX.k])
                        if i < 5:
                            pXT = yield from galloc()
                            for h in range(4):
                                M.pe(lambda e: e.matmul(v4(pXT)[:, h, :], lhsT=X.t[:, h, :], rhs=XT.t[:, h, :], start=True, stop=True), r=[XT.k, X.k], w=[pXT.k])
                        yield
                        M.act(lambda e: e.copy(out=Xn.t[:], in_=v4(pX)), r=[pX.k], w=[Xn.k])
                        rel(pX)
                        if i < 5:
                            M.act(lambda e: e.copy(out=XTn.t[:], in_=v4(pXT)), r=[pXT.k], w=[XTn.k])
                            rel(pXT)
                        yield
                        pP = yield from galloc()
                        for h in range(4):
                            M.pe(lambda e: e.matmul(v4(pP)[:, h, :], lhsT=Xn.t[:, h, :], rhs=Pb.t[:, h, :], start=True, stop=True), r=[Xn.k, Pb.k], w=[pP.k])
                        yield
                        Pn = Ps[i % 2]
                        Pbn = Bn["PT"] if i == 5 else Pbs[i % 2]
                        if i < 5:
                            M.dve(lambda e: e.tensor_tensor(out=Pn.t[:], in0=P_.t[:], in1=v4(pP), op=ALU.add), r=[P_.k, pP.k], w=[Pn.k])
                        M.dve(lambda e: e.tensor_tensor(out=Pbn.t[:], in0=P_.t[:], in1=v4(pP), op=ALU.add), r=[P_.k, pP.k], w=[Pbn.k])
                        rel(pP)
                        X, XT, P_, Pb = Xn, XTn, Pn, Pbn
                        yield
                    free_T.append(ti)
                    ready[d][idx] = bi

                def chain(d):
                    si = 0
                    S = Sr[d][0]
                    Sb = Sbr[d][0]
                    M.dve(lambda e: e.memset(S.t[:], 0.0), w=[S.k])
                    M.dve(lambda e: e.memset(Sb.t[:], 0.0), w=[Sb.k])
                    pY = pYd[d]
                    for idx in range(NTL):
                        while idx not in ready[d]:
                            yield
                        bi = ready[d][idx]
                        Bn = BS[bi]
                        j = orders[d][idx]
                        rows = slice(j * 64, (j + 1) * 64)
                        v, X2, AkT, BakT, BkT, PT_, akh, kdh, ecT = (Bn[n] for n in ("v", "X2", "AkT", "BakT", "BkT", "PT", "akh", "kdh", "ecT"))
                        pW = yield from galloc()
                        for h in range(4):
                            hs = slice(h * 64, (h + 1) * 64)
                            M.pe(lambda e: e.matmul(v4(pW)[:, h, :], lhsT=X2.t[:, h, 0, :], rhs=Sb.t[:, h, :], start=(h == 0), stop=False, skip_group_check=True),
                                 r=[X2.k, Sb.k], w=[pW.k])
                            M.pe(lambda e: e.matmul(v4(pW)[:, h, :], lhsT=AkT.t[:, h, :], rhs=v.t[:, hs], start=False, stop=True, skip_group_check=True),
                                 r=[AkT.k, v.k], w=[pW.k])
                        for h in range(4):
                            hs = slice(h * 64, (h + 1) * 64)
                            M.pe(lambda e: e.matmul(pY.t[:, h, :], lhsT=X2.t[:, h, 1, :], rhs=Sb.t[:, h, :], start=(h == 0), stop=False, skip_group_check=True),
                                 r=[X2.k, Sb.k], w=[pY.k])
                            M.pe(lambda e: e.matmul(pY.t[:, h, :], lhsT=BkT.t[:, h, :], rhs=v.t[:, hs], start=False, stop=False, skip_group_check=True),
                                 r=[BkT.k, v.k], w=[pY.k])
                        yield
                        M.act(lambda e: e.mul(out=Wn[d].t[:], in_=v4(pW), mul=-1.0), r=[pW.k], w=[Wn[d].k])
                        rel(pW)
                        yield
                        pU = yield from galloc()
                        for h in range(4):
                            M.pe(lambda e: e.matmul(v4(pU)[:, h, :], lhsT=PT_.t[:, h, :], rhs=Wn[d].t[:, h, :], start=True, stop=True), r=[PT_.k, Wn[d].k], w=[pU.k])
                        yield
                        M.act(lambda e: e.copy(out=U[d].t[:], in_=v4(pU)), r=[pU.k], w=[U[d].k])
                        rel(pU)
                        yield
                        pS = yield from galloc()
                        for h in range(4):
                            hs = slice(h * 64, (h + 1) * 64)
                            M.pe(lambda e: e.matmul(v4(pS)[:, h, :], lhsT=akh.t[:, hs], rhs=U[d].t[:, h, :], start=(h == 0), stop=False, skip_group_check=True),
                                 r=[akh.k, U[d].k], w=[pS.k])
                            M.pe(lambda e: e.matmul(v4(pS)[:, h, :], lhsT=kdh.t[:, hs], rhs=v.t[:, hs], start=False, stop=True, skip_group_check=True),
                                 r=[kdh.k, v.k], w=[pS.k])
                        for h in range(4):
                            M.pe(lambda e: e.matmul(pY.t[:, h, :], lhsT=BakT.t[:, h, :], rhs=U[d].t[:, h, :], start=False, stop=True, skip_group_check=True),
                                 r=[BakT.k, U[d].k], w=[pY.k])
                        si = (si + 1) % 3
                        Sn = Sr[d][si]
                        col = 63 if d == 0 else 0
                        M.dve(lambda e: e.tensor_tensor(out=Sn.t[:], in0=S.t[:], in1=ecT.t[:, :, col:col + 1].to_broadcast([64, 4, 64]), op=ALU.mult),
                              r=[S.k, ecT.k], w=[Sn.k])
                        yield
                        Sbn = Sbr[d][si]
                        M.dve(lambda e: e.tensor_tensor(out=Sbn.t[:], in0=Sn.t[:], in1=v4(pS), op=ALU.add), r=[Sn.k, pS.k], w=[Sbn.k])
                        M.dve(lambda e: e.tensor_tensor(out=Sn.t[:], in0=Sn.t[:], in1=v4(pS), op=ALU.add), r=[Sn.k, pS.k], w=[Sn.k])
                        rel(pS)
                        S, Sb = Sn, Sbn
                        y_ = yo[d][idx % 2]
                        M.act(lambda e: e.copy(out=y_.t[:].rearrange("p (h d) -> p h d", h=4), in_=pY.t[:]), r=[pY.k], w=[y_.k])
                        yield
                        M.dma("pool", yscr.t[d, rows, :], y_.t[:], r=[y_.k], w=[yscr.k])
                        free_B.append(bi)

                tasks = []
                for idx in range(NTL):
                    tasks.append((0, idx))
                    tasks.append((1, idx))
                active = [chain(0), chain(1)]
                while active:
                    while tasks and free_T and free_B:
                        d_, idx_ = tasks.pop(0)
                        active.append(prep(d_, idx_, free_T.pop(0), free_B.pop(0)))
                    for g in list(active):
                        try:
                            next(g)
                        except StopIteration:
                            active.remove(g)
                M.barrier()
            with ExitStack() as e1:
                KR = 5
                RS_ = []
                for i in range(KR):
                    t = {}
                    for nm in ("r", "k", "v", "yf", "yb", "sq", "af", "ab", "bon"):
                        t[nm] = self.sb(e1, f"ro{nm}{i}", [64, 256])
                    t["s4"] = self.sb(e1, f"ros4{i}", [64, 4])
                    t["s5"] = self.sb(e1, f"ros5{i}", [64, 4])
                    t["sgd"] = self.sb(e1, f"rosgd{i}", [128, 64])
                    t["mo"] = self.sb(e1, f"romo{i}", [64, 256], BF16)
                    RS_.append(t)

                def readout(j, T):
                    rows = slice(j * 64, (j + 1) * 64)
                    r, k, v, yf, yb, sq, af, ab, bon, s4, s5, sgd, m_ = (T[n] for n in ("r", "k", "v", "yf", "yb", "sq", "af", "ab", "bon", "s4", "s5", "sgd", "mo"))
                    M.dma("sp", r.t[:], self.ptm.t[rows, 1280:1536], r=[self.ptm.k], w=[r.k])
                    M.dma("sp", k.t[:], self.ptm.t[rows, 1536:1792], r=[self.ptm.k], w=[k.k])
                    M.dma("sp", v.t[:], self.ptm.t[rows, 1792:2048], r=[self.ptm.k], w=[v.k])
                    M.dma("sp", yf.t[:], yscr.t[0, rows, :], r=[yscr.k], w=[yf.k])
                    M.dma("sp", yb.t[:], yscr.t[1, rows, :], r=[yscr.k], w=[yb.k])
                    M.act(lambda e: e.activation(out=sgd.t[:], in_=lrT.t[:, 2, rows], func=AF.Sigmoid), r=[lrT.k], w=[sgd.k])
                    pz = yield from galloc()
                    for d in range(2):
                        P0 = 64 * d
                        M.pe(lambda e: e.matmul(pz.t[:, d * 256:(d + 1) * 256], lhsT=lrT.t[P0:P0 + 64, 1, rows], rhs=aup.t[P0:P0 + 64, :], start=(d == 0), stop=False,
                                                skip_group_check=True), r=[lrT.k, aup.k], w=[pz.k])
                        M.pe(lambda e: e.matmul(pz.t[:, d * 256:(d + 1) * 256], lhsT=self.ones.t[0:1, 0:64], rhs=brow.t[0:1, 2 + d, :], start=False, stop=(d == 1),
                                                skip_group_check=True), r=[self.ones.k, brow.k], w=[pz.k])
                    yield
                    y3 = yf.t[:].rearrange("p (h d) -> p h d", h=4)
                    M.dve(lambda e: e.tensor_tensor(out=yf.t[:], in0=yf.t[:], in1=yb.t[:], op=ALU.add), r=[yf.k, yb.k], w=[yf.k])
                    M.dve(lambda e: e.tensor_reduce(out=s4.t[:], in_=y3, axis=AX.X, op=ALU.add), r=[yf.k], w=[s4.k])
                    M.dve(lambda e: e.tensor_scalar(out=s4.t[:], in0=s4.t[:], scalar1=1.0 / 64, scalar2=None, op0=ALU.mult), r=[s4.k], w=[s4.k])
                    M.dve(lambda e: e.tensor_tensor(out=y3, in0=y3, in1=_bc(s4.t[:], [64, 4, 64], 2), op=ALU.subtract), r=[yf.k, s4.k], w=[yf.k])
                    M.act(lambda e: e.activation(out=af.t[:], in_=pz.t[:, 0:256], func=AF.Sigmoid), r=[pz.k], w=[af.k])
                    M.act(lambda e: e.activation(out=ab.t[:], in_=pz.t[:, 256:512], func=AF.Sigmoid), r=[pz.k], w=[ab.k])
                    rel(pz)
                    yield
                    M.act(lambda e: e.activation(out=sq.t[:], in_=yf.t[:], func=AF.Square), r=[yf.k], w=[sq.k])
                    pg_ = yield from galloc()
                    M.pe(lambda e: e.matmul(pg_.t[:, 0:256], lhsT=sgd.t[:], rhs=gup.t[:], start=True, stop=True), r=[sgd.k, gup.k], w=[pg_.k])
                    M.dve(lambda e: e.tensor_tensor(out=af.t[:], in0=af.t[:], in1=ab.t[:], op=ALU.add), r=[af.k, ab.k], w=[af.k])
                    M.dve(lambda e: e.tensor_scalar(out=af.t[:], in0=af.t[:], scalar1=-2.0, scalar2=None, op0=ALU.add), r=[af.k], w=[af.k])
                    M.dve(lambda e: e.tensor_tensor(out=af.t[:], in0=af.t[:], in1=kab.t[:], op=ALU.mult), r=[af.k, kab.k], w=[af.k])
                    M.dve(lambda e: e.tensor_scalar(out=af.t[:], in0=af.t[:], scalar1=2.0, scalar2=None, op0=ALU.add), r=[af.k], w=[af.k])
                    M.dve(lambda e: e.tensor_tensor(out=af.t[:], in0=af.t[:], in1=k.t[:], op=ALU.mult), r=[af.k, k.k], w=[af.k])
                    M.dve(lambda e: e.tensor_tensor(out=af.t[:], in0=af.t[:], in1=r.t[:], op=ALU.mult), r=[af.k, r.k], w=[af.k])
                    M.dve(lambda e: e.tensor_tensor(out=af.t[:], in0=af.t[:], in1=bt["rwkv_r_k"].t[:], op=ALU.mult), r=[af.k, bt["rwkv_r_k"].k], w=[af.k])
                    M.dve(lambda e: e.tensor_reduce(out=s5.t[:], in_=af.t[:].rearrange("p (h d) -> p h d", h=4), axis=AX.X, op=ALU.add), r=[af.k], w=[s5.k])
                    M.dve(lambda e: e.tensor_tensor(out=bon.t[:].rearrange("p (h d) -> p h d", h=4), in0=v.t[:].rearrange("p (h d) -> p h d", h=4),
                                                    in1=_bc(s5.t[:], [64, 4, 64], 2), op=ALU.mult), r=[v.k, s5.k], w=[bon.k])
                    yield
                    M.dve(lambda e: e.tensor_reduce(out=s4.t[:], in_=sq.t[:].rearrange("p (h d) -> p h d", h=4), axis=AX.X, op=ALU.add), r=[sq.k], w=[s4.k])
                    M.dve(lambda e: e.tensor_scalar(out=s4.t[:], in0=s4.t[:], scalar1=1.0 / 64, scalar2=RWKV_LN_EPS, op0=ALU.mult, op1=ALU.add), r=[s4.k], w=[s4.k])
                    yield
                    M.act(lambda e: e.activation(out=s4.t[:], in_=s4.t[:], func=AF.Sqrt), r=[s4.k], w=[s4.k])
                    yield
                    M.dve(lambda e: e.reciprocal(out=s4.t[:], in_=s4.t[:]), r=[s4.k], w=[s4.k])
                    M.dve(lambda e: e.tensor_tensor(out=y3, in0=y3, in1=_bc(s4.t[:], [64, 4, 64], 2), op=ALU.mult), r=[yf.k, s4.k], w=[yf.k])
                    M.dve(lambda e: e.tensor_tensor(out=yf.t[:], in0=yf.t[:], in1=bt["rwkv_ln_gain"].t[:], op=ALU.mult), r=[yf.k, bt["rwkv_ln_gain"].k], w=[yf.k])
                    M.dve(lambda e: e.tensor_tensor(out=yf.t[:], in0=yf.t[:], in1=bt["rwkv_ln_bias"].t[:], op=ALU.add), r=[yf.k, bt["rwkv_ln_bias"].k], w=[yf.k])
                    M.dve(lambda e: e.tensor_tensor(out=yf.t[:], in0=yf.t[:], in1=bon.t[:], op=ALU.add), r=[yf.k, bon.k], w=[yf.k])
                    M.dve(lambda e: e.tensor_tensor(out=m_.t[:], in0=yf.t[:], in1=pg_.t[:, 0:256], op=ALU.mult), r=[yf.k, pg_.k], w=[m_.k])
                    rel(pg_)
                    yield
                    M.dma("pool", self.mixs.t[rows, 256:512], m_.t[:], r=[m_.k], w=[self.mixs.k])

                todo = list(range(NTL))
                active = []
                freeR = list(range(KR))

                def wrap(j, ri):
                    yield from readout(j, RS_[ri])
                    freeR.append(ri)

                while todo or active:
                    while todo and freeR:
                        active.append(wrap(todo.pop(0), freeR.pop(0)))
                    for g in list(active):
                        try:
                            next(g)
                        except StopIteration:
                            active.remove(g)
                M.barrier()


def kernel(**inputs):
    P = ProgV5(NL=4, dbg=False, moe_in=True)
    P.build_all()
    sh = prep_shared(inputs, NL=4, moe_in=True)
    in_maps = [prep_core(inputs, b, sh) for b in range(8)]
    res = run_bass_kernel_spmd(P.nc, in_maps, core_ids=list(range(8)))
    out = np.stack([np.asarray(r["out"], dtype=np.float32) for r in res.results], 0)
    return out.astype(np.asarray(inputs["x"]).dtype)


class ProgV3(ProgV2):
    def stage_hgrn(self, l):
        from contextlib import ExitStack
        M, nc, I = self.M, self.nc, self.I
        cst = self.cst
        C = cst.t
        with ExitStack() as es:
            lb = [self.sb(es, f"lb{d}", [128, 256]) for d in range(2)]
            oml = [self.sb(es, f"oml{d}", [128, 256]) for d in range(2)]
            for d in range(2):
                M.dma("sp", lb[d].t[:], self.lbs.t[d, l:l + 1, :].partition_broadcast(128), r=[self.lbs.k], w=[lb[d].k])
                M.dve(lambda e: e.tensor_scalar(out=oml[d].t[:], in0=lb[d].t[:], scalar1=-1.0, scalar2=1.0, op0=ALU.mult, op1=ALU.add),
                      r=[lb[d].k], w=[oml[d].k])
            gnb = self.sb(es, "gnb", [128, 256])
            self.bload("sp", gnb, I["hgrn_gn_gain"][l:l + 1, :], 256)
            obuf = [self.sb(es, f"obuf{d}", [128, NT, 256]) for d in range(2)]
            pool = [self.ps(es, f"hp{i}", [128, 512]) for i in range(8)]
            busy = [False] * 8
            gi = [0]

            def alloc():
                for i in range(8):
                    kx = (gi[0] + 1 + i) % 8
                    if not busy[kx]:
                        busy[kx] = True
                        gi[0] = kx
                        return pool[kx]
                return None

            def rel(p):
                busy[pool.index(p)] = False

            def galloc():
                while True:
                    p = alloc()
                    if p is not None:
                        return p
                    yield

            with ExitStack() as e1:
                WS = {}
                for d in range(2):
                    for par in range(2):
                        t = {}
                        for nm in ("q", "f", "v", "kk", "ec", "en", "er", "qt", "kt", "kh"):
                            t[nm] = self.sb(e1, f"h{nm}{d}{par}", [128, 256])
                        t["khm"] = self.sb(e1, f"hkhm{d}{par}", [128, 4, 256], BF16)
                        t["vb"] = self.sb(e1, f"hvb{d}{par}", [128, 256], BF16)
                        for nm in ("qtT", "ktT"):
                            t[nm] = self.sb(e1, f"h{nm}{d}{par}", [64, 4, 128], BF16)
                        t["ecT"] = self.sb(e1, f"hecT{d}{par}", [64, 4, 128])
                        t["qtTm"] = self.sb(e1, f"hqtTm{d}{par}", [64, 4, 4, 128], BF16)
                        t["scm"] = self.sb(e1, f"hscm{d}{par}", [128, 4, 128], BF16)
                        WS[(d, par)] = t
                Sr = [[self.sb(e1, f"HS{d}{i}", [64, 4, 64]) for i in range(3)] for d in range(2)]
                Sb_ = [[self.sb(e1, f"HSb{d}{i}", [64, 4, 64], BF16) for i in range(3)] for d in range(2)]

                def v64(p_):
                    return p_.t[0:64, :].rearrange("p (h t) -> p h t", h=4)

                def v128(p_):
                    return p_.t[:, :].rearrange("p (h t) -> p h t", h=4)

                def hg(d):
                    CUM = [C_U32, C_L32][d]
                    REM = [C_SL32, C_SU32][d]
                    fcol = 256 + 256 * d
                    order = list(range(NT)) if d == 0 else [1, 0] + list(range(NT - 1, 1, -1))
                    chunks = [0, 1, 2, 3] if d == 0 else [3, 2, 1, 0]
                    si = 0
                    S = Sr[d][0]
                    Sb = Sb_[d][0]
                    M.dve(lambda e: e.memset(S.t[:], 0.0), w=[S.k])
                    M.dve(lambda e: e.memset(Sb.t[:], 0.0), w=[Sb.k])
                    for it, j in enumerate(order):
                        T = WS[(d, it % 2)]
                        vb = T["vb"]
                        q, f, v, kk, ec, en, er, qt, kt, kh, khm, qtT, ktT, ecT, qtTm, scm = (T[n] for n in (
                            "q", "f", "v", "kk", "ec", "en", "er", "qt", "kt", "kh", "khm", "qtT", "ktT", "ecT", "qtTm", "scm"))
                        rows = slice(j * 128, (j + 1) * 128)
                        M.dma("sp", q.t[:], self.ptm.t[rows, 0:256], r=[self.ptm.k], w=[q.k])
                        M.dma("sp", f.t[:], self.ptm.t[rows, fcol:fcol + 256], r=[self.ptm.k], w=[f.k])
                        M.dma("pool", vb.t[:], self.ptm.t[rows, 768:1024], r=[self.ptm.k], w=[vb.k])
                        yield
                        M.act(lambda e: e.activation(out=f.t[:], in_=f.t[:], func=AF.Sigmoid), r=[f.k], w=[f.k])
                        yield
                        M.dve(lambda e: e.tensor_tensor(out=f.t[:], in0=f.t[:], in1=oml[d].t[:], op=ALU.mult), r=[f.k, oml[d].k], w=[f.k])
                        M.dve(lambda e: e.tensor_tensor(out=f.t[:], in0=f.t[:], in1=lb[d].t[:], op=ALU.add), r=[f.k, lb[d].k], w=[f.k])
                        M.dve(lambda e: e.tensor_scalar(out=kk.t[:], in0=f.t[:], scalar1=-1.0, scalar2=1.0, op0=ALU.mult, op1=ALU.add), r=[f.k], w=[kk.k])
                        M.dve(lambda e: e.tensor_scalar(out=f.t[:], in0=f.t[:], scalar1=1e-20, scalar2=None, op0=ALU.max), r=[f.k], w=[f.k])
                        yield
                        M.act(lambda e: e.activation(out=f.t[:], in_=f.t[:], func=AF.Ln), r=[f.k], w=[f.k])
                        yield
                        pcr = yield from galloc()
                        M.pe(lambda e: e.matmul(pcr.t[:, 0:256], lhsT=C[:, CUM:CUM + 128], rhs=f.t[:], start=True, stop=True), r=[cst.k, f.k], w=[pcr.k])
                        M.pe(lambda e: e.matmul(pcr.t[:, 256:512], lhsT=C[:, REM:REM + 128], rhs=f.t[:], start=True, stop=True), r=[cst.k, f.k], w=[pcr.k])
                        yield
                        M.act(lambda e: e.activation(out=ec.t[:], in_=pcr.t[:, 0:256], func=AF.Exp), r=[pcr.k], w=[ec.k])
                        M.act(lambda e: e.activation(out=en.t[:], in_=pcr.t[:, 0:256], func=AF.Exp, scale=-1.0), r=[pcr.k], w=[en.k])
                        M.act(lambda e: e.activation(out=er.t[:], in_=pcr.t[:, 256:512], func=AF.Exp), r=[pcr.k], w=[er.k])
                        rel(pcr)
                        yield
                        M.dve(lambda e: e.tensor_tensor(out=qt.t[:], in0=q.t[:], in1=ec.t[:], op=ALU.mult), r=[q.k, ec.k], w=[qt.k])
                        M.dve(lambda e: e.tensor_tensor(out=kt.t[:], in0=kk.t[:], in1=en.t[:], op=ALU.mult), r=[kk.k, en.k], w=[kt.k])
                        M.dve(lambda e: e.tensor_tensor(out=kh.t[:], in0=kk.t[:], in1=er.t[:], op=ALU.mult), r=[kk.k, er.k], w=[kh.k])
                        for c in range(4):
                            M.dve(lambda e: e.tensor_scalar(out=khm.t[:, c, :], in0=kh.t[:], scalar1=C[:, C_CM32 + c:C_CM32 + c + 1], scalar2=None, op0=ALU.mult),
                                  r=[kh.k, cst.k], w=[khm.k])
                        yield
                        for (src, dst) in ((qt, qtT), (kt, ktT), (ec, ecT)):
                            pt_ = yield from galloc()
                            for h in range(4):
                                M.pe(lambda e: e.transpose(v64(pt_)[:, h, :], src.t[:, h * 64:(h + 1) * 64], C[:, C_ID:C_ID + 128]), r=[src.k, cst.k], w=[pt_.k])
                            yield
                            M.act(lambda e: e.copy(out=dst.t[:], in_=v64(pt_)), r=[pt_.k], w=[dst.k])
                            rel(pt_)
                        yield
                        for c in range(4):
                            M.dve(lambda e: e.tensor_tensor(out=qtTm.t[:, c, :, :], in0=qtT.t[:], in1=_bc(C[0:64, C_COL32 + c * 128:C_COL32 + (c + 1) * 128], [64, 4, 128], 1),
                                                            op=ALU.mult), r=[qtT.k, cst.k], w=[qtTm.k])
                        psc = yield from galloc()
                        for h in range(4):
                            M.pe(lambda e: e.matmul(v128(psc)[:, h, :], lhsT=ktT.t[:, h, :], rhs=qtT.t[:, h, :], start=True, stop=True), r=[ktT.k, qtT.k], w=[psc.k])
                        yield
                        M.dve(lambda e: e.tensor_tensor(out=scm.t[:], in0=v128(psc), in1=_bc(C[:, CUM:CUM + 128], [128, 4, 128], 1), op=ALU.mult),
                              r=[psc.k, cst.k], w=[scm.k])
                        rel(psc)
                        yield
                        po = yield from galloc()
                        pov = po.t[:, 0:256].rearrange("p (h d) -> p h d", h=4)
                        for h in range(4):
                            M.pe(lambda e: e.matmul(pov[:, h, :], lhsT=scm.t[:, h, :], rhs=vb.t[:, h * 64:(h + 1) * 64], start=(h == 0), stop=False, skip_group_check=True),
                                 r=[scm.k, vb.k], w=[po.k])
                        for ci, c in enumerate(chunks):
                            pkv = yield from galloc()
                            kvv = pkv.t[0:64, 0:256].rearrange("p (h d) -> p h d", h=4)
                            for h in range(4):
                                M.pe(lambda e: e.matmul(kvv[:, h, :], lhsT=khm.t[:, c, h * 64:(h + 1) * 64], rhs=vb.t[:, h * 64:(h + 1) * 64], start=True, stop=True),
                                     r=[khm.k, vb.k], w=[pkv.k])
                            for h in range(4):
                                M.pe(lambda e: e.matmul(pov[:, h, :], lhsT=qtTm.t[:, c, h, :], rhs=Sb.t[:, h, :], start=False, stop=(ci == 3), skip_group_check=True),
                                     r=[qtTm.k, Sb.k], w=[po.k])
                            si = (si + 1) % 3
                            Sn = Sr[d][si]
                            col = 32 * c + 31 if d == 0 else 32 * c
                            M.dve(lambda e: e.tensor_tensor(out=Sn.t[:], in0=S.t[:], in1=ecT.t[:, :, col:col + 1].to_broadcast([64, 4, 64]), op=ALU.mult),
                                  r=[S.k, ecT.k], w=[Sn.k])
                            yield
                            Sbn = Sb_[d][si]
                            M.dve(lambda e: e.tensor_tensor(out=Sbn.t[:], in0=Sn.t[:], in1=kvv, op=ALU.add), r=[Sn.k, pkv.k], w=[Sbn.k])
                            M.dve(lambda e: e.tensor_tensor(out=Sn.t[:], in0=Sn.t[:], in1=kvv, op=ALU.add), r=[Sn.k, pkv.k], w=[Sn.k])
                            rel(pkv)
                            S, Sb = Sn, Sbn
                            yield
                        M.act(lambda e: e.copy(out=obuf[d].t[:, j, :], in_=po.t[:, 0:256]), r=[po.k], w=[obuf[d].k])
                        rel(po)
                        yield

                run_streams([hg(0), hg(1)])
                M.barrier()
            with ExitStack() as e1:
                KR = 4
                RS_ = []
                for i in range(KR):
                    t = {nm: self.sb(e1, f"hr{nm}{i}", [128, 256]) for nm in ("g", "osum", "sq")}
                    t["st4"] = self.sb(e1, f"hrst4{i}", [128, 4])
                    t["mo"] = self.sb(e1, f"hrmo{i}", [128, 256], BF16)
                    RS_.append(t)

                def ro(j, T):
                    g, osum, sq, st4, m_ = T["g"], T["osum"], T["sq"], T["st4"], T["mo"]
                    rows = slice(j * 128, (j + 1) * 128)
                    M.dma("sp", g.t[:], self.ptm.t[rows, 1024:1280], r=[self.ptm.k], w=[g.k])
                    M.dve(lambda e: e.tensor_tensor(out=osum.t[:], in0=obuf[0].t[:, j, :], in1=obuf[1].t[:, j, :], op=ALU.add), r=[obuf[0].k, obuf[1].k], w=[osum.k])
                    yield
                    M.act(lambda e: e.activation(out=sq.t[:], in_=osum.t[:], func=AF.Square), r=[osum.k], w=[sq.k])
                    M.act(lambda e: e.activation(out=g.t[:], in_=g.t[:], func=AF.Silu), r=[g.k], w=[g.k])
                    yield
                    M.dve(lambda e: e.tensor_reduce(out=st4.t[:], in_=sq.t[:].rearrange("p (h d) -> p h d", h=4), axis=AX.X, op=ALU.add), r=[sq.k], w=[st4.k])
                    M.dve(lambda e: e.tensor_scalar(out=st4.t[:], in0=st4.t[:], scalar1=1.0 / 64, scalar2=EPS, op0=ALU.mult, op1=ALU.add), r=[st4.k], w=[st4.k])
                    yield
                    M.act(lambda e: e.activation(out=st4.t[:], in_=st4.t[:], func=AF.Sqrt), r=[st4.k], w=[st4.k])
                    yield
                    M.dve(lambda e: e.reciprocal(out=st4.t[:], in_=st4.t[:]), r=[st4.k], w=[st4.k])
                    M.dve(lambda e: e.tensor_tensor(out=osum.t[:].rearrange("p (h d) -> p h d", h=4), in0=osum.t[:].rearrange("p (h d) -> p h d", h=4),
                                                    in1=_bc(st4.t[:], [128, 4, 64], 2), op=ALU.mult), r=[osum.k, st4.k], w=[osum.k])
                    M.dve(lambda e: e.tensor_tensor(out=osum.t[:], in0=osum.t[:], in1=gnb.t[:], op=ALU.mult), r=[osum.k, gnb.k], w=[osum.k])
                    M.dve(lambda e: e.tensor_tensor(out=m_.t[:], in0=osum.t[:], in1=g.t[:], op=ALU.mult), r=[osum.k, g.k], w=[m_.k])
                    yield
                    M.dma("pool", self.mixs.t[rows, 0:256], m_.t[:], r=[m_.k], w=[self.mixs.k])

                todo = list(range(NT))
                active = []
                freeR = list(range(KR))

                def wrap(j, ri):
                    yield from ro(j, RS_[ri])
                    freeR.append(ri)

                while todo or active:
                    while todo and freeR:
                        active.append(wrap(todo.pop(0), freeR.pop(0)))
                    for gth in list(active):
                        try:
                            next(gth)
                        except StopIteration:
                            active.remove(gth)
                M.barrier()


class ProgV4(ProgV3):
    def stage_mid(self, l, es, xres, h2T, comb, last=False):
        from contextlib import ExitStack
        M, nc, I = self.M, self.nc, self.I
        cst = self.cst
        C = cst.t
        with ExitStack() as e2:
            wout = self.sb(e2, "wout", [128, 8, D], BF16)
            wv = I["w_out"][l].rearrange("(k p) n -> p k n", p=128)
            for k in range(8):
                M.dma("pool", wout.t[:, k, :], wv[:, k, :], w=[wout.k])
            g1b = [self.sb(e2, f"g1b{w}", [128, D]) for w in range(2)]
            for w in range(2):
                M.dma("sp", g1b[w].t[:], self.modrow(l, w, 2).partition_broadcast(128), r=[self.mods.k], w=[g1b[w].k])
            A, S = self.norm_mod_tiles(e2, l, "norm2_gain", 4, 3)
            wr = self.sb(e2, "wr", [128, 8, 36])
            br = self.sb(e2, "br", [1, 36])
            M.dma("sp", wr.t[:], I["w_router"][l].rearrange("(k p) n -> p k n", p=128), w=[wr.k])
            M.dma("sp", br.t[:], I["b_router"][l:l + 1, :], w=[br.k])
            NS = 2
            SETS = []
            for i in range(NS):
                t = {}
                t["mx"] = self.sb(e2, f"mx{i}", [128, D], BF16)
                t["mT"] = self.sb(e2, f"mT{i}", [128, 8, 128], BF16)
                t["tmp"] = self.sb(e2, f"tmpo{i}", [128, 512])
                t["sq"] = self.sb(e2, f"sq2{i}", [128, D], BF16)
                t["ss"] = self.sb(e2, f"ss2{i}", [128, 1])
                t["hf"] = self.sb(e2, f"hf2{i}", [128, D])
                t["hb"] = self.sb(e2, f"hb2{i}", [128, D], BF16)
                t["hTf"] = self.sb(e2, f"hTf{i}", [128, 8, 128])
                t["lg"] = self.sb(e2, f"lg{i}", [128, 36])
                for n in ("oh", "esel", "mk1", "e2", "mk2", "ew"):
                    t[n] = self.sb(e2, f"{n}{i}", [128, 8])
                for n in ("gm", "ngm", "gs", "m1", "m2", "dm", "w1", "w2"):
                    t[n] = self.sb(e2, f"{n}{i}", [128, 1])
                t["junk"] = self.sb(e2, f"junk{i}", [128, 4])
                t["msk3"] = self.sb(e2, f"msk3{i}", [128, 4, 8])
                t["ptb"] = self.ps(e2, f"ptb{i}", [128, 8, 128], BF16)
                SETS.append(t)
            pool = [self.ps(e2, f"mp{i}", [128, 512]) for i in range(6)]
            busy = [False] * 6
            gi = [0]

            def alloc():
                for i in range(6):
                    kx = (gi[0] + 1 + i) % 6
                    if not busy[kx]:
                        busy[kx] = True
                        gi[0] = kx
                        return pool[kx]
                return None

            def rel(p):
                busy[pool.index(p)] = False

            def galloc():
                while True:
                    p = alloc()
                    if p is not None:
                        return p
                    yield

            xk = [Tk(f"x{j}") for j in range(NT)]
            hk = [Tk(f"h{j}") for j in range(NT)]
            ck = [Tk(f"c{j}") for j in range(NT)]

            def tile(j, T):
                rows = slice(j * 128, (j + 1) * 128)
                which = 1 if j < 2 else 0
                m_, mT, tmp, sq, ss, hf, hb, hTf, lg, junk, msk3, ptb = (T[n] for n in ("mx", "mT", "tmp", "sq", "ss", "hf", "hb", "hTf", "lg", "junk", "msk3", "ptb"))
                xj = xres.t[:, j, :]
                M.dma("sp", m_.t[:], self.mixs.t[rows, :], r=[self.mixs.k], w=[m_.k])
                if l == 0:
                    M.dma("sp", xj, I["x_all"][rows, :], w=[xk[j]])
                else:
                    M.dma("sp", xj, self.xs.t[rows, :], r=[self.xs.k], w=[xk[j]])
                yield
                for k in range(8):
                    M.pe(lambda e: e.transpose(ptb.t[:, k, :], m_.t[:, k * 128:(k + 1) * 128], self.idb.t[:]), r=[m_.k, self.idb.k], w=[ptb.k])
                yield
                M.act(lambda e: e.copy(out=mT.t[:], in_=ptb.t[:]), r=[ptb.k], w=[mT.k])
                yield
                for hf_ in range(2):
                    p_ = yield from galloc()
                    cs_ = slice(hf_ * 512, (hf_ + 1) * 512)
                    for k in range(8):
                        M.pe(lambda e: e.matmul(p_.t[:, :], lhsT=mT.t[:, k, :], rhs=wout.t[:, k, cs_], start=(k == 0), stop=(k == 7)), r=[mT.k, wout.k], w=[p_.k])
                    yield
                    M.dve(lambda e: e.tensor_tensor(out=tmp.t[:], in0=p_.t[:], in1=g1b[which].t[:, cs_], op=ALU.mult), r=[p_.k, g1b[which].k], w=[tmp.k])
                    rel(p_)
                    M.dve(lambda e: e.tensor_tensor(out=xres.t[:, j, cs_], in0=xres.t[:, j, cs_], in1=tmp.t[:], op=ALU.add), r=[xk[j], tmp.k], w=[xk[j]])
                yield
                M.act(lambda e: e.activation(out=sq.t[:], in_=xj, func=AF.Square, accum_out=ss.t[:]), r=[xk[j]], w=[sq.k, ss.k])
                yield
                M.dve(lambda e: e.tensor_scalar(out=ss.t[:], in0=ss.t[:], scalar1=1.0 / D, scalar2=EPS, op0=ALU.mult, op1=ALU.add), r=[ss.k], w=[ss.k])
                yield
                M.act(lambda e: e.activation(out=ss.t[:], in_=ss.t[:], func=AF.Sqrt), r=[ss.k], w=[ss.k])
                yield
                M.dve(lambda e: e.reciprocal(out=ss.t[:], in_=ss.t[:]), r=[ss.k], w=[ss.k])
                M.dve(lambda e: e.scalar_tensor_tensor(out=hf.t[:], in0=xj, scalar=ss.t[:, 0:1], in1=A[which].t[:], op0=ALU.mult, op1=ALU.mult),
                      r=[xk[j], ss.k, A[which].k], w=[hf.k])
                M.dve(lambda e: e.tensor_tensor(out=hf.t[:], in0=hf.t[:], in1=S[which].t[:], op=ALU.add), r=[hf.k, S[which].k], w=[hf.k])
                yield
                M.act(lambda e: e.copy(out=hb.t[:], in_=hf.t[:]), r=[hf.k], w=[hb.k])
                ptf = []
                for q4 in range(2):
                    pf = yield from galloc()
                    ptf.append(pf)
                    pfv = pf.t[:, :].rearrange("p (k t) -> p k t", k=4)
                    for k in range(4):
                        kk_ = q4 * 4 + k
                        M.pe(lambda e: e.transpose(pfv[:, k, :], hf.t[:, kk_ * 128:(kk_ + 1) * 128], C[:, C_ID:C_ID + 128]), r=[hf.k, cst.k], w=[pf.k])
                yield
                for k in range(8):
                    M.pe(lambda e: e.transpose(ptb.t[:, k, :], hb.t[:, k * 128:(k + 1) * 128], self.idb.t[:]), r=[hb.k, self.idb.k], w=[ptb.k])
                for q4 in range(2):
                    M.act(lambda e: e.copy(out=hTf.t[:, q4 * 4:(q4 + 1) * 4, :], in_=ptf[q4].t[:, :].rearrange("p (k t) -> p k t", k=4)), r=[ptf[q4].k], w=[hTf.k])
                    rel(ptf[q4])
                yield
                M.act(lambda e: e.copy(out=h2T.t[:, :, rows], in_=ptb.t[:]), r=[ptb.k], w=[hk[j]])
                plg = yield from galloc()
                for k in range(8):
                    M.pe(lambda e: e.matmul(plg.t[:, 0:36], lhsT=hTf.t[:, k, :], rhs=wr.t[:, k, :], start=(k == 0), stop=False), r=[hTf.k, wr.k], w=[plg.k])
                M.pe(lambda e: e.matmul(plg.t[:, 0:36], lhsT=self.ones.t[0:1, 0:128], rhs=br.t[0:1, :], start=False, stop=True), r=[self.ones.k, br.k], w=[plg.k])
                yield
                M.act(lambda e: e.copy(out=lg.t[:], in_=plg.t[:, 0:36]), r=[plg.k], w=[lg.k])
                rel(plg)
                yield
                G = lg.t[:, 0:4]
                E3 = lg.t[:, 4:36].rearrange("p (g e) -> p g e", g=4)
                oh, esel, mk1, e2_, mk2, ew = (T[n] for n in ("oh", "esel", "mk1", "e2", "mk2", "ew"))
                gm, ngm, gs, m1, m2, dm, w1, w2 = (T[n] for n in ("gm", "ngm", "gs", "m1", "m2", "dm", "w1", "w2"))
                M.dve(lambda e: e.tensor_reduce(out=gm.t[:], in_=G, axis=AX.X, op=ALU.max), r=[lg.k], w=[gm.k])
                M.dve(lambda e: e.tensor_scalar(out=oh.t[:, 0:4], in0=G, scalar1=gm.t[:, 0:1], scalar2=None, op0=ALU.is_equal), r=[lg.k, gm.k], w=[oh.k])
                M.dve(lambda e: e.tensor_scalar(out=ngm.t[:], in0=gm.t[:], scalar1=-1.0, scalar2=None, op0=ALU.mult), r=[gm.k], w=[ngm.k])
                M.dve(lambda e: e.tensor_tensor(out=msk3.t[:], in0=E3, in1=_bc(oh.t[:, 0:4], [128, 4, 8], 2), op=ALU.mult), r=[lg.k, oh.k], w=[msk3.k])
                M.dve(lambda e: e.tensor_reduce(out=esel.t[:], in_=msk3.t[:].rearrange("p g e -> p e g"), axis=AX.X, op=ALU.add), r=[msk3.k], w=[esel.k])
                M.dve(lambda e: e.tensor_reduce(out=m1.t[:], in_=esel.t[:], axis=AX.X, op=ALU.max), r=[esel.k], w=[m1.k])
                yield
                M.act(lambda e: e.activation(out=junk.t[:], in_=G, func=AF.Exp, bias=ngm.t[:, 0:1], accum_out=gs.t[:]), r=[lg.k, ngm.k], w=[junk.k, gs.k])
                M.dve(lambda e: e.tensor_scalar(out=mk1.t[:], in0=esel.t[:], scalar1=m1.t[:, 0:1], scalar2=None, op0=ALU.is_equal), r=[esel.k, m1.k], w=[mk1.k])
                M.dve(lambda e: e.scalar_tensor_tensor(out=e2_.t[:], in0=mk1.t[:], scalar=-1e30, in1=esel.t[:], op0=ALU.mult, op1=ALU.add), r=[mk1.k, esel.k], w=[e2_.k])
                M.dve(lambda e: e.tensor_reduce(out=m2.t[:], in_=e2_.t[:], axis=AX.X, op=ALU.max), r=[e2_.k], w=[m2.k])
                M.dve(lambda e: e.tensor_scalar(out=mk2.t[:], in0=e2_.t[:], scalar1=m2.t[:, 0:1], scalar2=None, op0=ALU.is_equal), r=[e2_.k, m2.k], w=[mk2.k])
                M.dve(lambda e: e.tensor_tensor(out=dm.t[:], in0=m2.t[:], in1=m1.t[:], op=ALU.subtract), r=[m1.k, m2.k], w=[dm.k])
                yield
                M.act(lambda e: e.activation(out=dm.t[:], in_=dm.t[:], func=AF.Exp), r=[dm.k], w=[dm.k])
                M.dve(lambda e: e.reciprocal(out=gs.t[:], in_=gs.t[:]), r=[gs.k], w=[gs.k])
                yield
                M.dve(lambda e: e.tensor_scalar(out=w1.t[:], in0=dm.t[:], scalar1=1.0, scalar2=None, op0=ALU.add), r=[dm.k], w=[w1.k])
                M.dve(lambda e: e.reciprocal(out=w1.t[:], in_=w1.t[:]), r=[w1.k], w=[w1.k])
                M.dve(lambda e: e.tensor_tensor(out=w1.t[:], in0=w1.t[:], in1=gs.t[:], op=ALU.mult), r=[w1.k, gs.k], w=[w1.k])
                M.dve(lambda e: e.tensor_tensor(out=w2.t[:], in0=w1.t[:], in1=dm.t[:], op=ALU.mult), r=[w1.k, dm.k], w=[w2.k])
                M.dve(lambda e: e.tensor_scalar(out=ew.t[:], in0=mk1.t[:], scalar1=w1.t[:, 0:1], scalar2=None, op0=ALU.mult), r=[mk1.k, w1.k], w=[ew.k])
                M.dve(lambda e: e.scalar_tensor_tensor(out=ew.t[:], in0=mk2.t[:], scalar=w2.t[:, 0:1], in1=ew.t[:], op0=ALU.mult, op1=ALU.add), r=[mk2.k, w2.k, ew.k], w=[ew.k])
                for g in range(4):
                    M.dve(lambda e: e.tensor_scalar(out=comb.t[:, j, g * 8:(g + 1) * 8], in0=ew.t[:], scalar1=oh.t[:, g:g + 1], scalar2=None, op0=ALU.mult),
                          r=[ew.k, oh.k], w=[ck[j]])
                yield

            todo = list(range(2 if last else 0, NT))
            active = []
            freeS = list(range(NS))

            def wrap(j, si_):
                yield from tile(j, SETS[si_])
                freeS.append(si_)

            while todo or active:
                while todo and freeS:
                    active.append(wrap(todo.pop(0), freeS.pop(0)))
                for gth in list(active):
                    try:
                        next(gth)
                    except StopIteration:
                        active.remove(gth)
            M.barrier()


class ProgV5(ProgV4):
    def stage_moe(self, l, xres, h2T, comb, last):
        from contextlib import ExitStack
        M, nc, I = self.M, self.nc, self.I
        with ExitStack() as e2:
            g2b = [self.sb(e2, f"g2b{w}", [128, D]) for w in range(2)]
            for w in range(2):
                M.dma("sp", g2b[w].t[:], self.modrow(l, w, 5).partition_broadcast(128), r=[self.mods.k], w=[g2b[w].k])
            Wg = [self.sb(e2, f"Wg{i}", [128, 8, 512], BF16) for i in range(2)]
            Wu = [self.sb(e2, f"Wu{i}", [128, 8, 512], BF16) for i in range(2)]
            Wd = [self.sb(e2, f"Wd{i}", [128, 4, D], BF16) for i in range(2)]
            hid = [self.sb(e2, f"hid{i}", [128, 4, 512], BF16) for i in range(2)]
            sg = [self.sb(e2, f"sg{i}", [128, 512]) for i in range(2)]
            tmp = [self.sb(e2, f"tmpm{i}", [128, 512]) for i in range(3)]
            pG = [self.ps(e2, f"pG{i}", [128, 512]) for i in range(2)]
            pU = [self.ps(e2, f"pU{i}", [128, 512]) for i in range(2)]
            pD = [self.ps(e2, f"pD{i}", [128, 512]) for i in range(4)]
            xk = [Tk(f"mx{j}") for j in range(NT)]
            chunks = list(range(256 if last else 0, TALL, 512))
            work = [(ge, ci) for ge in range(32) for ci in range(len(chunks))]
            st = {"gu_done": -1, "d_done": -1}

            def gu():
                cnt = 0
                for wi, (ge, ci) in enumerate(work):
                    b = ge % 2
                    while st["d_done"] < wi - 2:
                        yield
                    if ci == 0:
                        gv = I["w_exp_gate"][l, ge].rearrange("(k p) f -> p k f", p=128)
                        uv = I["w_exp_up"][l, ge].rearrange("(k p) f -> p k f", p=128)
                        dv = I["w_exp_down"][l, ge].rearrange("(c p) d -> p c d", p=128)
                        for k in range(0, 8, 2):
                            M.dma("pool", Wg[b].t[:, k:k + 2, :], gv[:, k:k + 2, :], w=[Wg[b].k])
                            M.dma("pool", Wu[b].t[:, k:k + 2, :], uv[:, k:k + 2, :], w=[Wu[b].k])
                        for c in range(4):
                            M.dma("pool", Wd[b].t[:, c, :], dv[:, c, :], w=[Wd[b].k])
                    t0 = chunks[ci]
                    t1 = min(TALL, t0 + 512)
                    n = t1 - t0
                    hd = hid[wi % 2]
                    for fc in range(4):
                        cnt += 1
                        g_, u_, s_ = pG[cnt % 2], pU[cnt % 2], sg[cnt % 2]
                        for k in range(8):
                            M.pe(lambda e: e.matmul(g_.t[:, 0:n], lhsT=Wg[b].t[:, k, fc * 128:(fc + 1) * 128], rhs=h2T.t[:, k, t0:t1], start=(k == 0), stop=(k == 7)),
                                 r=[Wg[b].k, h2T.k], w=[g_.k])
                        yield
                        for k in range(8):
                            M.pe(lambda e: e.matmul(u_.t[:, 0:n], lhsT=Wu[b].t[:, k, fc * 128:(fc + 1) * 128], rhs=h2T.t[:, k, t0:t1], start=(k == 0), stop=(k == 7)),
                                 r=[Wu[b].k, h2T.k], w=[u_.k])
                        M.act(lambda e: e.activation(out=s_.t[:, 0:n], in_=g_.t[:, 0:n], func=AF.Silu), r=[g_.k], w=[s_.k])
                        yield
                        M.dve(lambda e: e.tensor_tensor(out=hd.t[:, fc, 0:n], in0=s_.t[:, 0:n], in1=u_.t[:, 0:n], op=ALU.mult), r=[s_.k, u_.k], w=[hd.k])
                    st["gu_done"] = wi
                    yield

            def dn():
                cnt = 0
                for wi, (ge, ci) in enumerate(work):
                    b = ge % 2
                    while st["gu_done"] < wi:
                        yield
                    t0 = chunks[ci]
                    t1 = min(TALL, t0 + 512)
                    n = t1 - t0
                    hd = hid[wi % 2]
                    for jt in range(n // 128):
                        j = t0 // 128 + jt
                        which = 1 if j < 2 else 0
                        for hf_ in range(2):
                            cnt += 1
                            d_, tm = pD[cnt % 4], tmp[cnt % 3]
                            cs_ = slice(hf_ * 512, (hf_ + 1) * 512)
                            for fc in range(4):
                                M.pe(lambda e: e.matmul(d_.t[:, :], lhsT=hd.t[:, fc, jt * 128:(jt + 1) * 128], rhs=Wd[b].t[:, fc, cs_], start=(fc == 0), stop=(fc == 3)),
                                     r=[hd.k, Wd[b].k], w=[d_.k])
                            M.dve(lambda e: e.tensor_tensor(out=tm.t[:], in0=d_.t[:], in1=g2b[which].t[:, cs_], op=ALU.mult), r=[d_.k, g2b[which].k], w=[tm.k])
                            M.dve(lambda e: e.scalar_tensor_tensor(out=xres.t[:, j, cs_], in0=tm.t[:], scalar=comb.t[:, j, ge:ge + 1], in1=xres.t[:, j, cs_],
                                                                   op0=ALU.mult, op1=ALU.add), r=[tm.k, comb.k, xk[j]], w=[xk[j]])
                            yield
                        if ge == 31:
                            if last:
                                M.dma("sp", self.out[(j - 2) * 128:(j - 1) * 128, :], xres.t[:, j, :], r=[xk[j]], w=[self.xs.k])
                            else:
                                M.dma("sp", self.xs.t[j * 128:(j + 1) * 128, :], xres.t[:, j, :], r=[xk[j]], w=[self.xs.k])
                    st["d_done"] = wi

            run_streams([gu(), dn()])
            M.barrier()
```

```python
import numpy as np
import concourse.bass as bass
import concourse.mybir as mybir
from concourse.bass_utils import run_bass_kernel_spmd

F32 = mybir.dt.float32
BF16 = mybir.dt.bfloat16
ALU = mybir.AluOpType
AF = mybir.ActivationFunctionType
AX = mybir.AxisListType


class Tk:
    __slots__ = ("w", "r", "name")

    def __init__(self, name=""):
        self.w = None
        self.r = {}
        self.name = name


class _Eng:
    def __init__(self, mgr, name, eng):
        self.mgr = mgr
        self.name = name
        self.eng = eng
        self.seen = {}
        self.sid = None
        self.cnt = 0
        self.dma_sids = []
        self.dma_cnt = []
        self.dma_rr = 0
        self.n_inst = 0

    def comp_token(self):
        if self.sid is None or self.cnt >= 30000:
            self.sid = self.mgr.new_sem(f"s_{self.name}_{len(self.mgr.sems)}")
            self.cnt = 0
        self.cnt += 1
        return (self.sid, self.cnt)

    def dma_token(self):
        if not self.dma_sids:
            for i in range(8):
                self.dma_sids.append(self.mgr.new_sem(f"d_{self.name}_{i}"))
                self.dma_cnt.append(0)
        k = self.dma_rr
        self.dma_rr = (self.dma_rr + 1) % len(self.dma_sids)
        if self.dma_cnt[k] >= 30000:
            self.dma_sids[k] = self.mgr.new_sem(f"d_{self.name}_{len(self.mgr.sems)}")
            self.dma_cnt[k] = 0
        self.dma_cnt[k] += 16
        return (self.dma_sids[k], self.dma_cnt[k])


class Mgr:
    def __init__(self, nc):
        self.nc = nc
        self.sems = []
        self.E = {
            "pe": _Eng(self, "pe", nc.tensor),
            "act": _Eng(self, "act", nc.scalar),
            "dve": _Eng(self, "dve", nc.vector),
            "pool": _Eng(self, "pool", nc.gpsimd),
            "sp": _Eng(self, "sp", nc.sync),
        }
        self.last_out_tok = []

    def new_sem(self, name):
        h = self.nc.alloc_semaphore(name)
        self.sems.append(h)
        return len(self.sems) - 1

    def op(self, q, fn, r=(), w=(), dma=False):
        E = self.E[q]
        waits = {}

        def need(tok):
            if tok is None:
                return
            sid, val = tok
            if waits.get(sid, 0) < val:
                waits[sid] = val

        for t in r:
            need(t.w)
        for t in w:
            if not (q == "pe" and not dma and t.w is not None and t.w[0] == E.sid):
                need(t.w)
            for tok in t.r.items():
                need(tok)
        for sid, val in waits.items():
            if E.seen.get(sid, 0) < val:
                E.eng.wait_ge(self.sems[sid], val)
                E.seen[sid] = val
        ins = fn(E.eng)
        tok = E.dma_token() if dma else E.comp_token()
        ins.then_inc(self.sems[tok[0]], 16 if dma else 1)
        E.n_inst += 1
        for t in r:
            if t.r.get(tok[0], 0) < tok[1]:
                t.r[tok[0]] = tok[1]
        for t in w:
            t.w = tok
            t.r = {}
        return tok

    def pe(self, fn, r=(), w=()):
        return self.op("pe", fn, r, w)

    def act(self, fn, r=(), w=()):
        return self.op("act", fn, r, w)

    def dve(self, fn, r=(), w=()):
        return self.op("dve", fn, r, w)

    def pool(self, fn, r=(), w=()):
        return self.op("pool", fn, r, w)

    def dma(self, q, out, in_, r=(), w=()):
        return self.op(q, lambda e: e.dma_start(out=out, in_=in_), r, w, dma=True)

    def barrier(self):
        toks = []
        for E in self.E.values():
            if E.sid is not None and E.cnt > 0:
                toks.append((E.sid, E.cnt))
            for s, c in zip(E.dma_sids, E.dma_cnt):
                if c > 0:
                    toks.append((s, c))
        for E in self.E.values():
            for sid, val in toks:
                if E.seen.get(sid, 0) < val:
                    E.eng.wait_ge(self.sems[sid], val)
                    E.seen[sid] = val

    def wait_tok(self, q, tok):
        E = self.E[q]
        if E.seen.get(tok[0], 0) < tok[1]:
            E.eng.wait_ge(self.sems[tok[0]], tok[1])
            E.seen[tok[0]] = tok[1]


NT = 18
TALL = 2304
D = 1024
PTOT = 3968
NEGB = -30000.0


def _tri(chunk, kind):
    i = np.arange(128)
    same = (i[:, None] // chunk) == (i[None, :] // chunk)
    s, t = i[:, None], i[None, :]
    m = {"U": s <= t, "L": s >= t, "SU": s < t, "SL": s > t}[kind]
    return (same & m).astype(np.float32)


C_ID, C_U32, C_L32, C_SU32, C_SL32, C_U64, C_L64, C_SU64, C_SL64 = [128 * i for i in range(9)]
C_CM32 = 1152
C_COL32 = 1156
C_CM64 = 1668
C_END = 1672


def make_consts():
    c = np.zeros((128, C_END), np.float32)
    c[:, C_ID:C_ID + 128] = np.eye(128)
    for off, (ch, kd) in zip([C_U32, C_L32, C_SU32, C_SL32, C_U64, C_L64, C_SU64, C_SL64],
                             [(32, "U"), (32, "L"), (32, "SU"), (32, "SL"), (64, "U"), (64, "L"), (64, "SU"), (64, "SL")]):
        c[:, off:off + 128] = _tri(ch, kd)
    p = np.arange(128)
    for cc in range(4):
        c[:, C_CM32 + cc] = (p // 32 == cc)
        c[:, C_COL32 + cc * 128:C_COL32 + (cc + 1) * 128] = (p[None, :] // 32 == cc)
    for cc in range(2):
        c[:, C_CM64 + cc] = (p // 64 == cc)
    return c


class B_:
    __slots__ = ("t", "k")

    def __init__(self, t, name):
        self.t = t
        self.k = Tk(name)


EPS = 1e-6
IN_SPECS = [
    ("x_all", [TALL, D]), ("c2", [D, 2]), ("consts", [128, C_END]),
    ("norm1_gain", [4, D]), ("norm2_gain", [4, D]), ("w_ada", [4, D, 6 * D]), ("b_ada", [4, 6 * D]),
    ("w_in", [4, D, PTOT]), ("w_out", [4, D, D]), ("hgrn_lb_logits", [2, 4, 256]), ("hgrn_gn_gain", [4, 256]),
    ("rwkv_mu", [4, 1152]), ("rwkv_w0", [4, 2, 256]), ("rwkv_w_up", [4, 2, 64, 256]), ("rwkv_a0", [4, 2, 256]),
    ("rwkv_a_up", [4, 2, 64, 256]), ("rwkv_g_up", [4, 128, 256]), ("rwkv_kk_scale", [4, 256]), ("rwkv_k_a", [4, 256]),
    ("rwkv_r_k", [4, 256]), ("rwkv_ln_gain", [4, 256]), ("rwkv_ln_bias", [4, 256]),
    ("na_q_gain", [4, 64]), ("na_k_gain", [4, 64]), ("rpbT", [4, 128, 8 * 14 * 64]), ("rope_cs", [TALL, 64]),
    ("w_router", [4, D, 36]), ("b_router", [4, 36]),
    ("w_exp_gate", [4, 32, D, 512]), ("w_exp_up", [4, 32, D, 512]), ("w_exp_down", [4, 32, 512, D]),
]


class Prog:
    def __init__(self, NL=4, dbg=False, moe_in=True):
        self.NL = NL
        self.dbg = dbg
        nc = self.nc = bass.Bass("TRN2", target_bir_lowering=False)
        self.M = Mgr(nc)
        self.I = {}
        self.moe_in = moe_in
        for name, shape in IN_SPECS:
            shape = list(shape)
            if shape[0] == 4 and name not in ("hgrn_lb_logits",):
                shape[0] = NL
            if name.startswith("w_exp") and not moe_in:
                shape = [1, 1, 128, 128]
            self.I[name] = nc.dram_tensor(name, shape, F32, kind="ExternalInput").ap()
        sk = "ExternalOutput" if dbg else "Internal"
        self.out = nc.dram_tensor("out", [2048, D], F32, kind="ExternalOutput").ap()
        self.xs = B_(nc.dram_tensor("xs", [TALL, D], F32, kind=sk).ap(), "xs")
        self.mods = B_(nc.dram_tensor("mods", [4, 2, 6 * D], F32, kind=sk).ap(), "mods")
        self.ptm = B_(nc.dram_tensor("ptm", [TALL, PTOT], F32, kind=sk).ap(), "ptm")
        self.mixs = B_(nc.dram_tensor("mixs", [TALL, D], BF16, kind=sk).ap(), "mixs")
        self.lbs = B_(nc.dram_tensor("lbs", [2, 4, 256], F32, kind=sk).ap(), "lbs")
        self.uid = 0

    def sb(self, es, name, shape, dt=F32):
        self.uid += 1
        t = es.enter_context(self.nc.sbuf_tensor(f"{name}_{self.uid}", list(shape), dt))
        return B_(t, name)

    def ps(self, es, name, shape, dt=F32):
        self.uid += 1
        t = es.enter_context(self.nc.psum_tensor(f"{name}_{self.uid}", list(shape), dt))
        return B_(t, name)

    def dump(self, name, buf, ap=None):
        if not self.dbg:
            return
        ap = buf.t[:] if ap is None else ap
        dt_ = self.nc.dram_tensor(name, list(ap.shape), buf.t.dtype, kind="ExternalOutput").ap()
        self.M.op("sp", lambda e: e.dma_start(out=dt_, in_=ap), r=[buf.k], w=[], dma=True)

    def bload(self, q, dst, src_row_ap, n):
        self.M.dma(q, dst.t[:, 0:n], src_row_ap.partition_broadcast(128), w=[dst.k])

    def setup(self, es):
        M, nc, I = self.M, self.nc, self.I
        self.cst = self.sb(es, "cst", [128, C_END])
        M.dma("sp", self.cst.t[:], I["consts"][:, :], w=[self.cst.k])
        self.idb = self.sb(es, "idb", [128, 128], BF16)
        M.dve(lambda e: e.tensor_copy(out=self.idb.t[:], in_=self.cst.t[:, C_ID:C_ID + 128]), r=[self.cst.k], w=[self.idb.k])
        self.ones = self.sb(es, "ones", [128, 128])
        M.dve(lambda e: e.memset(self.ones.t[:], 1.0), w=[self.ones.k])
        self.scT = self.sb(es, "scT", [128, 8, 2])
        M.dma("sp", self.scT.t[:], I["c2"].rearrange("(k p) j -> p k j", p=128), w=[self.scT.k])
        M.act(lambda e: e.activation(out=self.scT.t[:], in_=self.scT.t[:], func=AF.Silu), r=[self.scT.k], w=[self.scT.k])
        from contextlib import ExitStack
        with ExitStack() as s2:
            lg = self.sb(s2, "lg", [2, 4, 256])
            mx = self.sb(s2, "mx", [2, 256])
            sm = self.sb(s2, "sm", [2, 256])
            M.dma("sp", lg.t[:], I["hgrn_lb_logits"][:, :, :], w=[lg.k])
            M.dve(lambda e: e.tensor_tensor(out=mx.t[:], in0=lg.t[:, 0, :], in1=lg.t[:, 1, :], op=ALU.max), r=[lg.k], w=[mx.k])
            M.dve(lambda e: e.tensor_tensor(out=mx.t[:], in0=mx.t[:], in1=lg.t[:, 2, :], op=ALU.max), r=[lg.k, mx.k], w=[mx.k])
            M.dve(lambda e: e.tensor_tensor(out=mx.t[:], in0=mx.t[:], in1=lg.t[:, 3, :], op=ALU.max), r=[lg.k, mx.k], w=[mx.k])
            M.dve(lambda e: e.tensor_tensor(out=lg.t[:], in0=lg.t[:], in1=mx.t[:].unsqueeze(1).to_broadcast([2, 4, 256]), op=ALU.subtract),
                  r=[lg.k, mx.k], w=[lg.k])
            M.act(lambda e: e.activation(out=lg.t[:], in_=lg.t[:], func=AF.Exp), r=[lg.k], w=[lg.k])
            M.dve(lambda e: e.tensor_tensor(out=sm.t[:], in0=lg.t[:, 0, :], in1=lg.t[:, 1, :], op=ALU.add), r=[lg.k], w=[sm.k])
            M.dve(lambda e: e.tensor_tensor(out=sm.t[:], in0=sm.t[:], in1=lg.t[:, 2, :], op=ALU.add), r=[lg.k, sm.k], w=[sm.k])
            M.dve(lambda e: e.tensor_tensor(out=sm.t[:], in0=sm.t[:], in1=lg.t[:, 3, :], op=ALU.add), r=[lg.k, sm.k], w=[sm.k])
            M.dve(lambda e: e.reciprocal(out=sm.t[:], in_=sm.t[:]), r=[sm.k], w=[sm.k])
            M.dve(lambda e: e.tensor_tensor(out=lg.t[:], in0=lg.t[:], in1=sm.t[:].unsqueeze(1).to_broadcast([2, 4, 256]), op=ALU.mult),
                  r=[lg.k, sm.k], w=[lg.k])
            M.dve(lambda e: e.memset(lg.t[:, 0, :], 0.0), r=[lg.k], w=[lg.k])
            for l in range(2, 4):
                M.dve(lambda e, l=l: e.tensor_tensor(out=lg.t[:, l, :], in0=lg.t[:, l, :], in1=lg.t[:, l - 1, :], op=ALU.add), r=[lg.k], w=[lg.k])
            M.dma("sp", self.lbs.t[:, :, :], lg.t[:], r=[lg.k], w=[self.lbs.k])
            M.barrier()

    def stage_mods(self, l):
        from contextlib import ExitStack
        M, nc, I = self.M, self.nc, self.I
        with ExitStack() as es:
            wb = [self.sb(es, f"wada{i}", [128, 8, 512]) for i in range(3)]
            brow = self.sb(es, "brow", [1, 6 * D])
            mrow = self.sb(es, "mrow", [2, 6 * D])
            pm = [self.ps(es, f"pm{i}", [2, 512]) for i in range(2)]
            M.dma("sp", brow.t[:], I["b_ada"][l:l + 1, :], w=[brow.k])
            wv = I["w_ada"][l].rearrange("(k p) n -> p k n", p=128)
            for n in range(12):
                w_ = wb[n % 3]
                p_ = pm[n % 2]
                M.dma("sp" if n % 2 == 0 else "pool", w_.t[:], wv[:, :, n * 512:(n + 1) * 512], w=[w_.k])
                for k in range(8):
                    M.pe(lambda e, k=k: e.matmul(p_.t[:, :], lhsT=self.scT.t[:, k, :], rhs=w_.t[:, k, :], start=(k == 0), stop=False),
                         r=[self.scT.k, w_.k], w=[p_.k])
                M.pe(lambda e: e.matmul(p_.t[:, :], lhsT=self.ones.t[0:1, 0:2], rhs=brow.t[0:1, n * 512:(n + 1) * 512], start=False, stop=True),
                     r=[self.ones.k, brow.k], w=[p_.k])
                M.act(lambda e: e.copy(out=mrow.t[:, n * 512:(n + 1) * 512], in_=p_.t[:, :]), r=[p_.k], w=[mrow.k])
            M.dma("sp", self.mods.t[l], mrow.t[:], r=[mrow.k], w=[self.mods.k])
            M.barrier()

    def modrow(self, l, which, idx):
        return self.mods.t[l, which:which + 1, idx * D:(idx + 1) * D]

    def norm_mod_tiles(self, es, l, gain_name, i_sc, i_sh):
        M, I = self.M, self.I
        gb = self.sb(es, "gb", [128, D])
        self.bload("sp", gb, I[gain_name][l:l + 1, :], D)
        A, S = [], []
        for which in range(2):
            a = self.sb(es, f"A{which}", [128, D])
            s = self.sb(es, f"S{which}", [128, D])
            M.dma("sp", a.t[:], self.modrow(l, which, i_sc).partition_broadcast(128), r=[self.mods.k], w=[a.k])
            M.dma("sp", s.t[:], self.modrow(l, which, i_sh).partition_broadcast(128), r=[self.mods.k], w=[s.k])
            M.dve(lambda e, a=a: e.scalar_tensor_tensor(out=a.t[:], in0=a.t[:], scalar=1.0, in1=gb.t[:], op0=ALU.add, op1=ALU.mult),
                  r=[a.k, gb.k], w=[a.k])
            A.append(a)
            S.append(s)
        return A, S

    def rms_mod(self, xt, A, S, sq, ss, hf, hout):
        M = self.M
        M.act(lambda e: e.activation(out=sq.t[:], in_=xt.t[:], func=AF.Square, accum_out=ss.t[:]), r=[xt.k], w=[sq.k, ss.k])
        M.dve(lambda e: e.tensor_scalar(out=ss.t[:], in0=ss.t[:], scalar1=1.0 / D, scalar2=EPS, op0=ALU.mult, op1=ALU.add), r=[ss.k], w=[ss.k])
        M.act(lambda e: e.activation(out=ss.t[:], in_=ss.t[:], func=AF.Sqrt), r=[ss.k], w=[ss.k])
        M.dve(lambda e: e.reciprocal(out=ss.t[:], in_=ss.t[:]), r=[ss.k], w=[ss.k])
        M.dve(lambda e: e.scalar_tensor_tensor(out=hf.t[:], in0=xt.t[:], scalar=ss.t[:, 0:1], in1=A.t[:], op0=ALU.mult, op1=ALU.mult),
              r=[xt.k, ss.k, A.k], w=[hf.k])
        M.dve(lambda e: e.tensor_tensor(out=hout.t[:], in0=hf.t[:], in1=S.t[:], op=ALU.add), r=[hf.k, S.k], w=[hout.k])

    def stage_proj(self, l, lrT):
        from contextlib import ExitStack
        M, nc, I = self.M, self.nc, self.I
        with ExitStack() as es:
            hT = self.sb(es, "hT", [128, 8, TALL], BF16)
            sT = self.sb(es, "sT", [128, 8, TALL], BF16)
            wbuf = self.sb(es, "wbuf", [128, 8, 2304], BF16)
            wbufC = self.sb(es, "wbufC", [128, 8, 1536], BF16)
            with ExitStack() as e2:
                A, S = self.norm_mod_tiles(e2, l, "norm1_gain", 1, 0)
                xt = [self.sb(e2, f"xt{i}", [128, D]) for i in range(2)]
                sq = self.sb(e2, "sq", [128, D])
                hf = self.sb(e2, "hf", [128, D])
                hb = [self.sb(e2, f"hb{i}", [128, D], BF16) for i in range(2)]
                ss = [self.sb(e2, f"ss{i}", [128, 1]) for i in range(2)]
                ptr = [self.ps(e2, f"ptr{i}", [128, 8, 128], BF16) for i in range(2)]
                wv = I["w_in"][l].rearrange("(k p) n -> p k n", p=128)
                for k in range(8):
                    M.dma("pool", wbuf.t[:, k, 0:1280], wv[:, k, 0:1280], w=[wbuf.k])
                for k in range(8):
                    M.dma("pool", wbufC.t[:, k, 0:1536], wv[:, k, 2432:3968], w=[wbufC.k])

                def shift_tile(jj):
                    a, b_ = (0, 256) if jj < 2 else (256, TALL)
                    c0, c1 = jj * 128, (jj + 1) * 128
                    lo = max(c0, a + 1)
                    if c0 == a:
                        M.dve(lambda e: e.memset(sT.t[:, :, a:a + 1], 0.0), w=[sT.k])
                    M.dve(lambda e: e.tensor_scalar(out=sT.t[:, :, lo:c1], in0=hT.t[:, :, lo - 1:c1 - 1], scalar1=0.5, scalar2=None, op0=ALU.mult),
                          r=[hT.k], w=[sT.k])
                    hi = min(c1, b_ - 1)
                    M.dve(lambda e: e.scalar_tensor_tensor(out=sT.t[:, :, c0:hi], in0=hT.t[:, :, c0 + 1:hi + 1], scalar=0.5, in1=sT.t[:, :, c0:hi],
                                                           op0=ALU.mult, op1=ALU.add), r=[hT.k, sT.k], w=[sT.k])

                for j in range(NT):
                    x_ = xt[j % 2]
                    which = 1 if j < 2 else 0
                    if l == 0:
                        M.dma("sp", x_.t[:], I["x_all"][j * 128:(j + 1) * 128, :], w=[x_.k])
                    else:
                        M.dma("sp", x_.t[:], self.xs.t[j * 128:(j + 1) * 128, :], r=[self.xs.k], w=[x_.k])
                    self.rms_mod(x_, A[which], S[which], sq, ss[j % 2], hf, hb[j % 2])
                    p_ = ptr[j % 2]
                    for k in range(8):
                        M.pe(lambda e, k=k: e.transpose(p_.t[:, k, :], hb[j % 2].t[:, k * 128:(k + 1) * 128], self.idb.t[:]),
                             r=[hb[j % 2].k, self.idb.k], w=[p_.k])
                    M.act(lambda e: e.copy(out=hT.t[:, :, j * 128:(j + 1) * 128], in_=p_.t[:, :, :]), r=[p_.k], w=[hT.k])
                    if j >= 1:
                        shift_tile(j - 1)
                shift_tile(NT - 1)
                M.barrier()
            pp = [self.ps(es, f"pp{i}", [128, 512]) for i in range(4)]
            ob = [self.sb(es, f"ob{i}", [128, 512]) for i in range(4)]
            st = [self.sb(es, f"wst{i}", [128, 1152]) for i in range(2)]
            mub = self.sb(es, "mub", [128, 1152])
            omub = self.sb(es, "omub", [128, 1152])
            cnt = [0]

            def tok_major(col0, wcols, ncols, dual, wbuf=wbuf):
                for j in range(NT):
                    for c0 in range(0, ncols, 512):
                        c1 = min(ncols, c0 + 512)
                        i = cnt[0] % 4
                        cnt[0] += 1
                        p_, o_ = pp[i], ob[i]
                        for k in range(8):
                            M.pe(lambda e, k=k: e.matmul(p_.t[:, 0:c1 - c0], lhsT=hT.t[:, k, j * 128:(j + 1) * 128], rhs=wbuf.t[:, k, wcols + c0:wcols + c1],
                                                         start=(k == 0), stop=(k == 7 and not dual)), r=[hT.k, wbuf.k], w=[p_.k])
                        if dual:
                            for k in range(8):
                                M.pe(lambda e, k=k: e.matmul(p_.t[:, 0:c1 - c0], lhsT=sT.t[:, k, j * 128:(j + 1) * 128],
                                                             rhs=wbuf.t[:, k, 1152 + wcols + c0:1152 + wcols + c1], start=False, stop=(k == 7)),
                                     r=[sT.k, wbuf.k], w=[p_.k])
                        M.act(lambda e: e.copy(out=o_.t[:, 0:c1 - c0], in_=p_.t[:, 0:c1 - c0]), r=[p_.k], w=[o_.k])
                        M.dma("sp", self.ptm.t[j * 128:(j + 1) * 128, col0 + c0:col0 + c1], o_.t[:, 0:c1 - c0], r=[o_.k], w=[self.ptm.k])

            tok_major(0, 0, 1280, False)
            self.bload("sp", mub, I["rwkv_mu"][l:l + 1, :], 1152)
            M.dve(lambda e: e.tensor_scalar(out=omub.t[:], in0=mub.t[:], scalar1=-1.0, scalar2=1.0, op0=ALU.mult, op1=ALU.add), r=[mub.k], w=[omub.k])
            for k in range(8):
                s_ = st[k % 2]
                M.dma("sp", s_.t[:], I["w_in"][l, k * 128:(k + 1) * 128, 1280:2432], w=[s_.k])
                M.dve(lambda e, k=k: e.tensor_tensor(out=wbuf.t[:, k, 0:1152], in0=s_.t[:], in1=omub.t[:], op=ALU.mult), r=[s_.k, omub.k], w=[wbuf.k])
                M.dve(lambda e, k=k: e.tensor_tensor(out=wbuf.t[:, k, 1152:2304], in0=s_.t[:], in1=mub.t[:], op=ALU.mult), r=[s_.k, mub.k], w=[wbuf.k])
            tok_major(2432, 0, 1536, False, wbufC)
            tok_major(1280, 0, 768, True)
            for m in range(3):
                for t0 in range(0, TALL, 512):
                    t1 = min(TALL, t0 + 512)
                    i = cnt[0] % 4
                    cnt[0] += 1
                    p_ = pp[i]
                    wc = 768 + m * 128
                    for k in range(8):
                        M.pe(lambda e, k=k: e.matmul(p_.t[:, 0:t1 - t0], lhsT=wbuf.t[:, k, wc:wc + 128], rhs=hT.t[:, k, t0:t1], start=(k == 0), stop=False),
                             r=[hT.k, wbuf.k], w=[p_.k])
                    for k in range(8):
                        M.pe(lambda e, k=k: e.matmul(p_.t[:, 0:t1 - t0], lhsT=wbuf.t[:, k, 1152 + wc:1152 + wc + 128], rhs=sT.t[:, k, t0:t1], start=False, stop=(k == 7)),
                             r=[sT.k, wbuf.k], w=[p_.k])
                    M.act(lambda e: e.copy(out=lrT.t[:, m, t0:t1], in_=p_.t[:, 0:t1 - t0]), r=[p_.k], w=[lrT.k])
            M.barrier()


def _rope_table():
    t = np.arange(2048)
    inv = (10000.0 ** (-np.arange(16, dtype=np.float32) / 16)).astype(np.float32)
    ang = np.concatenate([(t // 64).astype(np.float32)[:, None] * inv, (t % 64).astype(np.float32)[:, None] * inv], -1)
    cs = np.zeros((TALL, 64), np.float32)
    cs[:256, 0:32] = 1.0
    cs[256:, 0:32] = np.cos(ang)
    cs[256:, 32:64] = np.sin(ang)
    return cs


def _rpb_table(rpb):
    L = rpb.shape[0]
    a = np.arange(2)[:, None, None, None]
    kc = np.arange(64)[None, :, None, None]
    dr = np.arange(14)[None, None, :, None]
    c = np.arange(64)[None, None, None, :]
    ws = np.clip(c - 8, 0, 48)
    inw = (kc >= ws) & (kc < ws + 16)
    ci = np.clip(kc - c + 15, 0, 30)
    ri = dr + a
    out = np.empty((L, 2, 64, 8, 14, 64), np.float32)
    for h in range(8):
        g = rpb[:, h][:, np.broadcast_to(ri, (2, 64, 14, 64)), np.broadcast_to(ci, (2, 64, 14, 64))]
        out[:, :, :, h] = np.where(np.broadcast_to(inw, (2, 64, 14, 64))[None], g, np.float32(NEGB))
    return np.ascontiguousarray(out.reshape(L, 128, 8 * 14 * 64))


def prep_shared(inp, NL=4, moe_in=True):
    f = lambda a: np.ascontiguousarray(np.asarray(a, dtype=np.float32))
    sh = {
        "consts": make_consts(),
        "norm1_gain": f(inp["norm1_gain"]), "norm2_gain": f(inp["norm2_gain"]), "w_ada": f(inp["w_ada"]), "b_ada": f(inp["b_ada"]),
        "w_in": f(inp["w_in"]), "w_out": f(inp["w_out"]), "hgrn_lb_logits": f(inp["hgrn_lb_logits"]),
        "hgrn_gn_gain": f(inp["hgrn_gn_gain"]).reshape(4, 256),
        "rwkv_mu": f(inp["rwkv_mu"]), "rwkv_w0": f(inp["rwkv_w0"]), "rwkv_w_up": f(inp["rwkv_w_up"]), "rwkv_a0": f(inp["rwkv_a0"]),
        "rwkv_a_up": f(inp["rwkv_a_up"]), "rwkv_g_up": f(inp["rwkv_g_up"]), "rwkv_kk_scale": f(inp["rwkv_kk_scale"]),
        "rwkv_k_a": f(inp["rwkv_k_a"]), "rwkv_r_k": f(inp["rwkv_r_k"]).reshape(4, 256), "rwkv_ln_gain": f(inp["rwkv_ln_gain"]),
        "rwkv_ln_bias": f(inp["rwkv_ln_bias"]), "na_q_gain": f(inp["na_q_gain"]), "na_k_gain": f(inp["na_k_gain"]),
        "rpbT": _rpb_table(f(inp["na_rpb"])), "rope_cs": _rope_table(),
        "w_router": np.ascontiguousarray(np.concatenate([f(inp["w_router_group"]), f(inp["w_router_expert"])], -1)),
        "b_router": np.ascontiguousarray(np.concatenate([f(inp["b_router_group"]), f(inp["b_router_expert"])], -1)),
        "w_exp_gate": f(inp["w_exp_gate"]).reshape(4, 32, D, 512), "w_exp_up": f(inp["w_exp_up"]).reshape(4, 32, D, 512),
        "w_exp_down": f(inp["w_exp_down"]).reshape(4, 32, 512, D),
    }
    for k in list(sh.keys()):
        if sh[k].shape[0] == 4 and k != "consts" and NL < 4:
            sh[k] = np.ascontiguousarray(sh[k][:NL])
        if k.startswith("w_exp") and not moe_in:
            sh[k] = np.zeros((1, 1, 128, 128), np.float32)
    return sh


def prep_core(inp, b, shared):
    d = dict(shared)
    d["x_all"] = np.ascontiguousarray(np.concatenate([np.asarray(inp["ctx"][b], np.float32), np.asarray(inp["x"][b], np.float32)], 0))
    d["c2"] = np.ascontiguousarray(np.stack([np.asarray(inp["c"][b], np.float32), np.asarray(inp["c_ctx"], np.float32)], 1))
    return d


def _bc(ap, shape, axis):
    return ap.unsqueeze(axis).to_broadcast(list(shape))


class ProgMix(Prog):
    def stage_hgrn(self, l):
        from contextlib import ExitStack
        M, nc, I = self.M, self.nc, self.I
        cst = self.cst
        with ExitStack() as es:
            lb = [self.sb(es, f"lb{d}", [128, 256]) for d in range(2)]
            oml = [self.sb(es, f"oml{d}", [128, 256]) for d in range(2)]
            for d in range(2):
                M.dma("sp", lb[d].t[:], self.lbs.t[d, l:l + 1, :].partition_broadcast(128), r=[self.lbs.k], w=[lb[d].k])
                M.dve(lambda e: e.tensor_scalar(out=oml[d].t[:], in0=lb[d].t[:], scalar1=-1.0, scalar2=1.0, op0=ALU.mult, op1=ALU.add),
                      r=[lb[d].k], w=[oml[d].k])
            gnb = self.sb(es, "gnb", [128, 256])
            self.bload("sp", gnb, I["hgrn_gn_gain"][l:l + 1, :], 256)
            ofw = self.sb(es, "ofw", [128, NT, 256])
            Sr = [self.sb(es, f"S{i}", [64, 4, 64]) for i in range(4)]
            NB = 2
            q_ = [self.sb(es, f"q{i}", [128, 256]) for i in range(NB)]
            f_ = [self.sb(es, f"f{i}", [128, 256]) for i in range(NB)]
            v_ = [self.sb(es, f"v{i}", [128, 256]) for i in range(NB)]
            g_ = [self.sb(es, f"g{i}", [128, 256]) for i in range(NB)]
            kk = self.sb(es, "kk", [128, 256])
            ec = self.sb(es, "ec", [128, 256])
            en = self.sb(es, "en", [128, 256])
            er = self.sb(es, "er", [128, 256])
            qt = self.sb(es, "qt", [128, 256])
            kt = self.sb(es, "kt", [128, 256])
            kh = self.sb(es, "kh", [128, 256])
            khm = self.sb(es, "khm", [128, 4, 256])
            qtT = self.sb(es, "qtT", [64, 4, 128])
            ktT = self.sb(es, "ktT", [64, 4, 128])
            ecT = self.sb(es, "ecT", [64, 4, 128])
            qtTm = self.sb(es, "qtTm", [64, 4, 4, 128])
            scm = self.sb(es, "scm", [128, 4, 128])
            osum = self.sb(es, "osum", [128, 256])
            sq = self.sb(es, "sqh", [128, 256])
            st4 = self.sb(es, "st4", [128, 4])
            mo = [self.sb(es, f"mo{i}", [128, 256], BF16) for i in range(2)]
            pcr = self.ps(es, "pcr", [128, 512])
            pT = [self.ps(es, f"pT{i}", [64, 4, 128]) for i in range(3)]
            psc = self.ps(es, "psc", [128, 4, 128])
            pkv = [self.ps(es, f"pkv{i}", [64, 8, 64]) for i in range(2)]
            po = self.ps(es, "po", [128, 4, 64])
            C = cst.t
            it = 0
            for d in range(2):
                CUM = [C_U32, C_L32][d]
                REM = [C_SL32, C_SU32][d]
                fcol = 256 + 256 * d
                order = list(range(NT)) if d == 0 else [1, 0] + list(range(NT - 1, 1, -1))
                chunks = [0, 1, 2, 3] if d == 0 else [3, 2, 1, 0]
                si = 0
                S = Sr[0]
                M.dve(lambda e: e.memset(S.t[:], 0.0), w=[S.k])
                for j in order:
                    b = it % NB
                    it += 1
                    rows = slice(j * 128, (j + 1) * 128)
                    q, f, v = q_[b], f_[b], v_[b]
                    M.dma("sp", q.t[:], self.ptm.t[rows, 0:256], r=[self.ptm.k], w=[q.k])
                    M.dma("sp", f.t[:], self.ptm.t[rows, fcol:fcol + 256], r=[self.ptm.k], w=[f.k])
                    M.dma("sp", v.t[:], self.ptm.t[rows, 768:1024], r=[self.ptm.k], w=[v.k])
                    if d == 1:
                        M.dma("sp", g_[b].t[:], self.ptm.t[rows, 1024:1280], r=[self.ptm.k], w=[g_[b].k])
                    M.act(lambda e: e.activation(out=f.t[:], in_=f.t[:], func=AF.Sigmoid), r=[f.k], w=[f.k])
                    M.dve(lambda e: e.tensor_tensor(out=f.t[:], in0=f.t[:], in1=oml[d].t[:], op=ALU.mult), r=[f.k, oml[d].k], w=[f.k])
                    M.dve(lambda e: e.tensor_tensor(out=f.t[:], in0=f.t[:], in1=lb[d].t[:], op=ALU.add), r=[f.k, lb[d].k], w=[f.k])
                    M.dve(lambda e: e.tensor_scalar(out=kk.t[:], in0=f.t[:], scalar1=-1.0, scalar2=1.0, op0=ALU.mult, op1=ALU.add), r=[f.k], w=[kk.k])
                    M.dve(lambda e: e.tensor_scalar(out=f.t[:], in0=f.t[:], scalar1=1e-20, scalar2=None, op0=ALU.max), r=[f.k], w=[f.k])
                    M.act(lambda e: e.activation(out=f.t[:], in_=f.t[:], func=AF.Ln), r=[f.k], w=[f.k])
                    M.pe(lambda e: e.matmul(pcr.t[:, 0:256], lhsT=C[:, CUM:CUM + 128], rhs=f.t[:], start=True, stop=True), r=[cst.k, f.k], w=[pcr.k])
                    M.pe(lambda e: e.matmul(pcr.t[:, 256:512], lhsT=C[:, REM:REM + 128], rhs=f.t[:], start=True, stop=True), r=[cst.k, f.k], w=[pcr.k])
                    M.act(lambda e: e.activation(out=ec.t[:], in_=pcr.t[:, 0:256], func=AF.Exp), r=[pcr.k], w=[ec.k])
                    M.act(lambda e: e.activation(out=en.t[:], in_=pcr.t[:, 0:256], func=AF.Exp, scale=-1.0), r=[pcr.k], w=[en.k])
                    M.act(lambda e: e.activation(out=er.t[:], in_=pcr.t[:, 256:512], func=AF.Exp), r=[pcr.k], w=[er.k])
                    M.dve(lambda e: e.tensor_tensor(out=qt.t[:], in0=q.t[:], in1=ec.t[:], op=ALU.mult), r=[q.k, ec.k], w=[qt.k])
                    M.dve(lambda e: e.tensor_tensor(out=kt.t[:], in0=kk.t[:], in1=en.t[:], op=ALU.mult), r=[kk.k, en.k], w=[kt.k])
                    M.dve(lambda e: e.tensor_tensor(out=kh.t[:], in0=kk.t[:], in1=er.t[:], op=ALU.mult), r=[kk.k, er.k], w=[kh.k])
                    for c in range(4):
                        M.dve(lambda e: e.tensor_scalar(out=khm.t[:, c, :], in0=kh.t[:], scalar1=C[:, C_CM32 + c:C_CM32 + c + 1], scalar2=None, op0=ALU.mult),
                              r=[kh.k, cst.k], w=[khm.k])
                    for (src, pt_, dst) in ((qt, pT[0], qtT), (kt, pT[1], ktT), (ec, pT[2], ecT)):
                        for h in range(4):
                            M.pe(lambda e: e.transpose(pt_.t[:, h, :], src.t[:, h * 64:(h + 1) * 64], C[:, C_ID:C_ID + 128]), r=[src.k, cst.k], w=[pt_.k])
                        M.act(lambda e: e.copy(out=dst.t[:], in_=pt_.t[:]), r=[pt_.k], w=[dst.k])
                    for c in range(4):
                        M.dve(lambda e: e.tensor_tensor(out=qtTm.t[:, c, :, :], in0=qtT.t[:], in1=_bc(C[0:64, C_COL32 + c * 128:C_COL32 + (c + 1) * 128], [64, 4, 128], 1),
                                                        op=ALU.mult), r=[qtT.k, cst.k], w=[qtTm.k])
                    for h in range(4):
                        M.pe(lambda e: e.matmul(psc.t[:, h, :], lhsT=ktT.t[:, h, :], rhs=qtT.t[:, h, :], start=True, stop=True), r=[ktT.k, qtT.k], w=[psc.k])
                    M.dve(lambda e: e.tensor_tensor(out=scm.t[:], in0=psc.t[:], in1=_bc(C[:, CUM:CUM + 128], [128, 4, 128], 1), op=ALU.mult),
                          r=[psc.k, cst.k], w=[scm.k])
                    for c in range(4):
                        for h in range(4):
                            M.pe(lambda e: e.matmul(pkv[c // 2].t[:, (c % 2) * 4 + h, :], lhsT=khm.t[:, c, h * 64:(h + 1) * 64], rhs=v.t[:, h * 64:(h + 1) * 64],
                                                    start=True, stop=True), r=[khm.k, v.k], w=[pkv[c // 2].k])
                    for h in range(4):
                        M.pe(lambda e: e.matmul(po.t[:, h, :], lhsT=scm.t[:, h, :], rhs=v.t[:, h * 64:(h + 1) * 64], start=(h == 0), stop=False, skip_group_check=True), r=[scm.k, v.k], w=[po.k])
                    for ci, c in enumerate(chunks):
                        for h in range(4):
                            M.pe(lambda e: e.matmul(po.t[:, h, :], lhsT=qtTm.t[:, c, h, :], rhs=S.t[:, h, :], start=False, stop=(ci == 3), skip_group_check=True),
                                 r=[qtTm.k, S.k], w=[po.k])
                        si = (si + 1) % 4
                        Sn = Sr[si]
                        col = 32 * c + 31 if d == 0 else 32 * c
                        M.dve(lambda e: e.tensor_tensor(out=Sn.t[:], in0=S.t[:], in1=ecT.t[:, :, col:col + 1].to_broadcast([64, 4, 64]), op=ALU.mult),
                              r=[S.k, ecT.k], w=[Sn.k])
                        M.dve(lambda e: e.tensor_tensor(out=Sn.t[:], in0=Sn.t[:], in1=pkv[c // 2].t[:, (c % 2) * 4:(c % 2) * 4 + 4, :], op=ALU.add),
                              r=[Sn.k, pkv[c // 2].k], w=[Sn.k])
                        S = Sn
                    if d == 0 and j == 0:
                        for nm_, b_ in (("d_logf", f), ("d_ec", ec), ("d_qt", qt), ("d_kt", kt), ("d_kh", kh), ("d_qtT", qtT), ("d_ktT", ktT), ("d_ecT", ecT),
                                        ("d_scm", scm), ("d_S", S), ("d_khm", khm), ("d_qtTm", qtTm)):
                            self.dump(nm_, b_)
                    if d == 0:
                        M.act(lambda e: e.copy(out=ofw.t[:, j, :], in_=po.t[:].rearrange("p h d -> p (h d)")), r=[po.k], w=[ofw.k])
                        if j == 0:
                            self.dump("d_o0", ofw, ofw.t[:, 0, :])
                    else:
                        g = g_[b]
                        m_ = mo[it % 2]
                        M.dve(lambda e: e.tensor_tensor(out=osum.t[:], in0=po.t[:].rearrange("p h d -> p (h d)"), in1=ofw.t[:, j, :], op=ALU.add),
                              r=[po.k, ofw.k], w=[osum.k])
                        M.act(lambda e: e.activation(out=sq.t[:], in_=osum.t[:], func=AF.Square), r=[osum.k], w=[sq.k])
                        M.dve(lambda e: e.tensor_reduce(out=st4.t[:], in_=sq.t[:].rearrange("p (h d) -> p h d", h=4), axis=AX.X, op=ALU.add), r=[sq.k], w=[st4.k])
                        M.dve(lambda e: e.tensor_scalar(out=st4.t[:], in0=st4.t[:], scalar1=1.0 / 64, scalar2=EPS, op0=ALU.mult, op1=ALU.add), r=[st4.k], w=[st4.k])
                        M.act(lambda e: e.activation(out=st4.t[:], in_=st4.t[:], func=AF.Sqrt), r=[st4.k], w=[st4.k])
                        M.dve(lambda e: e.reciprocal(out=st4.t[:], in_=st4.t[:]), r=[st4.k], w=[st4.k])
                        M.dve(lambda e: e.tensor_tensor(out=osum.t[:].rearrange("p (h d) -> p h d", h=4), in0=osum.t[:].rearrange("p (h d) -> p h d", h=4),
                                                        in1=_bc(st4.t[:], [128, 4, 64], 2), op=ALU.mult), r=[osum.k, st4.k], w=[osum.k])
                        M.dve(lambda e: e.tensor_tensor(out=osum.t[:], in0=osum.t[:], in1=gnb.t[:], op=ALU.mult), r=[osum.k, gnb.k], w=[osum.k])
                        M.act(lambda e: e.activation(out=g.t[:], in_=g.t[:], func=AF.Silu), r=[g.k], w=[g.k])
                        M.dve(lambda e: e.tensor_tensor(out=m_.t[:], in0=osum.t[:], in1=g.t[:], op=ALU.mult), r=[osum.k, g.k], w=[m_.k])
                        M.dma("pool", self.mixs.t[rows, 0:256], m_.t[:], r=[m_.k], w=[self.mixs.k])
            M.barrier()


RWKV_LN_EPS = 64e-5
WSCALE = -float(np.exp(-0.5))


class ProgMix2(ProgMix):
    def stage_rwkv(self, l, lrT):
        from contextlib import ExitStack
        M, nc, I = self.M, self.nc, self.I
        cst = self.cst
        C = cst.t
        with ExitStack() as es:
            wup = self.sb(es, "wup", [128, 256])
            aup = self.sb(es, "aup", [128, 256])
            gup = self.sb(es, "gup", [128, 256])
            brow = self.sb(es, "browr", [1, 4, 256])
            M.dma("sp", wup.t[:], I["rwkv_w_up"][l].rearrange("d r n -> (d r) n"), w=[wup.k])
            M.dma("sp", aup.t[:], I["rwkv_a_up"][l].rearrange("d r n -> (d r) n"), w=[aup.k])
            M.dma("sp", gup.t[:], I["rwkv_g_up"][l], w=[gup.k])
            M.dma("sp", brow.t[0:1, 0:2, :], I["rwkv_w0"][l:l + 1, :, :], w=[brow.k])
            M.dma("sp", brow.t[0:1, 2:4, :], I["rwkv_a0"][l:l + 1, :, :], w=[brow.k])
            bt = {}
            for nm in ("rwkv_kk_scale", "rwkv_k_a", "rwkv_r_k", "rwkv_ln_gain", "rwkv_ln_bias"):
                bt[nm] = self.sb(es, nm, [64, 256])
                M.dma("sp", bt[nm].t[:], I[nm][l:l + 1, :].partition_broadcast(64), w=[bt[nm].k])
            kab = bt["rwkv_k_a"]
            omka = self.sb(es, "omka", [64, 256])
            M.dve(lambda e: e.tensor_scalar(out=omka.t[:], in0=kab.t[:], scalar1=-1.0, scalar2=1.0, op0=ALU.mult, op1=ALU.add), r=[kab.k], w=[omka.k])
            yfw = self.sb(es, "yfw", [64, 36, 256])
            Sr = [self.sb(es, f"RS{i}", [64, 4, 64]) for i in range(3)]
            r_ = [self.sb(es, f"r{i}", [64, 256]) for i in range(2)]
            k_ = [self.sb(es, f"k{i}", [64, 256]) for i in range(2)]
            v_ = [self.sb(es, f"v{i}", [64, 256]) for i in range(2)]
            tw = self.sb(es, "tw", [128, 64])
            sz = self.sb(es, "sz", [64, 512])
            lw = self.sb(es, "lw", [64, 256])
            kk = self.sb(es, "kk", [64, 256])
            sq = self.sb(es, "sq", [64, 256])
            s4 = self.sb(es, "s4", [64, 4])
            t1 = self.sb(es, "t1", [64, 256])
            kd = self.sb(es, "kd", [64, 256])
            ak = self.sb(es, "ak", [64, 256])
            ecx = self.sb(es, "ecx", [64, 512])
            en = self.sb(es, "en", [64, 256])
            er = self.sb(es, "er", [64, 256])
            rt = self.sb(es, "rt", [64, 256])
            kkt = self.sb(es, "kkt", [64, 256])
            akt = self.sb(es, "akt", [64, 256])
            kdt = self.sb(es, "kdt", [64, 256])
            akh = self.sb(es, "akh", [64, 256])
            kdh = self.sb(es, "kdh", [64, 256])
            X2 = self.sb(es, "X2", [64, 4, 2, 64])
            aktT = self.sb(es, "aktT", [64, 4, 64])
            kdtT = self.sb(es, "kdtT", [64, 4, 64])
            ecT = self.sb(es, "ecT", [64, 4, 64])
            NTm = self.sb(es, "NTm", [64, 4, 64])
            BakT = self.sb(es, "BakT", [64, 4, 64])
            AkT = self.sb(es, "AkT", [64, 4, 64])
            BkT = self.sb(es, "BkT", [64, 4, 64])
            Xa = [self.sb(es, f"Xa{i}", [64, 4, 64]) for i in range(2)]
            XTa = [self.sb(es, f"XTa{i}", [64, 4, 64]) for i in range(2)]
            PT = [self.sb(es, f"PT{i}", [64, 4, 64]) for i in range(2)]
            Wn = self.sb(es, "Wn", [64, 4, 64])
            U = self.sb(es, "U", [64, 4, 64])
            ysum = self.sb(es, "ysum", [64, 256])
            sgd = self.sb(es, "sgd", [128, 64])
            af = self.sb(es, "af", [64, 256])
            bon = self.sb(es, "bon", [64, 256])
            mo = [self.sb(es, f"rmo{i}", [64, 256], BF16) for i in range(2)]
            pz = self.ps(es, "pz", [64, 512])
            pc0 = self.ps(es, "pc0", [64, 512])
            pc1 = self.ps(es, "pc1", [64, 512])
            pT = [self.ps(es, f"rpT{i}", [64, 4, 64]) for i in range(2)]
            pg = [self.ps(es, f"pg{i}", [64, 4, 64]) for i in range(3)]
            pYb = B_(pc1.t[:, 256:512].rearrange("p (h d) -> p h d", h=4), "pY")
            gi = [0]

            def nextpg():
                gi[0] = (gi[0] + 1) % 3
                return pg[gi[0]]

            it = 0
            for d in range(2 if getattr(self, 'rw_stop', 99) >= 9 else 1):
                CUM, SCUM, REM = [(C_U64, C_SU64, C_SL64), (C_L64, C_SL64, C_SU64)][d]
                MSTR, MINC, MSTRT = [(C_SU64, C_U64, C_SL64), (C_SL64, C_L64, C_SU64)][d]
                order = list(range(36)) if d == 0 else [3, 2, 1, 0] + list(range(35, 3, -1))
                chunks = [0]
                P0 = 64 * d
                si = 0
                S = Sr[0]
                M.dve(lambda e: e.memset(S.t[:], 0.0), w=[S.k])
                for j in order:
                    b = it % 2
                    it += 1
                    rows = slice(j * 64, (j + 1) * 64)
                    tok = slice(j * 64, (j + 1) * 64)
                    r, k, v = r_[b], k_[b], v_[b]
                    M.dma("sp", r.t[:], self.ptm.t[rows, 1280:1536], r=[self.ptm.k], w=[r.k])
                    M.dma("sp", k.t[:], self.ptm.t[rows, 1536:1792], r=[self.ptm.k], w=[k.k])
                    M.dma("sp", v.t[:], self.ptm.t[rows, 1792:2048], r=[self.ptm.k], w=[v.k])
                    M.act(lambda e: e.activation(out=tw.t[P0:P0 + 64, :], in_=lrT.t[P0:P0 + 64, 0, tok], func=AF.Tanh), r=[lrT.k], w=[tw.k])
                    M.pe(lambda e: e.matmul(pz.t[:, 0:256], lhsT=tw.t[P0:P0 + 64, :], rhs=wup.t[P0:P0 + 64, :], start=True, stop=False, skip_group_check=True),
                         r=[tw.k, wup.k], w=[pz.k])
                    M.pe(lambda e: e.matmul(pz.t[:, 0:256], lhsT=self.ones.t[0:1, 0:64], rhs=brow.t[0:1, d, :], start=False, stop=False, skip_group_check=True),
                         r=[self.ones.k, brow.k], w=[pz.k])
                    M.pe(lambda e: e.matmul(pz.t[:, 256:512], lhsT=lrT.t[P0:P0 + 64, 1, tok], rhs=aup.t[P0:P0 + 64, :], start=False, stop=False, skip_group_check=True),
                         r=[lrT.k, aup.k], w=[pz.k])
                    M.pe(lambda e: e.matmul(pz.t[:, 256:512], lhsT=self.ones.t[0:1, 0:64], rhs=brow.t[0:1, 2 + d, :], start=False, stop=True, skip_group_check=True),
                         r=[self.ones.k, brow.k], w=[pz.k])
                    M.act(lambda e: e.activation(out=sz.t[:], in_=pz.t[:], func=AF.Sigmoid), r=[pz.k], w=[sz.k])
                    M.dve(lambda e: e.tensor_scalar(out=lw.t[:], in0=sz.t[:, 0:256], scalar1=WSCALE, scalar2=None, op0=ALU.mult), r=[sz.k], w=[lw.k])
                    if getattr(self, 'rw_stop', 99) == 1:
                        M.barrier()
                        return

                    M.dve(lambda e: e.tensor_tensor(out=kk.t[:], in0=k.t[:], in1=bt["rwkv_kk_scale"].t[:], op=ALU.mult), r=[k.k, bt["rwkv_kk_scale"].k], w=[kk.k])
                    M.act(lambda e: e.activation(out=sq.t[:], in_=kk.t[:], func=AF.Square), r=[kk.k], w=[sq.k])
                    M.dve(lambda e: e.tensor_reduce(out=s4.t[:], in_=sq.t[:].rearrange("p (h d) -> p h d", h=4), axis=AX.X, op=ALU.add), r=[sq.k], w=[s4.k])
                    M.act(lambda e: e.activation(out=s4.t[:], in_=s4.t[:], func=AF.Sqrt), r=[s4.k], w=[s4.k])
                    M.dve(lambda e: e.tensor_scalar(out=s4.t[:], in0=s4.t[:], scalar1=1e-12, scalar2=None, op0=ALU.max), r=[s4.k], w=[s4.k])
                    M.dve(lambda e: e.reciprocal(out=s4.t[:], in_=s4.t[:]), r=[s4.k], w=[s4.k])
                    M.dve(lambda e: e.tensor_tensor(out=kk.t[:].rearrange("p (h d) -> p h d", h=4), in0=kk.t[:].rearrange("p (h d) -> p h d", h=4),
                                                    in1=_bc(s4.t[:], [64, 4, 64], 2), op=ALU.mult), r=[kk.k, s4.k], w=[kk.k])
                    a_ = sz.t[:, 256:512]
                    M.dve(lambda e: e.tensor_tensor(out=t1.t[:], in0=a_, in1=kab.t[:], op=ALU.mult), r=[sz.k, kab.k], w=[t1.k])
                    M.dve(lambda e: e.tensor_tensor(out=t1.t[:], in0=t1.t[:], in1=omka.t[:], op=ALU.add), r=[t1.k, omka.k], w=[t1.k])
                    M.dve(lambda e: e.tensor_tensor(out=kd.t[:], in0=k.t[:], in1=t1.t[:], op=ALU.mult), r=[k.k, t1.k], w=[kd.k])
                    M.dve(lambda e: e.tensor_tensor(out=ak.t[:], in0=a_, in1=kk.t[:], op=ALU.mult), r=[sz.k, kk.k], w=[ak.k])
                    if getattr(self, 'rw_stop', 99) == 2:
                        M.barrier()
                        return

                    M.pe(lambda e: e.matmul(pc0.t[:, 0:256], lhsT=C[0:64, CUM:CUM + 64], rhs=lw.t[:], start=True, stop=True), r=[cst.k, lw.k], w=[pc0.k])
                    M.pe(lambda e: e.matmul(pc0.t[:, 256:512], lhsT=C[0:64, SCUM:SCUM + 64], rhs=lw.t[:], start=True, stop=True), r=[cst.k, lw.k], w=[pc0.k])
                    M.pe(lambda e: e.matmul(pc1.t[:, 0:256], lhsT=C[0:64, REM:REM + 64], rhs=lw.t[:], start=True, stop=True), r=[cst.k, lw.k], w=[pc1.k])
                    M.act(lambda e: e.activation(out=ecx.t[:], in_=pc0.t[:], func=AF.Exp), r=[pc0.k], w=[ecx.k])
                    M.act(lambda e: e.activation(out=en.t[:], in_=pc0.t[:, 0:256], func=AF.Exp, scale=-1.0), r=[pc0.k], w=[en.k])
                    M.act(lambda e: e.activation(out=er.t[:], in_=pc1.t[:, 0:256], func=AF.Exp), r=[pc1.k], w=[er.k])
                    for (o_, a0, a1) in ((rt, r.t[:], ecx.t[:, 0:256]), (kkt, kk.t[:], ecx.t[:, 256:512]), (akt, ak.t[:], en.t[:]), (kdt, kd.t[:], en.t[:]),
                                         (akh, ak.t[:], er.t[:]), (kdh, kd.t[:], er.t[:])):
                        M.dve(lambda e: e.tensor_tensor(out=o_.t[:], in0=a0, in1=a1, op=ALU.mult), r=[r.k, kk.k, ak.k, kd.k, ecx.k, en.k, er.k], w=[o_.k])
                    if getattr(self, 'rw_stop', 99) == 3:
                        M.barrier()
                        return

                    ti = 0
                    for (src, dst_ap, dstb) in ((kkt, X2.t[:, :, 0, :], X2), (rt, X2.t[:, :, 1, :], X2), (akt, aktT.t[:], aktT), (kdt, kdtT.t[:], kdtT),
                                                (ecx, ecT.t[:], ecT)):
                        p_ = pT[ti % 2]
                        ti += 1
                        for h in range(4):
                            M.pe(lambda e: e.transpose(p_.t[:, h, :], src.t[:, h * 64:(h + 1) * 64], C[0:64, C_ID:C_ID + 64]), r=[src.k, cst.k], w=[p_.k])
                        M.act(lambda e: e.copy(out=dst_ap, in_=p_.t[:]), r=[p_.k], w=[dstb.k])
                    if getattr(self, 'rw_stop', 99) == 4:
                        M.barrier()
                        return

                    for (lhs, ridx, msk, dst) in ((aktT, 0, MSTR, NTm), (aktT, 1, MINC, BakT), (kdtT, 0, MSTR, AkT), (kdtT, 1, MINC, BkT)):
                        p_ = nextpg()
                        for h in range(4):
                            M.pe(lambda e: e.matmul(p_.t[:, h, :], lhsT=lhs.t[:, h, :], rhs=X2.t[:, h, ridx, :], start=True, stop=True), r=[lhs.k, X2.k], w=[p_.k])
                        M.dve(lambda e: e.tensor_tensor(out=dst.t[:], in0=p_.t[:], in1=_bc(C[0:64, msk:msk + 64], [64, 4, 64], 1), op=ALU.mult),
                              r=[p_.k, cst.k], w=[dst.k])
                    p_ = nextpg()
                    for h in range(4):
                        M.pe(lambda e: e.matmul(p_.t[:, h, :], lhsT=X2.t[:, h, 0, :], rhs=aktT.t[:, h, :], start=True, stop=True), r=[aktT.k, X2.k], w=[p_.k])
                    X, XT, P_ = Xa[0], NTm, PT[0]
                    M.dve(lambda e: e.tensor_tensor(out=X.t[:], in0=p_.t[:], in1=_bc(C[0:64, MSTRT:MSTRT + 64], [64, 4, 64], 1), op=ALU.mult),
                          r=[p_.k, cst.k], w=[X.k])
                    if getattr(self, 'rw_stop', 99) == 5:
                        M.barrier()
                        return

                    M.dve(lambda e: e.scalar_tensor_tensor(out=P_.t[:], in0=XT.t[:], scalar=-1.0, in1=_bc(C[0:64, C_ID:C_ID + 64], [64, 4, 64], 1), op0=ALU.mult, op1=ALU.add),
                          r=[XT.k, cst.k], w=[P_.k])
                    for i in range(1, 6):
                        Xn = Xa[i % 2]
                        XTn = XTa[i % 2]
                        pX = nextpg()
                        for h in range(4):
                            M.pe(lambda e: e.matmul(pX.t[:, h, :], lhsT=XT.t[:, h, :], rhs=X.t[:, h, :], start=True, stop=True), r=[XT.k, X.k], w=[pX.k])
                        M.act(lambda e: e.copy(out=Xn.t[:], in_=pX.t[:]), r=[pX.k], w=[Xn.k])
                        if i < 5:
                            pXT = nextpg()
                            for h in range(4):
                                M.pe(lambda e: e.matmul(pXT.t[:, h, :], lhsT=X.t[:, h, :], rhs=XT.t[:, h, :], start=True, stop=True), r=[XT.k, X.k], w=[pXT.k])
                            M.act(lambda e: e.copy(out=XTn.t[:], in_=pXT.t[:]), r=[pXT.k], w=[XTn.k])
                        pP = nextpg()
                        for h in range(4):
                            M.pe(lambda e: e.matmul(pP.t[:, h, :], lhsT=Xn.t[:, h, :], rhs=P_.t[:, h, :], start=True, stop=True), r=[Xn.k, P_.k], w=[pP.k])
                        Pn = PT[i % 2]
                        M.dve(lambda e: e.tensor_tensor(out=Pn.t[:], in0=P_.t[:], in1=pP.t[:], op=ALU.add), r=[P_.k, pP.k], w=[Pn.k])
                        X, XT, P_ = Xn, XTn, Pn
                    if getattr(self, 'rw_stop', 99) == 6:
                        M.barrier()
                        return

                    pY = pYb
                    for ci, c in enumerate(chunks):
                        R = slice(64 * c, 64 * c + 64)
                        pW = nextpg()
                        for h in range(4):
                            hs = slice(h * 64, (h + 1) * 64)
                            M.pe(lambda e: e.matmul(pW.t[R, h, 0:64], lhsT=X2.t[:, h, 0, R], rhs=S.t[:, h, :], start=(h == 0), stop=False, skip_group_check=True),
                                 r=[X2.k, S.k], w=[pW.k])
                            M.pe(lambda e: e.matmul(pW.t[R, h, 0:64], lhsT=AkT.t[R, h, R], rhs=v.t[R, hs], start=False, stop=True, skip_group_check=True),
                                 r=[AkT.k, v.k], w=[pW.k])
                        M.act(lambda e: e.mul(out=Wn.t[R, :, :], in_=pW.t[R, :, 0:64], mul=-1.0), r=[pW.k], w=[Wn.k])
                        if getattr(self, 'rw_stop', 99) == 61 or (getattr(self, 'rw_stop', 99) == 65 and ci == 1):
                            M.barrier()
                            return

                        pU = nextpg()
                        for h in range(4):
                            M.pe(lambda e: e.matmul(pU.t[R, h, 0:64], lhsT=P_.t[R, h, R], rhs=Wn.t[R, h, :], start=True, stop=True), r=[P_.k, Wn.k], w=[pU.k])
                        M.act(lambda e: e.copy(out=U.t[R, :, :], in_=pU.t[R, :, 0:64]), r=[pU.k], w=[U.k])
                        if getattr(self, 'rw_stop', 99) == 62 or (getattr(self, 'rw_stop', 99) == 66 and ci == 1):
                            M.barrier()
                            return

                        for h in range(4):
                            hs = slice(h * 64, (h + 1) * 64)
                            M.pe(lambda e: e.matmul(pY.t[R, h, 0:64], lhsT=X2.t[:, h, 1, R], rhs=S.t[:, h, :], start=(h == 0), stop=False, skip_group_check=True),
                                 r=[X2.k, S.k], w=[pY.k])
                            M.pe(lambda e: e.matmul(pY.t[R, h, 0:64], lhsT=BakT.t[R, h, R], rhs=U.t[R, h, :], start=False, stop=False, skip_group_check=True),
                                 r=[BakT.k, U.k], w=[pY.k])
                            M.pe(lambda e: e.matmul(pY.t[R, h, 0:64], lhsT=BkT.t[R, h, R], rhs=v.t[R, hs], start=False, stop=True, skip_group_check=True),
                                 r=[BkT.k, v.k], w=[pY.k])
                        if getattr(self, 'rw_stop', 99) == 63 or (getattr(self, 'rw_stop', 99) == 67 and ci == 1):
                            M.barrier()
                            return

                        pS = nextpg()
                        for h in range(4):
                            hs = slice(h * 64, (h + 1) * 64)
                            M.pe(lambda e: e.matmul(pS.t[0:64, h, 0:64], lhsT=akh.t[R, hs], rhs=U.t[R, h, :], start=(h == 0), stop=False, skip_group_check=True),
                                 r=[akh.k, U.k], w=[pS.k])
                            M.pe(lambda e: e.matmul(pS.t[0:64, h, 0:64], lhsT=kdh.t[R, hs], rhs=v.t[R, hs], start=False, stop=True, skip_group_check=True),
                                 r=[kdh.k, v.k], w=[pS.k])
                        si = (si + 1) % 3
                        Sn = Sr[si]
                        col = 64 * c + 63 if d == 0 else 64 * c
                        M.dve(lambda e: e.tensor_tensor(out=Sn.t[:], in0=S.t[:], in1=ecT.t[:, :, col:col + 1].to_broadcast([64, 4, 64]), op=ALU.mult),
                              r=[S.k, ecT.k], w=[Sn.k])
                        M.dve(lambda e: e.tensor_tensor(out=Sn.t[:], in0=Sn.t[:], in1=pS.t[0:64, :, 0:64], op=ALU.add), r=[Sn.k, pS.k], w=[Sn.k])
                        if getattr(self, 'rw_stop', 99) == 64:
                            M.barrier()
                            return

                        S = Sn
                    if getattr(self, 'rw_stop', 99) == 7:
                        M.barrier()
                        return
                    if d == 0:
                        M.act(lambda e: e.copy(out=yfw.t[:, j, :].rearrange("p (h d) -> p h d", h=4), in_=pY.t[:, :, 0:64]), r=[pY.k], w=[yfw.k])
                    else:
                        m_ = mo[it % 2]
                        y3 = ysum.t[:].rearrange("p (h d) -> p h d", h=4)
                        M.dve(lambda e: e.tensor_tensor(out=y3, in0=pY.t[:, :, 0:64], in1=yfw.t[:, j, :].rearrange("p (h d) -> p h d", h=4), op=ALU.add),
                              r=[pY.k, yfw.k], w=[ysum.k])
                        M.dve(lambda e: e.tensor_reduce(out=s4.t[:], in_=y3, axis=AX.X, op=ALU.add), r=[ysum.k], w=[s4.k])
                        M.dve(lambda e: e.tensor_scalar(out=s4.t[:], in0=s4.t[:], scalar1=1.0 / 64, scalar2=None, op0=ALU.mult), r=[s4.k], w=[s4.k])
                        M.dve(lambda e: e.tensor_tensor(out=y3, in0=y3, in1=_bc(s4.t[:], [64, 4, 64], 2), op=ALU.subtract), r=[ysum.k, s4.k], w=[ysum.k])
                        M.act(lambda e: e.activation(out=sq.t[:], in_=ysum.t[:], func=AF.Square), r=[ysum.k], w=[sq.k])
                        M.dve(lambda e: e.tensor_reduce(out=s4.t[:], in_=sq.t[:].rearrange("p (h d) -> p h d", h=4), axis=AX.X, op=ALU.add), r=[sq.k], w=[s4.k])
                        M.dve(lambda e: e.tensor_scalar(out=s4.t[:], in0=s4.t[:], scalar1=1.0 / 64, scalar2=RWKV_LN_EPS, op0=ALU.mult, op1=ALU.add), r=[s4.k], w=[s4.k])
                        M.act(lambda e: e.activation(out=s4.t[:], in_=s4.t[:], func=AF.Sqrt), r=[s4.k], w=[s4.k])
                        M.dve(lambda e: e.reciprocal(out=s4.t[:], in_=s4.t[:]), r=[s4.k], w=[s4.k])
                        M.dve(lambda e: e.tensor_tensor(out=y3, in0=y3, in1=_bc(s4.t[:], [64, 4, 64], 2), op=ALU.mult), r=[ysum.k, s4.k], w=[ysum.k])
                        M.dve(lambda e: e.tensor_tensor(out=ysum.t[:], in0=ysum.t[:], in1=bt["rwkv_ln_gain"].t[:], op=ALU.mult), r=[ysum.k, bt["rwkv_ln_gain"].k], w=[ysum.k])
                        M.dve(lambda e: e.tensor_tensor(out=ysum.t[:], in0=ysum.t[:], in1=bt["rwkv_ln_bias"].t[:], op=ALU.add), r=[ysum.k, bt["rwkv_ln_bias"].k], w=[ysum.k])
                        M.pe(lambda e: e.matmul(pz.t[:, 0:256], lhsT=lrT.t[0:64, 1, tok], rhs=aup.t[0:64, :], start=True, stop=False, skip_group_check=True),
                             r=[lrT.k, aup.k], w=[pz.k])
                        M.pe(lambda e: e.matmul(pz.t[:, 0:256], lhsT=self.ones.t[0:1, 0:64], rhs=brow.t[0:1, 2, :], start=False, stop=True, skip_group_check=True),
                             r=[self.ones.k, brow.k], w=[pz.k])
                        M.act(lambda e: e.activation(out=af.t[:], in_=pz.t[:, 0:256], func=AF.Sigmoid), r=[pz.k], w=[af.k])
                        M.dve(lambda e: e.tensor_tensor(out=af.t[:], in0=af.t[:], in1=sz.t[:, 256:512], op=ALU.add), r=[af.k, sz.k], w=[af.k])
                        M.dve(lambda e: e.tensor_scalar(out=af.t[:], in0=af.t[:], scalar1=-2.0, scalar2=None, op0=ALU.add), r=[af.k], w=[af.k])
                        M.dve(lambda e: e.tensor_tensor(out=af.t[:], in0=af.t[:], in1=kab.t[:], op=ALU.mult), r=[af.k, kab.k], w=[af.k])
                        M.dve(lambda e: e.tensor_scalar(out=af.t[:], in0=af.t[:], scalar1=2.0, scalar2=None, op0=ALU.add), r=[af.k], w=[af.k])
                        M.dve(lambda e: e.tensor_tensor(out=af.t[:], in0=af.t[:], in1=k.t[:], op=ALU.mult), r=[af.k, k.k], w=[af.k])
                        M.dve(lambda e: e.tensor_tensor(out=af.t[:], in0=af.t[:], in1=r.t[:], op=ALU.mult), r=[af.k, r.k], w=[af.k])
                        M.dve(lambda e: e.tensor_tensor(out=af.t[:], in0=af.t[:], in1=bt["rwkv_r_k"].t[:], op=ALU.mult), r=[af.k, bt["rwkv_r_k"].k], w=[af.k])
                        M.dve(lambda e: e.tensor_reduce(out=s4.t[:], in_=af.t[:].rearrange("p (h d) -> p h d", h=4), axis=AX.X, op=ALU.add), r=[af.k], w=[s4.k])
                        M.dve(lambda e: e.tensor_tensor(out=bon.t[:].rearrange("p (h d) -> p h d", h=4), in0=v.t[:].rearrange("p (h d) -> p h d", h=4),
                                                        in1=_bc(s4.t[:], [64, 4, 64], 2), op=ALU.mult), r=[v.k, s4.k], w=[bon.k])
                        M.dve(lambda e: e.tensor_tensor(out=ysum.t[:], in0=ysum.t[:], in1=bon.t[:], op=ALU.add), r=[ysum.k, bon.k], w=[ysum.k])
                        M.act(lambda e: e.activation(out=sgd.t[:], in_=lrT.t[:, 2, tok], func=AF.Sigmoid), r=[lrT.k], w=[sgd.k])
                        M.pe(lambda e: e.matmul(pz.t[:, 256:512], lhsT=sgd.t[:], rhs=gup.t[:], start=True, stop=True), r=[sgd.k, gup.k], w=[pz.k])
                        M.dve(lambda e: e.tensor_tensor(out=m_.t[:], in0=ysum.t[:], in1=pz.t[:, 256:512], op=ALU.mult), r=[ysum.k, pz.k], w=[m_.k])
                        M.dma("pool", self.mixs.t[rows, 256:512], m_.t[:], r=[m_.k], w=[self.mixs.k])
            M.barrier()


class ProgFull(ProgMix2):
    def stage_na(self, l):
        from contextlib import ExitStack
        M, nc, I = self.M, self.nc, self.I
        cst = self.cst
        C = cst.t
        with ExitStack() as es:
            T2 = self.sb(es, "T2", [128, 8, 14, 64])
            M.dma("sp", T2.t[:].rearrange("p h r c -> p (h r c)"), I["rpbT"][l], w=[T2.k])
            qT = self.sb(es, "qT", [128, 4, TALL], BF16)
            kT = self.sb(es, "kT", [128, 4, TALL], BF16)
            Va = self.sb(es, "Va", [128, NT, 8, 65], BF16)
            M.dve(lambda e: e.memset(Va.t[:], 1.0), w=[Va.k])
            gb = [self.sb(es, f"nag{i}", [128, 64]) for i in range(2)]
            M.dma("sp", gb[0].t[:], I["na_q_gain"][l:l + 1, :].partition_broadcast(128), w=[gb[0].k])
            M.dma("sp", gb[1].t[:], I["na_k_gain"][l:l + 1, :].partition_broadcast(128), w=[gb[1].k])
            M.dve(lambda e: e.tensor_scalar(out=gb[0].t[:], in0=gb[0].t[:], scalar1=0.125, scalar2=None, op0=ALU.mult), r=[gb[0].k], w=[gb[0].k])
            with ExitStack() as e2:
                qk_ = [self.sb(e2, f"qk{i}", [128, 512]) for i in range(4)]
                vv_ = [self.sb(e2, f"vv{i}", [128, 512]) for i in range(2)]
                cs_ = [self.sb(e2, f"cs{i}", [128, 64]) for i in range(2)]
                sq_ = [self.sb(e2, f"nsq{i}", [128, 512]) for i in range(2)] * 2
                s8_ = [self.sb(e2, f"s8{i}", [128, 8]) for i in range(4)]
                ta_ = [self.sb(e2, f"ta{i}", [128, 8, 32]) for i in range(4)]
                tb_ = [self.sb(e2, f"tb{i}", [128, 8, 32]) for i in range(4)]
                tc_, td_ = ta_, tb_
                qr_ = [self.sb(e2, f"qr{i}", [128, 8, 2, 32], BF16) for i in range(4)]
                pT = [self.ps(e2, f"npT{i}", [128, 4, 128], BF16) for i in range(4)]
                for j in range(NT):
                    vv, cs = vv_[j % 2], cs_[j % 2]
                    qk = [qk_[(j % 2) * 2], qk_[(j % 2) * 2 + 1]]
                    rows = slice(j * 128, (j + 1) * 128)
                    M.dma("sp", cs.t[:], I["rope_cs"][rows, :], w=[cs.k])
                    M.dma("sp", vv.t[:], self.ptm.t[rows, 3456:3968], r=[self.ptm.k], w=[vv.k])
                    M.dve(lambda e: e.tensor_copy(out=Va.t[:, j, :, 0:64], in_=vv.t[:].rearrange("p (h d) -> p h d", h=8)), r=[vv.k], w=[Va.k])
                    cosb = _bc(cs.t[:, 0:32], [128, 8, 32], 1)
                    sinb = _bc(cs.t[:, 32:64], [128, 8, 32], 1)
                    for i, (c0, dstT) in enumerate(((2432, qT), (2944, kT))):
                        x = qk[i]
                        si_ = (j % 2) * 2 + i
                        sq, s8, ta, tb, tc, td, qr = sq_[si_], s8_[si_], ta_[si_], tb_[si_], tc_[si_], td_[si_], qr_[si_]
                        M.dma("sp", x.t[:], self.ptm.t[rows, c0:c0 + 512], r=[self.ptm.k], w=[x.k])
                        M.act(lambda e: e.activation(out=sq.t[:], in_=x.t[:], func=AF.Square), r=[x.k], w=[sq.k])
                        M.dve(lambda e: e.tensor_reduce(out=s8.t[:], in_=sq.t[:].rearrange("p (h d) -> p h d", h=8), axis=AX.X, op=ALU.add), r=[sq.k], w=[s8.k])
                        M.dve(lambda e: e.tensor_scalar(out=s8.t[:], in0=s8.t[:], scalar1=1.0 / 64, scalar2=EPS, op0=ALU.mult, op1=ALU.add), r=[s8.k], w=[s8.k])
                        M.act(lambda e: e.activation(out=s8.t[:], in_=s8.t[:], func=AF.Sqrt), r=[s8.k], w=[s8.k])
                        M.dve(lambda e: e.reciprocal(out=s8.t[:], in_=s8.t[:]), r=[s8.k], w=[s8.k])
                        x3 = x.t[:].rearrange("p (h d) -> p h d", h=8)
                        M.dve(lambda e: e.tensor_tensor(out=x3, in0=x3, in1=_bc(s8.t[:], [128, 8, 64], 2), op=ALU.mult), r=[x.k, s8.k], w=[x.k])
                        M.dve(lambda e: e.tensor_tensor(out=x3, in0=x3, in1=_bc(gb[i].t[:], [128, 8, 64], 1), op=ALU.mult), r=[x.k, gb[i].k], w=[x.k])
                        x4 = x.t[:].rearrange("p (h t d) -> p h t d", h=8, t=2)
                        t1_, t2_ = x4[:, :, 0, :], x4[:, :, 1, :]
                        M.dve(lambda e: e.tensor_tensor(out=ta.t[:], in0=t1_, in1=cosb, op=ALU.mult), r=[x.k, cs.k], w=[ta.k])
                        M.dve(lambda e: e.tensor_tensor(out=tb.t[:], in0=t2_, in1=sinb, op=ALU.mult), r=[x.k, cs.k], w=[tb.k])
                        M.dve(lambda e: e.tensor_tensor(out=qr.t[:, :, 0, :], in0=ta.t[:], in1=tb.t[:], op=ALU.subtract), r=[ta.k, tb.k], w=[qr.k])
                        M.dve(lambda e: e.tensor_tensor(out=tc.t[:], in0=t2_, in1=cosb, op=ALU.mult), r=[x.k, cs.k], w=[tc.k])
                        M.dve(lambda e: e.tensor_tensor(out=td.t[:], in0=t1_, in1=sinb, op=ALU.mult), r=[x.k, cs.k], w=[td.k])
                        M.dve(lambda e: e.tensor_tensor(out=qr.t[:, :, 1, :], in0=tc.t[:], in1=td.t[:], op=ALU.add), r=[tc.k, td.k], w=[qr.k])
                        p_ = pT[si_]
                        qf = qr.t[:].rearrange("p h t d -> p (h t d)")
                        for hp in range(4):
                            M.pe(lambda e: e.transpose(p_.t[:, hp, :], qf[:, hp * 128:(hp + 1) * 128], self.idb.t[:]), r=[qr.k, self.idb.k], w=[p_.k])
                        M.act(lambda e: e.copy(out=dstT.t[:, :, rows], in_=p_.t[:]), r=[p_.k], w=[dstT.k])
                M.barrier()
            NB = 4
            E = [self.sb(es, f"E{i}", [128, 7, 128], BF16) for i in range(NB)]
            TM = [self.sb(es, f"TM{i}", [128, 5, 64]) for i in range(NB)]
            ost = [self.sb(es, f"ost{i}", [128, 8, 64], BF16) for i in range(3)]
            rec = [self.sb(es, f"rec{i}", [128, 1]) for i in range(NB)]
            pS = [self.ps(es, f"pS{i}", [128, 512]) for i in range(NB)]
            pO = [self.ps(es, f"pO{i}", [128, 65]) for i in range(NB)]
            units = []
            for qt_ in range(2):
                for h in range(8):
                    units.append(("c", qt_, h))
            for r in range(32):
                for h in range(8):
                    units.append(("l", r, h))

            def front(u):
                kind, r, h = units[u]
                hp, po = h // 2, (h % 2) * 64
                ps_, e_ = pS[u % NB], E[u % NB]
                if kind == "c":
                    pv = ps_.t[:, 0:256].rearrange("p (m q) -> p m q", m=2)
                    for m in range(2):
                        M.pe(lambda e: e.matmul(pv[:, m, :], lhsT=kT.t[po:po + 64, hp, m * 128:(m + 1) * 128], rhs=qT.t[po:po + 64, hp, r * 128:(r + 1) * 128],
                                                start=True, stop=True), r=[kT.k, qT.k], w=[ps_.k])
                    M.act(lambda e: e.activation(out=e_.t[:, 0:2, :], in_=pv, func=AF.Exp), r=[ps_.k], w=[e_.k])
                    return
                rs = min(max(r - 4, 0), 24)
                odd = rs % 2
                e0 = rs - odd
                nch = 5 if odd else 4
                dre = e0 - r + 7
                q0 = 256 + r * 64
                pv = ps_.t[:, 0:448].rearrange("p (m q) -> p m q", m=7)
                for m in range(nch + 2):
                    k0 = 256 + (e0 + 2 * m) * 64 if m < nch else (m - nch) * 128
                    M.pe(lambda e: e.matmul(pv[:, m, :], lhsT=kT.t[po:po + 64, hp, k0:k0 + 128], rhs=qT.t[po:po + 64, hp, q0:q0 + 64], start=True, stop=True),
                         r=[kT.k, qT.k], w=[ps_.k])
                tm_ = TM[u % NB]
                M.dve(lambda e: e.tensor_tensor(out=tm_.t[:, 0:nch, :], in0=pv[:, 0:nch, :], in1=T2.t[:, h, dre:dre + 2 * nch - 1:2, :], op=ALU.add),
                      r=[ps_.k, T2.k], w=[tm_.k])
                M.act(lambda e: e.activation(out=e_.t[:, 0:nch, 0:64], in_=tm_.t[:, 0:nch, :], func=AF.Exp), r=[tm_.k], w=[e_.k])
                M.act(lambda e: e.activation(out=e_.t[:, nch:nch + 2, 0:64], in_=pv[:, nch:nch + 2, :], func=AF.Exp), r=[ps_.k], w=[e_.k])
                if odd:
                    M.dve(lambda e: e.tensor_scalar(out=e_.t[:, 0, 0:64], in0=e_.t[:, 0, 0:64], scalar1=C[:, C_CM64 + 1:C_CM64 + 2], scalar2=None, op0=ALU.mult),
                          r=[e_.k, cst.k], w=[e_.k])
                    M.dve(lambda e: e.tensor_scalar(out=e_.t[:, 4, 0:64], in0=e_.t[:, 4, 0:64], scalar1=C[:, C_CM64:C_CM64 + 1], scalar2=None, op0=ALU.mult),
                          r=[e_.k, cst.k], w=[e_.k])

            def back(u):
                kind, r, h = units[u]
                e_, pO_, rc = E[u % NB], pO[u % NB], rec[u % NB]
                if kind == "c":
                    o_ = ost[r % 3]
                    for m in range(2):
                        M.pe(lambda e: e.matmul(pO_.t[:, :], lhsT=e_.t[:, m, :], rhs=Va.t[:, m, h, :], start=(m == 0), stop=(m == 1)), r=[e_.k, Va.k], w=[pO_.k])
                    M.dve(lambda e: e.reciprocal(out=rc.t[:], in_=pO_.t[:, 64:65]), r=[pO_.k], w=[rc.k])
                    M.dve(lambda e: e.tensor_scalar(out=o_.t[:, h, :], in0=pO_.t[:, 0:64], scalar1=rc.t[:, 0:1], scalar2=None, op0=ALU.mult), r=[pO_.k, rc.k], w=[o_.k])
                    if h == 7:
                        M.dma("pool", self.mixs.t[r * 128:(r + 1) * 128, 512:1024], o_.t[:].rearrange("p h d -> p (h d)"), r=[o_.k], w=[self.mixs.k])
                    return
                rs = min(max(r - 4, 0), 24)
                odd = rs % 2
                e0 = rs - odd
                nch = 5 if odd else 4
                q0 = 256 + r * 64
                o_ = ost[r % 3]
                for m in range(nch + 2):
                    kt_ = 2 + (e0 + 2 * m) // 2 if m < nch else (m - nch)
                    M.pe(lambda e: e.matmul(pO_.t[0:64, :], lhsT=e_.t[:, m, 0:64], rhs=Va.t[:, kt_, h, :], start=(m == 0), stop=(m == nch + 1)),
                         r=[e_.k, Va.k], w=[pO_.k])
                M.dve(lambda e: e.reciprocal(out=rc.t[0:64, :], in_=pO_.t[0:64, 64:65]), r=[pO_.k], w=[rc.k])
                M.dve(lambda e: e.tensor_scalar(out=o_.t[0:64, h, :], in0=pO_.t[0:64, 0:64], scalar1=rc.t[0:64, 0:1], scalar2=None, op0=ALU.mult),
                      r=[pO_.k, rc.k], w=[o_.k])
                if h == 7:
                    M.dma("pool", self.mixs.t[q0:q0 + 64, 512:1024], o_.t[0:64, :, :].rearrange("p h d -> p (h d)"), r=[o_.k], w=[self.mixs.k])

            LA = NB - 1
            nu = len(units)
            for u in range(min(LA, nu)):
                front(u)
            for u in range(nu):
                if u + LA < nu:
                    front(u + LA)
                back(u)
            M.barrier()


class ProgAll(ProgFull):
    def stage_mid(self, l, es, xres, h2T, comb, last=False):
        from contextlib import ExitStack
        M, nc, I = self.M, self.nc, self.I
        cst = self.cst
        C = cst.t
        with ExitStack() as e2:
            wout = self.sb(e2, "wout", [128, 8, D], BF16)
            wv = I["w_out"][l].rearrange("(k p) n -> p k n", p=128)
            for k in range(8):
                M.dma("pool", wout.t[:, k, :], wv[:, k, :], w=[wout.k])
            g1b = [self.sb(e2, f"g1b{w}", [128, D]) for w in range(2)]
            for w in range(2):
                M.dma("sp", g1b[w].t[:], self.modrow(l, w, 2).partition_broadcast(128), r=[self.mods.k], w=[g1b[w].k])
            A, S = self.norm_mod_tiles(e2, l, "norm2_gain", 4, 3)
            wr = self.sb(e2, "wr", [128, 8, 36])
            br = self.sb(e2, "br", [1, 36])
            M.dma("sp", wr.t[:], I["w_router"][l].rearrange("(k p) n -> p k n", p=128), w=[wr.k])
            M.dma("sp", br.t[:], I["b_router"][l:l + 1, :], w=[br.k])
            mx = [self.sb(e2, f"mx{i}", [128, D], BF16) for i in range(2)]
            mT = self.sb(e2, "mT", [128, 8, 128], BF16)
            tmp = self.sb(e2, "tmpo", [128, 512])
            sq = self.sb(e2, "sq2", [128, D])
            ss = self.sb(e2, "ss2", [128, 1])
            hf = self.sb(e2, "hf2", [128, D])
            hb = self.sb(e2, "hb2", [128, D], BF16)
            hTf = self.sb(e2, "hTf", [128, 8, 128])
            lg = self.sb(e2, "lg", [128, 36])
            sm = {n: self.sb(e2, n, [128, 8]) for n in ("oh", "esel", "mk1", "e2", "mk2", "ew", "msk")}
            sc = {n: self.sb(e2, n, [128, 1]) for n in ("gm", "ngm", "gs", "m1", "m2", "dm", "w1", "w2")}
            junk = self.sb(e2, "junk", [128, 4])
            msk3 = self.sb(e2, "msk3", [128, 4, 8])
            ptb = self.ps(e2, "ptb", [128, 8, 128], BF16)
            ptf = [self.ps(e2, f"ptf{i}", [128, 4, 128]) for i in range(2)]
            pp = [self.ps(e2, f"ppo{i}", [128, 512]) for i in range(2)]
            plg = self.ps(e2, "plg", [128, 36])
            for j in range(2 if last else 0, NT):
                rows = slice(j * 128, (j + 1) * 128)
                which = 1 if j < 2 else 0
                m_ = mx[j % 2]
                xj = xres.t[:, j, :]
                M.dma("sp", m_.t[:], self.mixs.t[rows, :], r=[self.mixs.k], w=[m_.k])
                M.dma("sp", xj, self.xs.t[rows, :], r=[self.xs.k], w=[xres.k])
                for k in range(8):
                    M.pe(lambda e: e.transpose(ptb.t[:, k, :], m_.t[:, k * 128:(k + 1) * 128], self.idb.t[:]), r=[m_.k, self.idb.k], w=[ptb.k])
                M.act(lambda e: e.copy(out=mT.t[:], in_=ptb.t[:]), r=[ptb.k], w=[mT.k])
                for hf_ in range(2):
                    p_ = pp[hf_]
                    cs_ = slice(hf_ * 512, (hf_ + 1) * 512)
                    for k in range(8):
                        M.pe(lambda e: e.matmul(p_.t[:, :], lhsT=mT.t[:, k, :], rhs=wout.t[:, k, cs_], start=(k == 0), stop=(k == 7)), r=[mT.k, wout.k], w=[p_.k])
                    M.dve(lambda e: e.tensor_tensor(out=tmp.t[:], in0=p_.t[:], in1=g1b[which].t[:, cs_], op=ALU.mult), r=[p_.k, g1b[which].k], w=[tmp.k])
                    M.dve(lambda e: e.tensor_tensor(out=xres.t[:, j, cs_], in0=xres.t[:, j, cs_], in1=tmp.t[:], op=ALU.add), r=[xres.k, tmp.k], w=[xres.k])
                M.act(lambda e: e.activation(out=sq.t[:], in_=xj, func=AF.Square, accum_out=ss.t[:]), r=[xres.k], w=[sq.k, ss.k])
                M.dve(lambda e: e.tensor_scalar(out=ss.t[:], in0=ss.t[:], scalar1=1.0 / D, scalar2=EPS, op0=ALU.mult, op1=ALU.add), r=[ss.k], w=[ss.k])
                M.act(lambda e: e.activation(out=ss.t[:], in_=ss.t[:], func=AF.Sqrt), r=[ss.k], w=[ss.k])
                M.dve(lambda e: e.reciprocal(out=ss.t[:], in_=ss.t[:]), r=[ss.k], w=[ss.k])
                M.dve(lambda e: e.scalar_tensor_tensor(out=hf.t[:], in0=xj, scalar=ss.t[:, 0:1], in1=A[which].t[:], op0=ALU.mult, op1=ALU.mult),
                      r=[xres.k, ss.k, A[which].k], w=[hf.k])
                M.dve(lambda e: e.tensor_tensor(out=hf.t[:], in0=hf.t[:], in1=S[which].t[:], op=ALU.add), r=[hf.k, S[which].k], w=[hf.k])
                M.act(lambda e: e.copy(out=hb.t[:], in_=hf.t[:]), r=[hf.k], w=[hb.k])
                for k in range(8):
                    M.pe(lambda e: e.transpose(ptb.t[:, k, :], hb.t[:, k * 128:(k + 1) * 128], self.idb.t[:]), r=[hb.k, self.idb.k], w=[ptb.k])
                M.act(lambda e: e.copy(out=h2T.t[:, :, rows], in_=ptb.t[:]), r=[ptb.k], w=[h2T.k])
                for q4 in range(2):
                    for k in range(4):
                        kk_ = q4 * 4 + k
                        M.pe(lambda e: e.transpose(ptf[q4].t[:, k, :], hf.t[:, kk_ * 128:(kk_ + 1) * 128], C[:, C_ID:C_ID + 128]), r=[hf.k, cst.k], w=[ptf[q4].k])
                    M.act(lambda e: e.copy(out=hTf.t[:, q4 * 4:(q4 + 1) * 4, :], in_=ptf[q4].t[:]), r=[ptf[q4].k], w=[hTf.k])
                for k in range(8):
                    M.pe(lambda e: e.matmul(plg.t[:, :], lhsT=hTf.t[:, k, :], rhs=wr.t[:, k, :], start=(k == 0), stop=False), r=[hTf.k, wr.k], w=[plg.k])
                M.pe(lambda e: e.matmul(plg.t[:, :], lhsT=self.ones.t[0:1, 0:128], rhs=br.t[0:1, :], start=False, stop=True), r=[self.ones.k, br.k], w=[plg.k])
                M.act(lambda e: e.copy(out=lg.t[:], in_=plg.t[:]), r=[plg.k], w=[lg.k])
                G = lg.t[:, 0:4]
                E3 = lg.t[:, 4:36].rearrange("p (g e) -> p g e", g=4)
                oh, esel, mk1, e2_, mk2, ew = (sm[n] for n in ("oh", "esel", "mk1", "e2", "mk2", "ew"))
                gm, ngm, gs, m1, m2, dm, w1, w2 = (sc[n] for n in ("gm", "ngm", "gs", "m1", "m2", "dm", "w1", "w2"))
                M.dve(lambda e: e.tensor_reduce(out=gm.t[:], in_=G, axis=AX.X, op=ALU.max), r=[lg.k], w=[gm.k])
                M.dve(lambda e: e.tensor_scalar(out=oh.t[:, 0:4], in0=G, scalar1=gm.t[:, 0:1], scalar2=None, op0=ALU.is_equal), r=[lg.k, gm.k], w=[oh.k])
                M.dve(lambda e: e.tensor_scalar(out=ngm.t[:], in0=gm.t[:], scalar1=-1.0, scalar2=None, op0=ALU.mult), r=[gm.k], w=[ngm.k])
                M.act(lambda e: e.activation(out=junk.t[:], in_=G, func=AF.Exp, bias=ngm.t[:, 0:1], accum_out=gs.t[:]), r=[lg.k, ngm.k], w=[junk.k, gs.k])
                M.dve(lambda e: e.reciprocal(out=gs.t[:], in_=gs.t[:]), r=[gs.k], w=[gs.k])
                M.dve(lambda e: e.tensor_tensor(out=msk3.t[:], in0=E3, in1=_bc(oh.t[:, 0:4], [128, 4, 8], 2), op=ALU.mult), r=[lg.k, oh.k], w=[msk3.k])
                M.dve(lambda e: e.tensor_reduce(out=esel.t[:], in_=msk3.t[:].rearrange("p g e -> p e g"), axis=AX.X, op=ALU.add), r=[msk3.k], w=[esel.k])
                M.dve(lambda e: e.tensor_reduce(out=m1.t[:], in_=esel.t[:], axis=AX.X, op=ALU.max), r=[esel.k], w=[m1.k])
                M.dve(lambda e: e.tensor_scalar(out=mk1.t[:], in0=esel.t[:], scalar1=m1.t[:, 0:1], scalar2=None, op0=ALU.is_equal), r=[esel.k, m1.k], w=[mk1.k])
                M.dve(lambda e: e.scalar_tensor_tensor(out=e2_.t[:], in0=mk1.t[:], scalar=-1e30, in1=esel.t[:], op0=ALU.mult, op1=ALU.add), r=[mk1.k, esel.k], w=[e2_.k])
                M.dve(lambda e: e.tensor_reduce(out=m2.t[:], in_=e2_.t[:], axis=AX.X, op=ALU.max), r=[e2_.k], w=[m2.k])
                M.dve(lambda e: e.tensor_scalar(out=mk2.t[:], in0=e2_.t[:], scalar1=m2.t[:, 0:1], scalar2=None, op0=ALU.is_equal), r=[e2_.k, m2.k], w=[mk2.k])
                M.dve(lambda e: e.tensor_tensor(out=dm.t[:], in0=m2.t[:], in1=m1.t[:], op=ALU.subtract), r=[m1.k, m2.k], w=[dm.k])
                M.act(lambda e: e.activation(out=dm.t[:], in_=dm.t[:], func=AF.Exp), r=[dm.k], w=[dm.k])
                M.dve(lambda e: e.tensor_scalar(out=w1.t[:], in0=dm.t[:], scalar1=1.0, scalar2=None, op0=ALU.add), r=[dm.k], w=[w1.k])
                M.dve(lambda e: e.reciprocal(out=w1.t[:], in_=w1.t[:]), r=[w1.k], w=[w1.k])
                M.dve(lambda e: e.tensor_tensor(out=w1.t[:], in0=w1.t[:], in1=gs.t[:], op=ALU.mult), r=[w1.k, gs.k], w=[w1.k])
                M.dve(lambda e: e.tensor_tensor(out=w2.t[:], in0=w1.t[:], in1=dm.t[:], op=ALU.mult), r=[w1.k, dm.k], w=[w2.k])
                M.dve(lambda e: e.tensor_scalar(out=ew.t[:], in0=mk1.t[:], scalar1=w1.t[:, 0:1], scalar2=None, op0=ALU.mult), r=[mk1.k, w1.k], w=[ew.k])
                M.dve(lambda e: e.scalar_tensor_tensor(out=ew.t[:], in0=mk2.t[:], scalar=w2.t[:, 0:1], in1=ew.t[:], op0=ALU.mult, op1=ALU.add), r=[mk2.k, w2.k, ew.k], w=[ew.k])
                for g in range(4):
                    M.dve(lambda e: e.tensor_scalar(out=comb.t[:, j, g * 8:(g + 1) * 8], in0=ew.t[:], scalar1=oh.t[:, g:g + 1], scalar2=None, op0=ALU.mult),
                          r=[ew.k, oh.k], w=[comb.k])
            M.barrier()

    def stage_moe(self, l, xres, h2T, comb, last):
        from contextlib import ExitStack
        M, nc, I = self.M, self.nc, self.I
        with ExitStack() as e2:
            g2b = [self.sb(e2, f"g2b{w}", [128, D]) for w in range(2)]
            for w in range(2):
                M.dma("sp", g2b[w].t[:], self.modrow(l, w, 5).partition_broadcast(128), r=[self.mods.k], w=[g2b[w].k])
            Wg = [self.sb(e2, f"Wg{i}", [128, 8, 512], BF16) for i in range(2)]
            Wu = [self.sb(e2, f"Wu{i}", [128, 8, 512], BF16) for i in range(2)]
            Wd = [self.sb(e2, f"Wd{i}", [128, 4, D], BF16) for i in range(2)]
            hid = [self.sb(e2, f"hid{i}", [128, 4, 512], BF16) for i in range(2)]
            sg = [self.sb(e2, f"sg{i}", [128, 512]) for i in range(2)]
            tmp = [self.sb(e2, f"tmpm{i}", [128, 512]) for i in range(2)]
            pG = [self.ps(e2, f"pG{i}", [128, 512]) for i in range(2)]
            pU = [self.ps(e2, f"pU{i}", [128, 512]) for i in range(2)]
            pD = [self.ps(e2, f"pD{i}", [128, 512]) for i in range(2)]
            cnt = 0
            for ge in range(32):
                b = ge % 2
                gv = I["w_exp_gate"][l, ge].rearrange("(k p) f -> p k f", p=128)
                uv = I["w_exp_up"][l, ge].rearrange("(k p) f -> p k f", p=128)
                dv = I["w_exp_down"][l, ge].rearrange("(c p) d -> p c d", p=128)
                if not (getattr(self, "moe_nodma", False) and ge >= 2):
                    for k in range(0, 8, 2):
                        M.dma("pool", Wg[b].t[:, k:k + 2, :], gv[:, k:k + 2, :], w=[Wg[b].k])
                        M.dma("pool", Wu[b].t[:, k:k + 2, :], uv[:, k:k + 2, :], w=[Wu[b].k])
                    for c in range(4):
                        M.dma("pool", Wd[b].t[:, c, :], dv[:, c, :], w=[Wd[b].k])
                for t0 in range(256 if last else 0, TALL, 512):
                    t1 = min(TALL, t0 + 512)
                    n = t1 - t0
                    hd = hid[(t0 // 512) % 2]
                    for fc in range(4):
                        cnt += 1
                        g_, u_, s_ = pG[cnt % 2], pU[cnt % 2], sg[cnt % 2]
                        for k in range(8):
                            M.pe(lambda e: e.matmul(g_.t[:, 0:n], lhsT=Wg[b].t[:, k, fc * 128:(fc + 1) * 128], rhs=h2T.t[:, k, t0:t1], start=(k == 0), stop=(k == 7)),
                                 r=[Wg[b].k, h2T.k], w=[g_.k])
                        for k in range(8):
                            M.pe(lambda e: e.matmul(u_.t[:, 0:n], lhsT=Wu[b].t[:, k, fc * 128:(fc + 1) * 128], rhs=h2T.t[:, k, t0:t1], start=(k == 0), stop=(k == 7)),
                                 r=[Wu[b].k, h2T.k], w=[u_.k])
                        M.act(lambda e: e.activation(out=s_.t[:, 0:n], in_=g_.t[:, 0:n], func=AF.Silu), r=[g_.k], w=[s_.k])
                        M.dve(lambda e: e.tensor_tensor(out=hd.t[:, fc, 0:n], in0=s_.t[:, 0:n], in1=u_.t[:, 0:n], op=ALU.mult), r=[s_.k, u_.k], w=[hd.k])
                    for jt in range(n // 128):
                        j = t0 // 128 + jt
                        which = 1 if j < 2 else 0
                        for hf_ in range(2):
                            cnt += 1
                            d_, tm = pD[cnt % 2], tmp[cnt % 2]
                            cs_ = slice(hf_ * 512, (hf_ + 1) * 512)
                            for fc in range(4):
                                M.pe(lambda e: e.matmul(d_.t[:, :], lhsT=hd.t[:, fc, jt * 128:(jt + 1) * 128], rhs=Wd[b].t[:, fc, cs_], start=(fc == 0), stop=(fc == 3)),
                                     r=[hd.k, Wd[b].k], w=[d_.k])
                            M.dve(lambda e: e.tensor_tensor(out=tm.t[:], in0=d_.t[:], in1=g2b[which].t[:, cs_], op=ALU.mult), r=[d_.k, g2b[which].k], w=[tm.k])
                            M.dve(lambda e: e.scalar_tensor_tensor(out=xres.t[:, j, cs_], in0=tm.t[:], scalar=comb.t[:, j, ge:ge + 1], in1=xres.t[:, j, cs_],
                                                                   op0=ALU.mult, op1=ALU.add), r=[tm.k, comb.k, xres.k], w=[xres.k])
            for j in range(NT):
                rows = slice(j * 128, (j + 1) * 128)
                if last:
                    if j >= 2:
                        M.dma("sp", self.out[(j - 2) * 128:(j - 1) * 128, :], xres.t[:, j, :], r=[xres.k], w=[self.xs.k])
                else:
                    M.dma("sp", self.xs.t[rows, :], xres.t[:, j, :], r=[xres.k], w=[self.xs.k])
            M.barrier()

    def build_all(self):
        from contextlib import ExitStack
        with ExitStack() as es:
            self.setup(es)
            for l in range(self.NL):
                self.stage_mods(l)
                with ExitStack() as e1:
                    lrT = self.sb(e1, "lrT", [128, 3, TALL])
                    self.stage_proj(l, lrT)
                    self.stage_hgrn(l)
                    self.stage_rwkv(l, lrT)
                    self.M.barrier()
                self.stage_na(l)
                with ExitStack() as e1:
                    xres = self.sb(e1, "xres", [128, NT, D])
                    h2T = self.sb(e1, "h2T", [128, 8, TALL], BF16)
                    comb = self.sb(e1, "comb", [128, NT, 32])
                    self.stage_mid(l, e1, xres, h2T, comb, last=(l == self.NL - 1))
                    self.stage_moe(l, xres, h2T, comb, last=(l == self.NL - 1))
            self.M.barrier()


def run_streams(gens):
    active = list(gens)
    while active:
        for g in list(active):
            try:
                next(g)
            except StopIteration:
                active.remove(g)


class ProgV2(ProgAll):
    def stage_rwkv(self, l, lrT):
        from contextlib import ExitStack
        M, nc, I = self.M, self.nc, self.I
        cst = self.cst
        C = cst.t
        NTL = 36
        if not hasattr(self, "yscr"):
            self.yscr = B_(nc.dram_tensor("yscr", [2, TALL, 256], F32, kind=("ExternalOutput" if self.dbg else "Internal")).ap(), "yscr")
        yscr = self.yscr
        with ExitStack() as es:
            wup = self.sb(es, "wup", [128, 256])
            aup = self.sb(es, "aup", [128, 256])
            gup = self.sb(es, "gup", [128, 256])
            brow = self.sb(es, "browr", [1, 4, 256])
            M.dma("sp", wup.t[:], I["rwkv_w_up"][l].rearrange("d r n -> (d r) n"), w=[wup.k])
            M.dma("sp", aup.t[:], I["rwkv_a_up"][l].rearrange("d r n -> (d r) n"), w=[aup.k])
            M.dma("sp", gup.t[:], I["rwkv_g_up"][l], w=[gup.k])
            M.dma("sp", brow.t[0:1, 0:2, :], I["rwkv_w0"][l:l + 1, :, :], w=[brow.k])
            M.dma("sp", brow.t[0:1, 2:4, :], I["rwkv_a0"][l:l + 1, :, :], w=[brow.k])
            bt = {}
            for nm in ("rwkv_kk_scale", "rwkv_k_a", "rwkv_r_k", "rwkv_ln_gain", "rwkv_ln_bias"):
                bt[nm] = self.sb(es, nm, [64, 256])
                M.dma("sp", bt[nm].t[:], I[nm][l:l + 1, :].partition_broadcast(64), w=[bt[nm].k])
            kab = bt["rwkv_k_a"]
            omka = self.sb(es, "omka", [64, 256])
            M.dve(lambda e: e.tensor_scalar(out=omka.t[:], in0=kab.t[:], scalar1=-1.0, scalar2=1.0, op0=ALU.mult, op1=ALU.add), r=[kab.k], w=[omka.k])
            pgr = [self.ps(es, f"pgr{i}", [64, 512]) for i in range(6)]
            pYd_ = [self.ps(es, f"pYd{i}", [64, 512]) for i in range(2)]
            pYd = []
            for p__ in pYd_:
                bb = B_(p__.t[:, 0:256].rearrange("p (h d) -> p h d", h=4), "pYv")
                bb.k = p__.k
                pYd.append(bb)
            gi = [0]

            busy = [False] * 6

            def alloc():
                for i in range(6):
                    kx = (gi[0] + 1 + i) % 6
                    if not busy[kx]:
                        busy[kx] = True
                        gi[0] = kx
                        return pgr[kx]
                return None

            def rel(p):
                busy[pgr.index(p)] = False

            def galloc():
                while True:
                    p = alloc()
                    if p is not None:
                        return p
                    yield

            def v4(p_):
                return p_.t[:, 0:256].rearrange("p (h d) -> p h d", h=4)

            with ExitStack() as e1:
                KT, NBU = 3, 6
                TS = []
                for i in range(KT):
                    t = {}
                    for nm in ("r", "k", "lw", "kk", "sq", "t1", "kd", "ak", "en", "er", "rt", "kkt", "akt", "kdt"):
                        t[nm] = self.sb(e1, f"{nm}{i}", [64, 256])
                    for nm in ("sz", "ecx"):
                        t[nm] = self.sb(e1, f"{nm}{i}", [64, 512])
                    t["tw"] = self.sb(e1, f"tw{i}", [128, 64])
                    t["s4"] = self.sb(e1, f"s4{i}", [64, 4])
                    for nm in ("NTm", "P0", "P1"):
                        t[nm] = self.sb(e1, f"{nm}{i}", [64, 4, 64])
                    for nm in ("aktT", "kdtT"):
                        t[nm] = self.sb(e1, f"{nm}{i}", [64, 4, 64], BF16)
                    for nm in ("Xa0", "Xa1", "XT0", "XT1", "NTb", "Pb0", "Pb1"):
                        t[nm] = self.sb(e1, f"{nm}{i}", [64, 4, 64], BF16)
                    TS.append(t)
                BS = []
                for i in range(NBU):
                    b = {}
                    for nm in ("v", "akh", "kdh"):
                        b[nm] = self.sb(e1, f"b{nm}{i}", [64, 256], BF16)
                    b["X2"] = self.sb(e1, f"bX2{i}", [64, 4, 2, 64], BF16)
                    for nm in ("AkT", "BakT", "BkT", "PT"):
                        b[nm] = self.sb(e1, f"b{nm}{i}", [64, 4, 64], BF16)
                    b["ecT"] = self.sb(e1, f"becT{i}", [64, 4, 64])
                    BS.append(b)
                Sr = [[self.sb(e1, f"RS{d}{i}", [64, 4, 64]) for i in range(3)] for d in range(2)]
                Wn = [self.sb(e1, f"Wn{d}", [64, 4, 64], BF16) for d in range(2)]
                U = [self.sb(e1, f"U{d}", [64, 4, 64], BF16) for d in range(2)]
                Sbr = [[self.sb(e1, f"RSb{d}{i}", [64, 4, 64], BF16) for i in range(3)] for d in range(2)]
                yo = [[self.sb(e1, f"yo{d}{i}", [64, 256]) for i in range(2)] for d in range(2)]
                orders = [list(range(NTL)), [3, 2, 1, 0] + list(range(NTL - 1, 3, -1))]
                ready = [dict(), dict()]
                free_T = list(range(KT))
                free_B = list(range(NBU))

                def prep(d, idx, ti, bi):
                    T, Bn = TS[ti], BS[bi]
                    j = orders[d][idx]
                    CUM, SCUM, REM = [(C_U64, C_SU64, C_SL64), (C_L64, C_SL64, C_SU64)][d]
                    MSTR, MINC, MSTRT = [(C_SU64, C_U64, C_SL64), (C_SL64, C_L64, C_SU64)][d]
                    P0 = 64 * d
                    rows = slice(j * 64, (j + 1) * 64)
                    r, k, v = T["r"], T["k"], Bn["v"]
                    tw, sz, lw, kk, sq, s4, t1, kd, ak, ecx, en, er = (T[n] for n in ("tw", "sz", "lw", "kk", "sq", "s4", "t1", "kd", "ak", "ecx", "en", "er"))
                    M.dma("sp", r.t[:], self.ptm.t[rows, 1280:1536], r=[self.ptm.k], w=[r.k])
                    M.dma("sp", k.t[:], self.ptm.t[rows, 1536:1792], r=[self.ptm.k], w=[k.k])
                    M.dma("pool", v.t[:], self.ptm.t[rows, 1792:2048], r=[self.ptm.k], w=[v.k])
                    M.act(lambda e: e.activation(out=tw.t[P0:P0 + 64, :], in_=lrT.t[P0:P0 + 64, 0, rows], func=AF.Tanh), r=[lrT.k], w=[tw.k])
                    yield
                    pz = yield from galloc()
                    M.pe(lambda e: e.matmul(pz.t[:, 0:256], lhsT=tw.t[P0:P0 + 64, :], rhs=wup.t[P0:P0 + 64, :], start=True, stop=False, skip_group_check=True),
                         r=[tw.k, wup.k], w=[pz.k])
                    M.pe(lambda e: e.matmul(pz.t[:, 0:256], lhsT=self.ones.t[0:1, 0:64], rhs=brow.t[0:1, d, :], start=False, stop=False, skip_group_check=True),
                         r=[self.ones.k, brow.k], w=[pz.k])
                    M.pe(lambda e: e.matmul(pz.t[:, 256:512], lhsT=lrT.t[P0:P0 + 64, 1, rows], rhs=aup.t[P0:P0 + 64, :], start=False, stop=False, skip_group_check=True),
                         r=[lrT.k, aup.k], w=[pz.k])
                    M.pe(lambda e: e.matmul(pz.t[:, 256:512], lhsT=self.ones.t[0:1, 0:64], rhs=brow.t[0:1, 2 + d, :], start=False, stop=True, skip_group_check=True),
                         r=[self.ones.k, brow.k], w=[pz.k])
                    yield
                    M.act(lambda e: e.activation(out=sz.t[:], in_=pz.t[:], func=AF.Sigmoid), r=[pz.k], w=[sz.k])
                    rel(pz)
                    M.dve(lambda e: e.tensor_tensor(out=kk.t[:], in0=k.t[:], in1=bt["rwkv_kk_scale"].t[:], op=ALU.mult), r=[k.k, bt["rwkv_kk_scale"].k], w=[kk.k])
                    yield
                    M.dve(lambda e: e.tensor_scalar(out=lw.t[:], in0=sz.t[:, 0:256], scalar1=WSCALE, scalar2=None, op0=ALU.mult), r=[sz.k], w=[lw.k])
                    M.act(lambda e: e.activation(out=sq.t[:], in_=kk.t[:], func=AF.Square), r=[kk.k], w=[sq.k])
                    yield
                    pc0 = yield from galloc()
                    pc1 = yield from galloc()
                    M.pe(lambda e: e.matmul(pc0.t[:, 0:256], lhsT=C[0:64, CUM:CUM + 64], rhs=lw.t[:], start=True, stop=True), r=[cst.k, lw.k], w=[pc0.k])
                    M.pe(lambda e: e.matmul(pc0.t[:, 256:512], lhsT=C[0:64, SCUM:SCUM + 64], rhs=lw.t[:], start=True, stop=True), r=[cst.k, lw.k], w=[pc0.k])
                    M.pe(lambda e: e.matmul(pc1.t[:, 0:256], lhsT=C[0:64, REM:REM + 64], rhs=lw.t[:], start=True, stop=True), r=[cst.k, lw.k], w=[pc1.k])
                    M.dve(lambda e: e.tensor_reduce(out=s4.t[:], in_=sq.t[:].rearrange("p (h d) -> p h d", h=4), axis=AX.X, op=ALU.add), r=[sq.k], w=[s4.k])
                    yield
                    M.act(lambda e: e.activation(out=s4.t[:], in_=s4.t[:], func=AF.Sqrt), r=[s4.k], w=[s4.k])
                    M.act(lambda e: e.activation(out=ecx.t[:], in_=pc0.t[:], func=AF.Exp), r=[pc0.k], w=[ecx.k])
                    M.act(lambda e: e.activation(out=en.t[:], in_=pc0.t[:, 0:256], func=AF.Exp, scale=-1.0), r=[pc0.k], w=[en.k])
                    M.act(lambda e: e.activation(out=er.t[:], in_=pc1.t[:, 0:256], func=AF.Exp), r=[pc1.k], w=[er.k])
                    rel(pc0)
                    rel(pc1)
                    a_ = sz.t[:, 256:512]
                    M.dve(lambda e: e.tensor_tensor(out=t1.t[:], in0=a_, in1=kab.t[:], op=ALU.mult), r=[sz.k, kab.k], w=[t1.k])
                    M.dve(lambda e: e.tensor_tensor(out=t1.t[:], in0=t1.t[:], in1=omka.t[:], op=ALU.add), r=[t1.k, omka.k], w=[t1.k])
                    M.dve(lambda e: e.tensor_tensor(out=kd.t[:], in0=k.t[:], in1=t1.t[:], op=ALU.mult), r=[k.k, t1.k], w=[kd.k])
                    yield
                    M.dve(lambda e: e.tensor_scalar(out=s4.t[:], in0=s4.t[:], scalar1=1e-12, scalar2=None, op0=ALU.max), r=[s4.k], w=[s4.k])
                    M.dve(lambda e: e.reciprocal(out=s4.t[:], in_=s4.t[:]), r=[s4.k], w=[s4.k])
                    M.dve(lambda e: e.tensor_tensor(out=kk.t[:].rearrange("p (h d) -> p h d", h=4), in0=kk.t[:].rearrange("p (h d) -> p h d", h=4),
                                                    in1=_bc(s4.t[:], [64, 4, 64], 2), op=ALU.mult), r=[kk.k, s4.k], w=[kk.k])
                    M.dve(lambda e: e.tensor_tensor(out=ak.t[:], in0=a_, in1=kk.t[:], op=ALU.mult), r=[sz.k, kk.k], w=[ak.k])
                    rt, kkt, akt, kdt = T["rt"], T["kkt"], T["akt"], T["kdt"]
                    akh, kdh = Bn["akh"], Bn["kdh"]
                    for pi_, (o_, a0, a1, rr) in enumerate(((rt, r.t[:], ecx.t[:, 0:256], [r.k, ecx.k]), (kkt, kk.t[:], ecx.t[:, 256:512], [kk.k, ecx.k]),
                                             (akt, ak.t[:], en.t[:], [ak.k, en.k]), (kdt, kd.t[:], en.t[:], [kd.k, en.k]),
                                             (akh, ak.t[:], er.t[:], [ak.k, er.k]), (kdh, kd.t[:], er.t[:], [kd.k, er.k]))):
                        M.op("dve", lambda e: e.tensor_tensor(out=o_.t[:], in0=a0, in1=a1, op=ALU.mult), r=rr, w=[o_.k])
                    yield
                    X2, aktT, kdtT, ecT = Bn["X2"], T["aktT"], T["kdtT"], Bn["ecT"]
                    for gidx, (src, dst_ap, dstb) in enumerate(((kkt, X2.t[:, :, 0, :], X2), (rt, X2.t[:, :, 1, :], X2), (akt, aktT.t[:], aktT),
                                                                (kdt, kdtT.t[:], kdtT), (ecx, ecT.t[:], ecT))):
                        p_ = yield from galloc()
                        for h in range(4):
                            M.pe(lambda e: e.transpose(v4(p_)[:, h, :], src.t[:, h * 64:(h + 1) * 64], C[0:64, C_ID:C_ID + 64]), r=[src.k, cst.k], w=[p_.k])
                        M.act(lambda e: e.copy(out=dst_ap, in_=v4(p_)), r=[p_.k], w=[dstb.k])
                        rel(p_)
                        if gidx % 2 == 1:
                            yield
                    yield
                    NTm = T["NTm"]
                    for gidx, (lhs, ridx, msk, dst) in enumerate(((aktT, 0, MSTR, NTm), (aktT, 1, MINC, Bn["BakT"]), (kdtT, 0, MSTR, Bn["AkT"]), (kdtT, 1, MINC, Bn["BkT"]))):
                        p_ = yield from galloc()
                        for h in range(4):
                            M.pe(lambda e: e.matmul(v4(p_)[:, h, :], lhsT=lhs.t[:, h, :], rhs=X2.t[:, h, ridx, :], start=True, stop=True), r=[lhs.k, X2.k], w=[p_.k])
                        M.dve(lambda e: e.tensor_tensor(out=dst.t[:], in0=v4(p_), in1=_bc(C[0:64, msk:msk + 64], [64, 4, 64], 1), op=ALU.mult),
                              r=[p_.k, cst.k], w=[dst.k])
                        if dst is NTm:
                            M.dve(lambda e: e.tensor_tensor(out=T["NTb"].t[:], in0=v4(p_), in1=_bc(C[0:64, msk:msk + 64], [64, 4, 64], 1), op=ALU.mult),
                                  r=[p_.k, cst.k], w=[T["NTb"].k])
                        rel(p_)
                        if gidx % 2 == 1:
                            yield
                    p_ = yield from galloc()
                    for h in range(4):
                        M.pe(lambda e: e.matmul(v4(p_)[:, h, :], lhsT=X2.t[:, h, 0, :], rhs=aktT.t[:, h, :], start=True, stop=True), r=[aktT.k, X2.k], w=[p_.k])
                    Xs = [T["Xa0"], T["Xa1"]]
                    XTs = [T["XT0"], T["XT1"]]
                    Ps = [T["P0"], T["P1"]]
                    Pbs = [T["Pb0"], T["Pb1"]]
                    X, XT, P_, Pb = Xs[0], T["NTb"], Ps[0], Pbs[0]
                    M.dve(lambda e: e.tensor_tensor(out=X.t[:], in0=v4(p_), in1=_bc(C[0:64, MSTRT:MSTRT + 64], [64, 4, 64], 1), op=ALU.mult),
                          r=[p_.k, cst.k], w=[X.k])
                    rel(p_)
                    M.dve(lambda e: e.scalar_tensor_tensor(out=P_.t[:], in0=NTm.t[:], scalar=-1.0, in1=_bc(C[0:64, C_ID:C_ID + 64], [64, 4, 64], 1), op0=ALU.mult, op1=ALU.add),
                          r=[NTm.k, cst.k], w=[P_.k])
                    M.dve(lambda e: e.scalar_tensor_tensor(out=Pb.t[:], in0=NTm.t[:], scalar=-1.0, in1=_bc(C[0:64, C_ID:C_ID + 64], [64, 4, 64], 1), op0=ALU.mult, op1=ALU.add),
                          r=[NTm.k, cst.k], w=[Pb.k])
                    yield
                    for i in range(1, 6):
                        Xn, XTn = Xs[i % 2], XTs[i % 2]
                        pX = yield from galloc()
                        for h in range(4):
                            M.pe(lambda e: e.matmul(v4(pX)[:, h, :], lhsT=XT.t[:, h, :], rhs=X.t[:, h, :], start=True, stop=True), r=[XT.k, X.k], w=[pX.k])
                        if i < 5:
                            pXT = yield from galloc()
                            for h in range(4):
                                M.pe(lambda e: e.matmul(v4(pXT)[:, h, :], lhsT=X.t[:, h, :], rhs=XT.t[:, h, :], start=True, stop=True), r=[XT.k, X.k], w=[pXT.k])
                        yield
                        M.act(lambda e: e.copy(out=Xn.t[:], in_=v4(pX)), r=[pX.k], w=[Xn.k])
                        rel(pX)
                        if i < 5:
                            M.act(lambda e: e.copy(out=XTn.t[:], in_=v4(pXT)), r=[pXT.k], w=[XTn.k])
                            rel(pXT)
                        yield
                        pP = yield from galloc()
                        for h in range(4):
                            M.pe(lambda e: e.matmul(v4(pP)[:, h, :], lhsT=Xn.t[:, h, :], rhs=Pb.t[:, h, :], start=True, stop=True), r=[Xn.k, Pb.k], w=[pP.k])
                        yield
                        Pn = Ps[i % 2]
                        Pbn = Bn["PT"] if i == 5 else Pbs[i % 2]
                        if i < 5:
                            M.dve(lambda e: e.tensor_tensor(out=Pn.t[:], in0=P_.t[:], in1=v4(pP), op=ALU.add), r=[P_.k, pP.k], w=[Pn.k])
                        M.dve(lambda e: e.tensor_tensor(out=Pbn.t[:], in0=P_.t[:], in1=v4(pP), op=ALU.add), r=[P_.k, pP.k], w=[Pbn.k])
                        rel(pP)
                        X, XT, P_, Pb = Xn, XTn, Pn, Pbn
                        yield
                    free_T.append(ti)
                    ready[d][idx] = bi

                def chain(d):
                    si = 0
                    S = Sr[d][0]
                    Sb = Sbr[d][0]
                    M.dve(lambda e: e.memset(S.t[:], 0.0), w=[S.k])
                    M.dve(lambda e: e.memset(Sb.t[:], 0.0), w=[Sb.k])
                    pY = pYd[d]
                    for idx in range(NTL):
                        while idx not in ready[d]:
                            yield
                        bi = ready[d][idx]
                        Bn = BS[bi]
                        j = orders[d][idx]
                        rows = slice(j * 64, (j + 1) * 64)
                        v, X2, AkT, BakT, BkT, PT_, akh, kdh, ecT = (Bn[n] for n in ("v", "X2", "AkT", "BakT", "BkT", "PT", "akh", "kdh", "ecT"))
                        pW = yield from galloc()
                        for h in range(4):
                            hs = slice(h * 64, (h + 1) * 64)
                            M.pe(lambda e: e.matmul(v4(pW)[:, h, :], lhsT=X2.t[:, h, 0, :], rhs=Sb.t[:, h, :], start=(h == 0), stop=False, skip_group_check=True),
                                 r=[X2.k, Sb.k], w=[pW.k])
                            M.pe(lambda e: e.matmul(v4(pW)[:, h, :], lhsT=AkT.t[:, h, :], rhs=v.t[:, hs], start=False, stop=True, skip_group_check=True),
                                 r=[AkT.k, v.k], w=[pW.k])
                        for h in range(4):
                            hs = slice(h * 64, (h + 1) * 64)
                            M.pe(lambda e: e.matmul(pY.t[:, h, :], lhsT=X2.t[:, h, 1, :], rhs=Sb.t[:, h, :], start=(h == 0), stop=False, skip_group_check=True),
                                 r=[X2.k, Sb.k], w=[pY.k])
                            M.pe(lambda e: e.matmul(pY.t[:, h, :], lhsT=BkT.t[:, h, :], rhs=v.t[:, hs], start=False, stop=False, skip_group_check=True),
                                 r=[BkT.k, v.k], w=[pY.k])
                        yield
                        M.act(lambda e: e.mul(out=Wn[d].t[:], in_=v4(pW), mul=-1.0), r=[pW.k], w=[Wn[d].k])
                        rel(pW)
                        yield
                        pU = yield from galloc()
                        for h in range(4):
                            M.pe(lambda e: e.matmul(v4(pU)[:, h, :], lhsT=PT_.t[:, h, :], rhs=Wn[d].t[:, h, :], start=True, stop=True), r=[PT_.k, Wn[d].k], w=[pU.k])
                        yield
                        M.act(lambda e: e.copy(out=U[d].t[:], in_=v4(pU)), r=[pU.k], w=[U[d].k])
                        rel(pU)
                        yield
                        pS = yield from galloc()
                        for h in range(4):
                            hs = slice(h * 64, (h + 1) * 64)
                            M.pe(lambda e: e.matmul(v4(pS)[:, h, :], lhsT=akh.t[:, hs], rhs=U[d].t[:, h, :], start=(h == 0), stop=False, skip_group_check=True),
                                 r=[akh.k, U[d].k], w=[pS.k])
                            M.pe(lambda e: e.matmul(v4(pS)[:, h, :], lhsT=kdh.t[:, hs], rhs=v.t[:, hs], start=False, stop=True, skip_group_check=True),
                                 r=[kdh.k, v.k], w=[pS.k])
                        for h in range(4):
                            M.pe(lambda e: e.matmul(pY.t[:, h, :], lhsT=BakT.t[:, h, :], rhs=U[d].t[:, h, :], start=False, stop=True, skip_group_check=True),
                                 r=[BakT.k, U[d].k], w=[pY.k])
                        si = (si + 1) % 3
                        Sn = Sr[d][si]
                        col = 63 if d == 0 else 0
                        M.dve(lambda e: e.tensor_tensor(out=Sn.t[:], in0=S.t[:], in1=ecT.t[:, :, col:col + 1].to_broadcast([64, 4, 64]), op=ALU.mult),
                              r=[S.k, ecT.k], w=[Sn.k])
                        yield
                        Sbn = Sbr[d][si]
                        M.dve(lambda e: e.tensor_tensor(out=Sbn.t[:], in0=Sn.t[:], in1=v4(pS), op=ALU.add), r=[Sn.k, pS.k], w=[Sbn.k])
                        M.dve(lambda e: e.tensor_tensor(out=Sn.t[:], in0=Sn.t[:], in1=v4(pS), op=ALU.add), r=[Sn.k, pS.k], w=[Sn.k])
                        rel(pS)
                        S, Sb = Sn, Sbn
                        y_ = yo[d][idx % 2]
                        M.act(lambda e: e.copy(out=y_.t[:].rearrange("p (h d) -> p h d", h=4), in_=pY.t[:]), r=[pY.k], w=[y_.k])
                        yield
                        M.dma("pool", yscr.t[d, rows, :], y_.t[:], r=[y_.k], w=[yscr.k])
                        free_B.append(bi)

                tasks = []
                for idx in range(NTL):
                    tasks.append((0, idx))
                    tasks.append((1, idx))
                active = [chain(0), chain(1)]
                while active:
                    while tasks and free_T and free_B:
                        d_, idx_ = tasks.pop(0)
                        active.append(prep(d_, idx_, free_T.pop(0), free_B.pop(0)))
                    for g in list(active):
                        try:
                            next(g)
                        except StopIteration:
                            active.remove(g)
                M.barrier()
            with ExitStack() as e1:
                KR = 5
                RS_ = []
                for i in range(KR):
                    t = {}
                    for nm in ("r", "k", "v", "yf", "yb", "sq", "af", "ab", "bon"):
                        t[nm] = self.sb(e1, f"ro{nm}{i}", [64, 256])
                    t["s4"] = self.sb(e1, f"ros4{i}", [64, 4])
                    t["s5"] = self.sb(e1, f"ros5{i}", [64, 4])
                    t["sgd"] = self.sb(e1, f"rosgd{i}", [128, 64])
                    t["mo"] = self.sb(e1, f"romo{i}", [64, 256], BF16)
                    RS_.append(t)

                def readout(j, T):
                    rows = slice(j * 64, (j + 1) * 64)
                    r, k, v, yf, yb, sq, af, ab, bon, s4, s5, sgd, m_ = (T[n] for n in ("r", "k", "v", "yf", "yb", "sq", "af", "ab", "bon", "s4", "s5", "sgd", "mo"))
                    M.dma("sp", r.t[:], self.ptm.t[rows, 1280:1536], r=[self.ptm.k], w=[r.k])
                    M.dma("sp", k.t[:], self.ptm.t[rows, 1536:1792], r=[self.ptm.k], w=[k.k])
                    M.dma("sp", v.t[:], self.ptm.t[rows, 1792:2048], r=[self.ptm.k], w=[v.k])
                    M.dma("sp", yf.t[:], yscr.t[0, rows, :], r=[yscr.k], w=[yf.k])
                    M.dma("sp", yb.t[:], yscr.t[1, rows, :], r=[yscr.k], w=[yb.k])
                    M.act(lambda e: e.activation(out=sgd.t[:], in_=lrT.t[:, 2, rows], func=AF.Sigmoid), r=[lrT.k], w=[sgd.k])
                    pz = yield from galloc()
                    for d in range(2):
                        P0 = 64 * d
                        M.pe(lambda e: e.matmul(pz.t[:, d * 256:(d + 1) * 256], lhsT=lrT.t[P0:P0 + 64, 1, rows], rhs=aup.t[P0:P0 + 64, :], start=(d == 0), stop=False,
                                                skip_group_check=True), r=[lrT.k, aup.k], w=[pz.k])
                        M.pe(lambda e: e.matmul(pz.t[:, d * 256:(d + 1) * 256], lhsT=self.ones.t[0:1, 0:64], rhs=brow.t[0:1, 2 + d, :], start=False, stop=(d == 1),
                                                skip_group_check=True), r=[self.ones.k, brow.k], w=[pz.k])
                    yield
                    y3 = yf.t[:].rearrange("p (h d) -> p h d", h=4)
                    M.dve(lambda e: e.tensor_tensor(out=yf.t[:], in0=yf.t[:], in1=yb.t[:], op=ALU.add), r=[yf.k, yb.k], w=[yf.k])
                    M.dve(lambda e: e.tensor_reduce(out=s4.t[:], in_=y3, axis=AX.X, op=ALU.add), r=[yf.k], w=[s4.k])
                    M.dve(lambda e: e.tensor_scalar(out=s4.t[:], in0=s4.t[:], scalar1=1.0 / 64, scalar2=None, op0=ALU.mult), r=[s4.k], w=[s4.k])
                    M.dve(lambda e: e.tensor_tensor(out=y3, in0=y3, in1=_bc(s4.t[:], [64, 4, 64], 2), op=ALU.subtract), r=[yf.k, s4.k], w=[yf.k])
                    M.act(lambda e: e.activation(out=af.t[:], in_=pz.t[:, 0:256], func=AF.Sigmoid), r=[pz.k], w=[af.k])
                    M.act(lambda e: e.activation(out=ab.t[:], in_=pz.t[:, 256:512], func=AF.Sigmoid), r=[pz.k], w=[ab.k])
                    rel(pz)
                    yield
                    M.act(lambda e: e.activation(out=sq.t[:], in_=yf.t[:], func=AF.Square), r=[yf.k], w=[sq.k])
                    pg_ = yield from galloc()
                    M.pe(lambda e: e.matmul(pg_.t[:, 0:256], lhsT=sgd.t[:], rhs=gup.t[:], start=True, stop=True), r=[sgd.k, gup.k], w=[pg_.k])
                    M.dve(lambda e: e.tensor_tensor(out=af.t[:], in0=af.t[:], in1=ab.t[:], op=ALU.add), r=[af.k, ab.k], w=[af.k])
                    M.dve(lambda e: e.tensor_scalar(out=af.t[:], in0=af.t[:], scalar1=-2.0, scalar2=None, op0=ALU.add), r=[af.k], w=[af.k])
                    M.dve(lambda e: e.tensor_tensor(out=af.t[:], in0=af.t[:], in1=kab.t[:], op=ALU.mult), r=[af.k, kab.k], w=[af.k])
                    M.dve(lambda e: e.tensor_scalar(out=af.t[:], in0=af.t[:], scalar1=2.0, scalar2=None, op0=ALU.add), r=[af.k], w=[af.k])
                    M.dve(lambda e: e.tensor_tensor(out=af.t[:], in0=af.t[:], in1=k.t[:], op=ALU.mult), r=[af.k, k.k], w=[af.k])
                    M.dve(lambda e: e.tensor_tensor(out=af.t[:], in0=af.t[:], in1=r.t[:], op=ALU.mult), r=[af.k, r.k], w=[af.k])
                    M.dve(lambda e: e.tensor_tensor(out=af.t[:], in0=af.t[:], in1=bt["rwkv_r_k"].t[:], op=ALU.mult), r=[af.k, bt["rwkv_r_k"].k], w=[af.k])
                    M.dve(lambda e: e.tensor_reduce(out=s5.t[:], in_=af.t[:].rearrange("p (h d) -> p h d", h=4), axis=AX.X, op=ALU.add), r=[af.k], w=[s5.k])
                    M.dve(lambda e: e.tensor_tensor(out=bon.t[:].rearrange("p (h d) -> p h d", h=4), in0=v.t[:].rearrange("p (h d) -> p h d", h=4),
                                                    in1=_bc(s5.t[:], [64, 4, 64], 2), op=ALU.mult), r=[v.k, s5.k], w=[bon.k])
                    yield
                    M.dve(lambda e: e.tensor_reduce(out=s4.t[:], in_=sq.t[:].rearrange("p (h d) -> p h d", h=4), axis=AX.X, op=ALU.add), r=[sq.k], w=[s4.k])
                    M.dve(lambda e: e.tensor_scalar(out=s4.t[:], in0=s4.t[:], scalar1=1.0 / 64, scalar2=RWKV_LN_EPS, op0=ALU.mult, op1=ALU.add), r=[s4.k], w=[s4.k])
                    yield
                    M.act(lambda e: e.activation(out=s4.t[:], in_=s4.t[:], func=AF.Sqrt), r=[s4.k], w=[s4.k])
                    yield
                    M.dve(lambda e: e.reciprocal(out=s4.t[:], in_=s4.t[:]), r=[s4.k], w=[s4.k])
                    M.dve(lambda e: e.tensor_tensor(out=y3, in0=y3, in1=_bc(s4.t[:], [64, 4, 64], 2), op=ALU.mult), r=[yf.k, s4.k], w=[yf.k])
                    M.dve(lambda e: e.tensor_tensor(out=yf.t[:], in0=yf.t[:], in1=bt["rwkv_ln_gain"].t[:], op=ALU.mult), r=[yf.k, bt["rwkv_ln_gain"].k], w=[yf.k])
                    M.dve(lambda e: e.tensor_tensor(out=yf.t[:], in0=yf.t[:], in1=bt["rwkv_ln_bias"].t[:], op=ALU.add), r=[yf.k, bt["rwkv_ln_bias"].k], w=[yf.k])
                    M.dve(lambda e: e.tensor_tensor(out=yf.t[:], in0=yf.t[:], in1=bon.t[:], op=ALU.add), r=[yf.k, bon.k], w=[yf.k])
                    M.dve(lambda e: e.tensor_tensor(out=m_.t[:], in0=yf.t[:], in1=pg_.t[:, 0:256], op=ALU.mult), r=[yf.k, pg_.k], w=[m_.k])
                    rel(pg_)
                    yield
                    M.dma("pool", self.mixs.t[rows, 256:512], m_.t[:], r=[m_.k], w=[self.mixs.k])

                todo = list(range(NTL))
                active = []
                freeR = list(range(KR))

                def wrap(j, ri):
                    yield from readout(j, RS_[ri])
                    freeR.append(ri)

                while todo or active:
                    while todo and freeR:
                        active.append(wrap(todo.pop(0), freeR.pop(0)))
                    for g in list(active):
                        try:
                            next(g)
                        except StopIteration:
                            active.remove(g)
                M.barrier()


def kernel(**inputs):
    P = ProgV5(NL=4, dbg=False, moe_in=True)
    P.build_all()
    sh = prep_shared(inputs, NL=4, moe_in=True)
    in_maps = [prep_core(inputs, b, sh) for b in range(8)]
    res = run_bass_kernel_spmd(P.nc, in_maps, core_ids=list(range(8)))
    out = np.stack([np.asarray(r["out"], dtype=np.float32) for r in res.results], 0)
    return out.astype(np.asarray(inputs["x"]).dtype)


class ProgV3(ProgV2):
    def stage_hgrn(self, l):
        from contextlib import ExitStack
        M, nc, I = self.M, self.nc, self.I
        cst = self.cst
        C = cst.t
        with ExitStack() as es:
            lb = [self.sb(es, f"lb{d}", [128, 256]) for d in range(2)]
            oml = [self.sb(es, f"oml{d}", [128, 256]) for d in range(2)]
            for d in range(2):
                M.dma("sp", lb[d].t[:], self.lbs.t[d, l:l + 1, :].partition_broadcast(128), r=[self.lbs.k], w=[lb[d].k])
                M.dve(lambda e: e.tensor_scalar(out=oml[d].t[:], in0=lb[d].t[:], scalar1=-1.0, scalar2=1.0, op0=ALU.mult, op1=ALU.add),
                      r=[lb[d].k], w=[oml[d].k])
            gnb = self.sb(es, "gnb", [128, 256])
            self.bload("sp", gnb, I["hgrn_gn_gain"][l:l + 1, :], 256)
            obuf = [self.sb(es, f"obuf{d}", [128, NT, 256]) for d in range(2)]
            pool = [self.ps(es, f"hp{i}", [128, 512]) for i in range(8)]
            busy = [False] * 8
            gi = [0]

            def alloc():
                for i in range(8):
                    kx = (gi[0] + 1 + i) % 8
                    if not busy[kx]:
                        busy[kx] = True
                        gi[0] = kx
                        return pool[kx]
                return None

            def rel(p):
                busy[pool.index(p)] = False

            def galloc():
                while True:
                    p = alloc()
                    if p is not None:
                        return p
                    yield

            with ExitStack() as e1:
                WS = {}
                for d in range(2):
                    for par in range(2):
                        t = {}
                        for nm in ("q", "f", "v", "kk", "ec", "en", "er", "qt", "kt", "kh"):
                            t[nm] = self.sb(e1, f"h{nm}{d}{par}", [128, 256])
                        t["khm"] = self.sb(e1, f"hkhm{d}{par}", [128, 4, 256], BF16)
                        t["vb"] = self.sb(e1, f"hvb{d}{par}", [128, 256], BF16)
                        for nm in ("qtT", "ktT"):
                            t[nm] = self.sb(e1, f"h{nm}{d}{par}", [64, 4, 128], BF16)
                        t["ecT"] = self.sb(e1, f"hecT{d}{par}", [64, 4, 128])
                        t["qtTm"] = self.sb(e1, f"hqtTm{d}{par}", [64, 4, 4, 128], BF16)
                        t["scm"] = self.sb(e1, f"hscm{d}{par}", [128, 4, 128], BF16)
                        WS[(d, par)] = t
                Sr = [[self.sb(e1, f"HS{d}{i}", [64, 4, 64]) for i in range(3)] for d in range(2)]
                Sb_ = [[self.sb(e1, f"HSb{d}{i}", [64, 4, 64], BF16) for i in range(3)] for d in range(2)]

                def v64(p_):
                    return p_.t[0:64, :].rearrange("p (h t) -> p h t", h=4)

                def v128(p_):
                    return p_.t[:, :].rearrange("p (h t) -> p h t", h=4)

                def hg(d):
                    CUM = [C_U32, C_L32][d]
                    REM = [C_SL32, C_SU32][d]
                    fcol = 256 + 256 * d
                    order = list(range(NT)) if d == 0 else [1, 0] + list(range(NT - 1, 1, -1))
                    chunks = [0, 1, 2, 3] if d == 0 else [3, 2, 1, 0]
                    si = 0
                    S = Sr[d][0]
                    Sb = Sb_[d][0]
                    M.dve(lambda e: e.memset(S.t[:], 0.0), w=[S.k])
                    M.dve(lambda e: e.memset(Sb.t[:], 0.0), w=[Sb.k])
                    for it, j in enumerate(order):
                        T = WS[(d, it % 2)]
                        vb = T["vb"]
                        q, f, v, kk, ec, en, er, qt, kt, kh, khm, qtT, ktT, ecT, qtTm, scm = (T[n] for n in (
                            "q", "f", "v", "kk", "ec", "en", "er", "qt", "kt", "kh", "khm", "qtT", "ktT", "ecT", "qtTm", "scm"))
                        rows = slice(j * 128, (j + 1) * 128)
                        M.dma("sp", q.t[:], self.ptm.t[rows, 0:256], r=[self.ptm.k], w=[q.k])
                        M.dma("sp", f.t[:], self.ptm.t[rows, fcol:fcol + 256], r=[self.ptm.k], w=[f.k])
                        M.dma("pool", vb.t[:], self.ptm.t[rows, 768:1024], r=[self.ptm.k], w=[vb.k])
                        yield
                        M.act(lambda e: e.activation(out=f.t[:], in_=f.t[:], func=AF.Sigmoid), r=[f.k], w=[f.k])
                        yield
                        M.dve(lambda e: e.tensor_tensor(out=f.t[:], in0=f.t[:], in1=oml[d].t[:], op=ALU.mult), r=[f.k, oml[d].k], w=[f.k])
                        M.dve(lambda e: e.tensor_tensor(out=f.t[:], in0=f.t[:], in1=lb[d].t[:], op=ALU.add), r=[f.k, lb[d].k], w=[f.k])
                        M.dve(lambda e: e.tensor_scalar(out=kk.t[:], in0=f.t[:], scalar1=-1.0, scalar2=1.0, op0=ALU.mult, op1=ALU.add), r=[f.k], w=[kk.k])
                        M.dve(lambda e: e.tensor_scalar(out=f.t[:], in0=f.t[:], scalar1=1e-20, scalar2=None, op0=ALU.max), r=[f.k], w=[f.k])
                        yield
                        M.act(lambda e: e.activation(out=f.t[:], in_=f.t[:], func=AF.Ln), r=[f.k], w=[f.k])
                        yield
                        pcr = yield from galloc()
                        M.pe(lambda e: e.matmul(pcr.t[:, 0:256], lhsT=C[:, CUM:CUM + 128], rhs=f.t[:], start=True, stop=True), r=[cst.k, f.k], w=[pcr.k])
                        M.pe(lambda e: e.matmul(pcr.t[:, 256:512], lhsT=C[:, REM:REM + 128], rhs=f.t[:], start=True, stop=True), r=[cst.k, f.k], w=[pcr.k])
                        yield
                        M.act(lambda e: e.activation(out=ec.t[:], in_=pcr.t[:, 0:256], func=AF.Exp), r=[pcr.k], w=[ec.k])
                        M.act(lambda e: e.activation(out=en.t[:], in_=pcr.t[:, 0:256], func=AF.Exp, scale=-1.0), r=[pcr.k], w=[en.k])
                        M.act(lambda e: e.activation(out=er.t[:], in_=pcr.t[:, 256:512], func=AF.Exp), r=[pcr.k], w=[er.k])
                        rel(pcr)
                        yield
                        M.dve(lambda e: e.tensor_tensor(out=qt.t[:], in0=q.t[:], in1=ec.t[:], op=ALU.mult), r=[q.k, ec.k], w=[qt.k])
                        M.dve(lambda e: e.tensor_tensor(out=kt.t[:], in0=kk.t[:], in1=en.t[:], op=ALU.mult), r=[kk.k, en.k], w=[kt.k])
                        M.dve(lambda e: e.tensor_tensor(out=kh.t[:], in0=kk.t[:], in1=er.t[:], op=ALU.mult), r=[kk.k, er.k], w=[kh.k])
                        for c in range(4):
                            M.dve(lambda e: e.tensor_scalar(out=khm.t[:, c, :], in0=kh.t[:], scalar1=C[:, C_CM32 + c:C_CM32 + c + 1], scalar2=None, op0=ALU.mult),
                                  r=[kh.k, cst.k], w=[khm.k])
                        yield
                        for (src, dst) in ((qt, qtT), (kt, ktT), (ec, ecT)):
                            pt_ = yield from galloc()
                            for h in range(4):
                                M.pe(lambda e: e.transpose(v64(pt_)[:, h, :], src.t[:, h * 64:(h + 1) * 64], C[:, C_ID:C_ID + 128]), r=[src.k, cst.k], w=[pt_.k])
                            yield
                            M.act(lambda e: e.copy(out=dst.t[:], in_=v64(pt_)), r=[pt_.k], w=[dst.k])
                            rel(pt_)
                        yield
                        for c in range(4):
                            M.dve(lambda e: e.tensor_tensor(out=qtTm.t[:, c, :, :], in0=qtT.t[:], in1=_bc(C[0:64, C_COL32 + c * 128:C_COL32 + (c + 1) * 128], [64, 4, 128], 1),
                                                            op=ALU.mult), r=[qtT.k, cst.k], w=[qtTm.k])
                        psc = yield from galloc()
                        for h in range(4):
                            M.pe(lambda e: e.matmul(v128(psc)[:, h, :], lhsT=ktT.t[:, h, :], rhs=qtT.t[:, h, :], start=True, stop=True), r=[ktT.k, qtT.k], w=[psc.k])
                        yield
                        M.dve(lambda e: e.tensor_tensor(out=scm.t[:], in0=v128(psc), in1=_bc(C[:, CUM:CUM + 128], [128, 4, 128], 1), op=ALU.mult),
                              r=[psc.k, cst.k], w=[scm.k])
                        rel(psc)
                        yield
                        po = yield from galloc()
                        pov = po.t[:, 0:256].rearrange("p (h d) -> p h d", h=4)
                        for h in range(4):
                            M.pe(lambda e: e.matmul(pov[:, h, :], lhsT=scm.t[:, h, :], rhs=vb.t[:, h * 64:(h + 1) * 64], start=(h == 0), stop=False, skip_group_check=True),
                                 r=[scm.k, vb.k], w=[po.k])
                        for ci, c in enumerate(chunks):
                            pkv = yield from galloc()
                            kvv = pkv.t[0:64, 0:256].rearrange("p (h d) -> p h d", h=4)
                            for h in range(4):
                                M.pe(lambda e: e.matmul(kvv[:, h, :], lhsT=khm.t[:, c, h * 64:(h + 1) * 64], rhs=vb.t[:, h * 64:(h + 1) * 64], start=True, stop=True),
                                     r=[khm.k, vb.k], w=[pkv.k])
                            for h in range(4):
                                M.pe(lambda e: e.matmul(pov[:, h, :], lhsT=qtTm.t[:, c, h, :], rhs=Sb.t[:, h, :], start=False, stop=(ci == 3), skip_group_check=True),
                                     r=[qtTm.k, Sb.k], w=[po.k])
                            si = (si + 1) % 3
                            Sn = Sr[d][si]
                            col = 32 * c + 31 if d == 0 else 32 * c
                            M.dve(lambda e: e.tensor_tensor(out=Sn.t[:], in0=S.t[:], in1=ecT.t[:, :, col:col + 1].to_broadcast([64, 4, 64]), op=ALU.mult),
                                  r=[S.k, ecT.k], w=[Sn.k])
                            yield
                            Sbn = Sb_[d][si]
                            M.dve(lambda e: e.tensor_tensor(out=Sbn.t[:], in0=Sn.t[:], in1=kvv, op=ALU.add), r=[Sn.k, pkv.k], w=[Sbn.k])
                            M.dve(lambda e: e.tensor_tensor(out=Sn.t[:], in0=Sn.t[:], in1=kvv, op=ALU.add), r=[Sn.k, pkv.k], w=[Sn.k])
                            rel(pkv)
                            S, Sb = Sn, Sbn
                            yield
                        M.act(lambda e: e.copy(out=obuf[d].t[:, j, :], in_=po.t[:, 0:256]), r=[po.k], w=[obuf[d].k])
                        rel(po)
                        yield

                run_streams([hg(0), hg(1)])
                M.barrier()
            with ExitStack() as e1:
                KR = 4
                RS_ = []
                for i in range(KR):
                    t = {nm: self.sb(e1, f"hr{nm}{i}", [128, 256]) for nm in ("g", "osum", "sq")}
                    t["st4"] = self.sb(e1, f"hrst4{i}", [128, 4])
                    t["mo"] = self.sb(e1, f"hrmo{i}", [128, 256], BF16)
                    RS_.append(t)

                def ro(j, T):
                    g, osum, sq, st4, m_ = T["g"], T["osum"], T["sq"], T["st4"], T["mo"]
                    rows = slice(j * 128, (j + 1) * 128)
                    M.dma("sp", g.t[:], self.ptm.t[rows, 1024:1280], r=[self.ptm.k], w=[g.k])
                    M.dve(lambda e: e.tensor_tensor(out=osum.t[:], in0=obuf[0].t[:, j, :], in1=obuf[1].t[:, j, :], op=ALU.add), r=[obuf[0].k, obuf[1].k], w=[osum.k])
                    yield
                    M.act(lambda e: e.activation(out=sq.t[:], in_=osum.t[:], func=AF.Square), r=[osum.k], w=[sq.k])
                    M.act(lambda e: e.activation(out=g.t[:], in_=g.t[:], func=AF.Silu), r=[g.k], w=[g.k])
                    yield
                    M.dve(lambda e: e.tensor_reduce(out=st4.t[:], in_=sq.t[:].rearrange("p (h d) -> p h d", h=4), axis=AX.X, op=ALU.add), r=[sq.k], w=[st4.k])
                    M.dve(lambda e: e.tensor_scalar(out=st4.t[:], in0=st4.t[:], scalar1=1.0 / 64, scalar2=EPS, op0=ALU.mult, op1=ALU.add), r=[st4.k], w=[st4.k])
                    yield
                    M.act(lambda e: e.activation(out=st4.t[:], in_=st4.t[:], func=AF.Sqrt), r=[st4.k], w=[st4.k])
                    yield
                    M.dve(lambda e: e.reciprocal(out=st4.t[:], in_=st4.t[:]), r=[st4.k], w=[st4.k])
                    M.dve(lambda e: e.tensor_tensor(out=osum.t[:].rearrange("p (h d) -> p h d", h=4), in0=osum.t[:].rearrange("p (h d) -> p h d", h=4),
                                                    in1=_bc(st4.t[:], [128, 4, 64], 2), op=ALU.mult), r=[osum.k, st4.k], w=[osum.k])
                    M.dve(lambda e: e.tensor_tensor(out=osum.t[:], in0=osum.t[:], in1=gnb.t[:], op=ALU.mult), r=[osum.k, gnb.k], w=[osum.k])
                    M.dve(lambda e: e.tensor_tensor(out=m_.t[:], in0=osum.t[:], in1=g.t[:], op=ALU.mult), r=[osum.k, g.k], w=[m_.k])
                    yield
                    M.dma("pool", self.mixs.t[rows, 0:256], m_.t[:], r=[m_.k], w=[self.mixs.k])

                todo = list(range(NT))
                active = []
                freeR = list(range(KR))

                def wrap(j, ri):
                    yield from ro(j, RS_[ri])
                    freeR.append(ri)

                while todo or active:
                    while todo and freeR:
                        active.append(wrap(todo.pop(0), freeR.pop(0)))
                    for gth in list(active):
                        try:
                            next(gth)
                        except StopIteration:
                            active.remove(gth)
                M.barrier()


class ProgV4(ProgV3):
    def stage_mid(self, l, es, xres, h2T, comb, last=False):
        from contextlib import ExitStack
        M, nc, I = self.M, self.nc, self.I
        cst = self.cst
        C = cst.t
        with ExitStack() as e2:
            wout = self.sb(e2, "wout", [128, 8, D], BF16)
            wv = I["w_out"][l].rearrange("(k p) n -> p k n", p=128)
            for k in range(8):
                M.dma("pool", wout.t[:, k, :], wv[:, k, :], w=[wout.k])
            g1b = [self.sb(e2, f"g1b{w}", [128, D]) for w in range(2)]
            for w in range(2):
                M.dma("sp", g1b[w].t[:], self.modrow(l, w, 2).partition_broadcast(128), r=[self.mods.k], w=[g1b[w].k])
            A, S = self.norm_mod_tiles(e2, l, "norm2_gain", 4, 3)
            wr = self.sb(e2, "wr", [128, 8, 36])
            br = self.sb(e2, "br", [1, 36])
            M.dma("sp", wr.t[:], I["w_router"][l].rearrange("(k p) n -> p k n", p=128), w=[wr.k])
            M.dma("sp", br.t[:], I["b_router"][l:l + 1, :], w=[br.k])
            NS = 2
            SETS = []
            for i in range(NS):
                t = {}
                t["mx"] = self.sb(e2, f"mx{i}", [128, D], BF16)
                t["mT"] = self.sb(e2, f"mT{i}", [128, 8, 128], BF16)
                t["tmp"] = self.sb(e2, f"tmpo{i}", [128, 512])
                t["sq"] = self.sb(e2, f"sq2{i}", [128, D], BF16)
                t["ss"] = self.sb(e2, f"ss2{i}", [128, 1])
                t["hf"] = self.sb(e2, f"hf2{i}", [128, D])
                t["hb"] = self.sb(e2, f"hb2{i}", [128, D], BF16)
                t["hTf"] = self.sb(e2, f"hTf{i}", [128, 8, 128])
                t["lg"] = self.sb(e2, f"lg{i}", [128, 36])
                for n in ("oh", "esel", "mk1", "e2", "mk2", "ew"):
                    t[n] = self.sb(e2, f"{n}{i}", [128, 8])
                for n in ("gm", "ngm", "gs", "m1", "m2", "dm", "w1", "w2"):
                    t[n] = self.sb(e2, f"{n}{i}", [128, 1])
                t["junk"] = self.sb(e2, f"junk{i}", [128, 4])
                t["msk3"] = self.sb(e2, f"msk3{i}", [128, 4, 8])
                t["ptb"] = self.ps(e2, f"ptb{i}", [128, 8, 128], BF16)
                SETS.append(t)
            pool = [self.ps(e2, f"mp{i}", [128, 512]) for i in range(6)]
            busy = [False] * 6
            gi = [0]

            def alloc():
                for i in range(6):
                    kx = (gi[0] + 1 + i) % 6
                    if not busy[kx]:
                        busy[kx] = True
                        gi[0] = kx
                        return pool[kx]
                return None

            def rel(p):
                busy[pool.index(p)] = False

            def galloc():
                while True:
                    p = alloc()
                    if p is not None:
                        return p
                    yield

            xk = [Tk(f"x{j}") for j in range(NT)]
            hk = [Tk(f"h{j}") for j in range(NT)]
            ck = [Tk(f"c{j}") for j in range(NT)]

            def tile(j, T):
                rows = slice(j * 128, (j + 1) * 128)
                which = 1 if j < 2 else 0
                m_, mT, tmp, sq, ss, hf, hb, hTf, lg, junk, msk3, ptb = (T[n] for n in ("mx", "mT", "tmp", "sq", "ss", "hf", "hb", "hTf", "lg", "junk", "msk3", "ptb"))
                xj = xres.t[:, j, :]
                M.dma("sp", m_.t[:], self.mixs.t[rows, :], r=[self.mixs.k], w=[m_.k])
                if l == 0:
                    M.dma("sp", xj, I["x_all"][rows, :], w=[xk[j]])
                else:
                    M.dma("sp", xj, self.xs.t[rows, :], r=[self.xs.k], w=[xk[j]])
                yield
                for k in range(8):
                    M.pe(lambda e: e.transpose(ptb.t[:, k, :], m_.t[:, k * 128:(k + 1) * 128], self.idb.t[:]), r=[m_.k, self.idb.k], w=[ptb.k])
                yield
                M.act(lambda e: e.copy(out=mT.t[:], in_=ptb.t[:]), r=[ptb.k], w=[mT.k])
                yield
                for hf_ in range(2):
                    p_ = yield from galloc()
                    cs_ = slice(hf_ * 512, (hf_ + 1) * 512)
                    for k in range(8):
                        M.pe(lambda e: e.matmul(p_.t[:, :], lhsT=mT.t[:, k, :], rhs=wout.t[:, k, cs_], start=(k == 0), stop=(k == 7)), r=[mT.k, wout.k], w=[p_.k])
                    yield
                    M.dve(lambda e: e.tensor_tensor(out=tmp.t[:], in0=p_.t[:], in1=g1b[which].t[:, cs_], op=ALU.mult), r=[p_.k, g1b[which].k], w=[tmp.k])
                    rel(p_)
                    M.dve(lambda e: e.tensor_tensor(out=xres.t[:, j, cs_], in0=xres.t[:, j, cs_], in1=tmp.t[:], op=ALU.add), r=[xk[j], tmp.k], w=[xk[j]])
                yield
                M.act(lambda e: e.activation(out=sq.t[:], in_=xj, func=AF.Square, accum_out=ss.t[:]), r=[xk[j]], w=[sq.k, ss.k])
                yield
                M.dve(lambda e: e.tensor_scalar(out=ss.t[:], in0=ss.t[:], scalar1=1.0 / D, scalar2=EPS, op0=ALU.mult, op1=ALU.add), r=[ss.k], w=[ss.k])
                yield
                M.act(lambda e: e.activation(out=ss.t[:], in_=ss.t[:], func=AF.Sqrt), r=[ss.k], w=[ss.k])
                yield
                M.dve(lambda e: e.reciprocal(out=ss.t[:], in_=ss.t[:]), r=[ss.k], w=[ss.k])
                M.dve(lambda e: e.scalar_tensor_tensor(out=hf.t[:], in0=xj, scalar=ss.t[:, 0:1], in1=A[which].t[:], op0=ALU.mult, op1=ALU.mult),
                      r=[xk[j], ss.k, A[which].k], w=[hf.k])
                M.dve(lambda e: e.tensor_tensor(out=hf.t[:], in0=hf.t[:], in1=S[which].t[:], op=ALU.add), r=[hf.k, S[which].k], w=[hf.k])
                yield
                M.act(lambda e: e.copy(out=hb.t[:], in_=hf.t[:]), r=[hf.k], w=[hb.k])
                ptf = []
                for q4 in range(2):
                    pf = yield from galloc()
                    ptf.append(pf)
                    pfv = pf.t[:, :].rearrange("p (k t) -> p k t", k=4)
                    for k in range(4):
                        kk_ = q4 * 4 + k
                        M.pe(lambda e: e.transpose(pfv[:, k, :], hf.t[:, kk_ * 128:(kk_ + 1) * 128], C[:, C_ID:C_ID + 128]), r=[hf.k, cst.k], w=[pf.k])
                yield
                for k in range(8):
                    M.pe(lambda e: e.transpose(ptb.t[:, k, :], hb.t[:, k * 128:(k + 1) * 128], self.idb.t[:]), r=[hb.k, self.idb.k], w=[ptb.k])
                for q4 in range(2):
                    M.act(lambda e: e.copy(out=hTf.t[:, q4 * 4:(q4 + 1) * 4, :], in_=ptf[q4].t[:, :].rearrange("p (k t) -> p k t", k=4)), r=[ptf[q4].k], w=[hTf.k])
                    rel(ptf[q4])
                yield
                M.act(lambda e: e.copy(out=h2T.t[:, :, rows], in_=ptb.t[:]), r=[ptb.k], w=[hk[j]])
                plg = yield from galloc()
                for k in range(8):
                    M.pe(lambda e: e.matmul(plg.t[:, 0:36], lhsT=hTf.t[:, k, :], rhs=wr.t[:, k, :], start=(k == 0), stop=False), r=[hTf.k, wr.k], w=[plg.k])
                M.pe(lambda e: e.matmul(plg.t[:, 0:36], lhsT=self.ones.t[0:1, 0:128], rhs=br.t[0:1, :], start=False, stop=True), r=[self.ones.k, br.k], w=[plg.k])
                yield
                M.act(lambda e: e.copy(out=lg.t[:], in_=plg.t[:, 0:36]), r=[plg.k], w=[lg.k])
                rel(plg)
                yield
                G = lg.t[:, 0:4]
                E3 = lg.t[:, 4:36].rearrange("p (g e) -> p g e", g=4)
                oh, esel, mk1, e2_, mk2, ew = (T[n] for n in ("oh", "esel", "mk1", "e2", "mk2", "ew"))
                gm, ngm, gs, m1, m2, dm, w1, w2 = (T[n] for n in ("gm", "ngm", "gs", "m1", "m2", "dm", "w1", "w2"))
                M.dve(lambda e: e.tensor_reduce(out=gm.t[:], in_=G, axis=AX.X, op=ALU.max), r=[lg.k], w=[gm.k])
                M.dve(lambda e: e.tensor_scalar(out=oh.t[:, 0:4], in0=G, scalar1=gm.t[:, 0:1], scalar2=None, op0=ALU.is_equal), r=[lg.k, gm.k], w=[oh.k])
                M.dve(lambda e: e.tensor_scalar(out=ngm.t[:], in0=gm.t[:], scalar1=-1.0, scalar2=None, op0=ALU.mult), r=[gm.k], w=[ngm.k])
                M.dve(lambda e: e.tensor_tensor(out=msk3.t[:], in0=E3, in1=_bc(oh.t[:, 0:4], [128, 4, 8], 2), op=ALU.mult), r=[lg.k, oh.k], w=[msk3.k])
                M.dve(lambda e: e.tensor_reduce(out=esel.t[:], in_=msk3.t[:].rearrange("p g e -> p e g"), axis=AX.X, op=ALU.add), r=[msk3.k], w=[esel.k])
                M.dve(lambda e: e.tensor_reduce(out=m1.t[:], in_=esel.t[:], axis=AX.X, op=ALU.max), r=[esel.k], w=[m1.k])
                yield
                M.act(lambda e: e.activation(out=junk.t[:], in_=G, func=AF.Exp, bias=ngm.t[:, 0:1], accum_out=gs.t[:]), r=[lg.k, ngm.k], w=[junk.k, gs.k])
                M.dve(lambda e: e.tensor_scalar(out=mk1.t[:], in0=esel.t[:], scalar1=m1.t[:, 0:1], scalar2=None, op0=ALU.is_equal), r=[esel.k, m1.k], w=[mk1.k])
                M.dve(lambda e: e.scalar_tensor_tensor(out=e2_.t[:], in0=mk1.t[:], scalar=-1e30, in1=esel.t[:], op0=ALU.mult, op1=ALU.add), r=[mk1.k, esel.k], w=[e2_.k])
                M.dve(lambda e: e.tensor_reduce(out=m2.t[:], in_=e2_.t[:], axis=AX.X, op=ALU.max), r=[e2_.k], w=[m2.k])
                M.dve(lambda e: e.tensor_scalar(out=mk2.t[:], in0=e2_.t[:], scalar1=m2.t[:, 0:1], scalar2=None, op0=ALU.is_equal), r=[e2_.k, m2.k], w=[mk2.k])
                M.dve(lambda e: e.tensor_tensor(out=dm.t[:], in0=m2.t[:], in1=m1.t[:], op=ALU.subtract), r=[m1.k, m2.k], w=[dm.k])
                yield
                M.act(lambda e: e.activation(out=dm.t[:], in_=dm.t[:], func=AF.Exp), r=[dm.k], w=[dm.k])
                M.dve(lambda e: e.reciprocal(out=gs.t[:], in_=gs.t[:]), r=[gs.k], w=[gs.k])
                yield
                M.dve(lambda e: e.tensor_scalar(out=w1.t[:], in0=dm.t[:], scalar1=1.0, scalar2=None, op0=ALU.add), r=[dm.k], w=[w1.k])
                M.dve(lambda e: e.reciprocal(out=w1.t[:], in_=w1.t[:]), r=[w1.k], w=[w1.k])
                M.dve(lambda e: e.tensor_tensor(out=w1.t[:], in0=w1.t[:], in1=gs.t[:], op=ALU.mult), r=[w1.k, gs.k], w=[w1.k])
                M.dve(lambda e: e.tensor_tensor(out=w2.t[:], in0=w1.t[:], in1=dm.t[:], op=ALU.mult), r=[w1.k, dm.k], w=[w2.k])
                M.dve(lambda e: e.tensor_scalar(out=ew.t[:], in0=mk1.t[:], scalar1=w1.t[:, 0:1], scalar2=None, op0=ALU.mult), r=[mk1.k, w1.k], w=[ew.k])
                M.dve(lambda e: e.scalar_tensor_tensor(out=ew.t[:], in0=mk2.t[:], scalar=w2.t[:, 0:1], in1=ew.t[:], op0=ALU.mult, op1=ALU.add), r=[mk2.k, w2.k, ew.k], w=[ew.k])
                for g in range(4):
                    M.dve(lambda e: e.tensor_scalar(out=comb.t[:, j, g * 8:(g + 1) * 8], in0=ew.t[:], scalar1=oh.t[:, g:g + 1], scalar2=None, op0=ALU.mult),
                          r=[ew.k, oh.k], w=[ck[j]])
                yield

            todo = list(range(2 if last else 0, NT))
            active = []
            freeS = list(range(NS))

            def wrap(j, si_):
                yield from tile(j, SETS[si_])
                freeS.append(si_)

            while todo or active:
                while todo and freeS:
                    active.append(wrap(todo.pop(0), freeS.pop(0)))
                for gth in list(active):
                    try:
                        next(gth)
                    except StopIteration:
                        active.remove(gth)
            M.barrier()


class ProgV5(ProgV4):
    def stage_moe(self, l, xres, h2T, comb, last):
        from contextlib import ExitStack
        M, nc, I = self.M, self.nc, self.I
        with ExitStack() as e2:
            g2b = [self.sb(e2, f"g2b{w}", [128, D]) for w in range(2)]
            for w in range(2):
                M.dma("sp", g2b[w].t[:], self.modrow(l, w, 5).partition_broadcast(128), r=[self.mods.k], w=[g2b[w].k])
            Wg = [self.sb(e2, f"Wg{i}", [128, 8, 512], BF16) for i in range(2)]
            Wu = [self.sb(e2, f"Wu{i}", [128, 8, 512], BF16) for i in range(2)]
            Wd = [self.sb(e2, f"Wd{i}", [128, 4, D], BF16) for i in range(2)]
            hid = [self.sb(e2, f"hid{i}", [128, 4, 512], BF16) for i in range(2)]
            sg = [self.sb(e2, f"sg{i}", [128, 512]) for i in range(2)]
            tmp = [self.sb(e2, f"tmpm{i}", [128, 512]) for i in range(3)]
            pG = [self.ps(e2, f"pG{i}", [128, 512]) for i in range(2)]
            pU = [self.ps(e2, f"pU{i}", [128, 512]) for i in range(2)]
            pD = [self.ps(e2, f"pD{i}", [128, 512]) for i in range(4)]
            xk = [Tk(f"mx{j}") for j in range(NT)]
            chunks = list(range(256 if last else 0, TALL, 512))
            work = [(ge, ci) for ge in range(32) for ci in range(len(chunks))]
            st = {"gu_done": -1, "d_done": -1}

            def gu():
                cnt = 0
                for wi, (ge, ci) in enumerate(work):
                    b = ge % 2
                    while st["d_done"] < wi - 2:
                        yield
                    if ci == 0:
                        gv = I["w_exp_gate"][l, ge].rearrange("(k p) f -> p k f", p=128)
                        uv = I["w_exp_up"][l, ge].rearrange("(k p) f -> p k f", p=128)
                        dv = I["w_exp_down"][l, ge].rearrange("(c p) d -> p c d", p=128)
                        for k in range(0, 8, 2):
                            M.dma("pool", Wg[b].t[:, k:k + 2, :], gv[:, k:k + 2, :], w=[Wg[b].k])
                            M.dma("pool", Wu[b].t[:, k:k + 2, :], uv[:, k:k + 2, :], w=[Wu[b].k])
                        for c in range(4):
                            M.dma("pool", Wd[b].t[:, c, :], dv[:, c, :], w=[Wd[b].k])
                    t0 = chunks[ci]
                    t1 = min(TALL, t0 + 512)
                    n = t1 - t0
                    hd = hid[wi % 2]
                    for fc in range(4):
                        cnt += 1
                        g_, u_, s_ = pG[cnt % 2], pU[cnt % 2], sg[cnt % 2]
                        for k in range(8):
                            M.pe(lambda e: e.matmul(g_.t[:, 0:n], lhsT=Wg[b].t[:, k, fc * 128:(fc + 1) * 128], rhs=h2T.t[:, k, t0:t1], start=(k == 0), stop=(k == 7)),
                                 r=[Wg[b].k, h2T.k], w=[g_.k])
                        yield
                        for k in range(8):
                            M.pe(lambda e: e.matmul(u_.t[:, 0:n], lhsT=Wu[b].t[:, k, fc * 128:(fc + 1) * 128], rhs=h2T.t[:, k, t0:t1], start=(k == 0), stop=(k == 7)),
                                 r=[Wu[b].k, h2T.k], w=[u_.k])
                        M.act(lambda e: e.activation(out=s_.t[:, 0:n], in_=g_.t[:, 0:n], func=AF.Silu), r=[g_.k], w=[s_.k])
                        yield
                        M.dve(lambda e: e.tensor_tensor(out=hd.t[:, fc, 0:n], in0=s_.t[:, 0:n], in1=u_.t[:, 0:n], op=ALU.mult), r=[s_.k, u_.k], w=[hd.k])
                    st["gu_done"] = wi
                    yield

            def dn():
                cnt = 0
                for wi, (ge, ci) in enumerate(work):
                    b = ge % 2
                    while st["gu_done"] < wi:
                        yield
                    t0 = chunks[ci]
                    t1 = min(TALL, t0 + 512)
                    n = t1 - t0
                    hd = hid[wi % 2]
                    for jt in range(n // 128):
                        j = t0 // 128 + jt
                        which = 1 if j < 2 else 0
                        for hf_ in range(2):
                            cnt += 1
                            d_, tm = pD[cnt % 4], tmp[cnt % 3]
                            cs_ = slice(hf_ * 512, (hf_ + 1) * 512)
                            for fc in range(4):
                                M.pe(lambda e: e.matmul(d_.t[:, :], lhsT=hd.t[:, fc, jt * 128:(jt + 1) * 128], rhs=Wd[b].t[:, fc, cs_], start=(fc == 0), stop=(fc == 3)),
                                     r=[hd.k, Wd[b].k], w=[d_.k])
                            M.dve(lambda e: e.tensor_tensor(out=tm.t[:], in0=d_.t[:], in1=g2b[which].t[:, cs_], op=ALU.mult), r=[d_.k, g2b[which].k], w=[tm.k])
                            M.dve(lambda e: e.scalar_tensor_tensor(out=xres.t[:, j, cs_], in0=tm.t[:], scalar=comb.t[:, j, ge:ge + 1], in1=xres.t[:, j, cs_],
                                                                   op0=ALU.mult, op1=ALU.add), r=[tm.k, comb.k, xk[j]], w=[xk[j]])
                            yield
                        if ge == 31:
                            if last:
                                M.dma("sp", self.out[(j - 2) * 128:(j - 1) * 128, :], xres.t[:, j, :], r=[xk[j]], w=[self.xs.k])
                            else:
                                M.dma("sp", self.xs.t[j * 128:(j + 1) * 128, :], xres.t[:, j, :], r=[xk[j]], w=[self.xs.k])
                    st["d_done"] = wi

            run_streams([gu(), dn()])
            M.barrier()
```
